# Optimizing a Trainium2 kernel written in Bass

```python
import jax, jax.numpy as jnp
from jax import lax
import numpy as np

D_MODEL = 1024
BATCH = 4
SEQ = 4096
DEPTH = 1

GRID_W = 64
EPS = 1e-6
D_CONV = 1024
CONV_WIDTH = 3
N_HEADS = 8
N_KV_HEADS = 2
HEAD_DIM = 128
AXIS_DIM = HEAD_DIM // 2
ROPE_THETA = 10000.0
Q_BLOCK = 128
D_Q = N_HEADS * HEAD_DIM
D_KV = N_KV_HEADS * HEAD_DIM
N_GROUPS = 8
EXPERTS_PER_GROUP = 8
N_EXPERTS = N_GROUPS * EXPERTS_PER_GROUP
TOP_K = 2
D_EXPERT = 512
MOE_BLOCK = 128
IN_SPLITS = (D_CONV, D_CONV, D_CONV, D_Q, D_KV, D_KV, D_MODEL, D_MODEL)
D_IN = D_CONV * 3 + D_Q + 2 * D_KV + 2 * D_MODEL

kernel_name = "hybrid_conv_attn_hiermoe_block"


def rmsnorm(x, g):
    xf = x.astype(jnp.float32)
    y = xf * lax.rsqrt(jnp.mean(xf * xf, axis=-1, keepdims=True) + EPS)
    return (y * g.astype(jnp.float32)).astype(x.dtype)


def split_columns(p):
    parts, off = [], 0
    for w in IN_SPLITS:
        parts.append(p[..., off:off + w])
        off += w
    return parts


def axial_rope_tables(S):
    rows = S // GRID_W
    row = jnp.repeat(jnp.arange(rows, dtype=jnp.float32), GRID_W)
    col = jnp.tile(jnp.arange(GRID_W, dtype=jnp.float32), rows)
    inv = ROPE_THETA ** (-jnp.arange(0, AXIS_DIM, 2, dtype=jnp.float32) / AXIS_DIM)
    ang = jnp.concatenate([row[:, None] * inv, col[:, None] * inv], axis=-1)
    return jnp.cos(ang), jnp.sin(ang)


def apply_rope(x, cos, sin):
    xf = x.astype(jnp.float32).reshape(*x.shape[:-1], HEAD_DIM // 2, 2)
    x0, x1 = xf[..., 0], xf[..., 1]
    c = cos[None, :, None, :]
    s = sin[None, :, None, :]
    out = jnp.stack([x0 * c - x1 * s, x0 * s + x1 * c], axis=-1)
    return out.reshape(x.shape).astype(x.dtype)


def bidirectional_gqa(q, k, v):
    B, S = q.shape[0], q.shape[1]
    G = N_HEADS // N_KV_HEADS
    nq = S // Q_BLOCK
    qb = q.reshape(B, nq, Q_BLOCK, N_KV_HEADS, G, HEAD_DIM).transpose(1, 0, 2, 3, 4, 5)

    def attend(qblk):
        s = jnp.einsum('bqkgd,bskd->bkgqs', qblk, k, preferred_element_type=jnp.float32)
        p = jax.nn.softmax(s, axis=-1)
        return jnp.einsum('bkgqs,bskd->bqkgd', p.astype(v.dtype), v)

    o = lax.map(attend, qb)
    return o.transpose(1, 0, 2, 3, 4, 5).reshape(B, S, D_Q)


def depthwise_conv_centred(z, w):
    rhs = w.reshape(CONV_WIDTH, 1, D_CONV)
    pad = (CONV_WIDTH - 1) // 2
    return lax.conv_general_dilated(z, rhs, window_strides=(1,), padding=((pad, pad),),
                                    dimension_numbers=('NWC', 'WIO', 'NWC'),
                                    feature_group_count=D_CONV)


def mixer_block(u, cos, sin, w_in, conv_w, q_norm_g, k_norm_g, w_conv_out, w_attn_out, w_o):
    B, S, _ = u.shape
    proj = u @ w_in
    cb, cc, cx, q, k, v, gc, ga = split_columns(proj)
    z = depthwise_conv_centred(cc * cx, conv_w)
    y_conv = (cb * z) @ w_conv_out
    q = rmsnorm(q.reshape(B, S, N_HEADS, HEAD_DIM), q_norm_g)
    k = rmsnorm(k.reshape(B, S, N_KV_HEADS, HEAD_DIM), k_norm_g)
    v = v.reshape(B, S, N_KV_HEADS, HEAD_DIM)
    q = apply_rope(q, cos, sin) * (HEAD_DIM ** -0.5)
    k = apply_rope(k, cos, sin)
    y_attn = bidirectional_gqa(q, k, v) @ w_attn_out
    merged = jax.nn.sigmoid(gc) * y_conv + jax.nn.sigmoid(ga) * y_attn
    return merged @ w_o


def hier_moe(u, w_group, w_router, w1, w3, w2):
    B, S, D = u.shape
    T = B * S
    hf = u.reshape(T, D)
    grp_logits = (hf @ w_group).astype(jnp.float32)
    grp_prob = jax.nn.softmax(grp_logits, axis=-1)
    g = jnp.argmax(grp_logits, axis=-1)
    pg = jnp.take_along_axis(grp_prob, g[:, None], axis=1)
    exp_logits = (hf @ w_router).astype(jnp.float32).reshape(T, N_GROUPS, EXPERTS_PER_GROUP)
    sel = jnp.take_along_axis(exp_logits, g[:, None, None], axis=1)[:, 0]
    top_v, top_i = lax.top_k(sel, TOP_K)
    weights = pg * jax.nn.softmax(top_v, axis=-1)
    expert_ids = g[:, None] * EXPERTS_PER_GROUP + top_i
    A = T * TOP_K
    e_flat = expert_ids.reshape(A).astype(jnp.int32)
    w_flat = weights.reshape(A)
    tok_flat = jnp.arange(A, dtype=jnp.int32) // TOP_K
    order = jnp.argsort(e_flat)
    se, st, sw = e_flat[order], tok_flat[order], w_flat[order]
    counts = jnp.bincount(e_flat, length=N_EXPERTS).astype(jnp.int32)
    starts = jnp.cumsum(counts) - counts
    pcounts = ((counts + MOE_BLOCK - 1) // MOE_BLOCK) * MOE_BLOCK
    pends = jnp.cumsum(pcounts)
    pstarts = pends - pcounts
    dest = pstarts[se] + (jnp.arange(A, dtype=jnp.int32) - starts[se])
    P = A + N_EXPERTS * MOE_BLOCK
    NB = P // MOE_BLOCK
    row_tok = jnp.full((P,), T, jnp.int32).at[dest].set(st)
    row_w = jnp.zeros((P,), jnp.float32).at[dest].set(sw)
    blk_expert = jnp.minimum(
        jnp.searchsorted(pends, jnp.arange(NB, dtype=jnp.int32) * MOE_BLOCK, side='right'),
        N_EXPERTS - 1).astype(jnp.int32)
    x_pad = jnp.concatenate([hf, jnp.zeros((1, D), hf.dtype)], axis=0)
    xs = x_pad[row_tok].reshape(NB, MOE_BLOCK, D)

    def expert_block(args):
        xb, e = args
        return (jax.nn.silu(xb @ w1[e]) * (xb @ w3[e])) @ w2[e]

    ys = lax.map(expert_block, (xs, blk_expert)).reshape(P, D)
    ys = ys * row_w[:, None].astype(ys.dtype)
    out = jnp.zeros((T + 1, D), ys.dtype).at[row_tok].add(ys)[:T]
    return out.reshape(B, S, D)


def setup_inputs(seed: int = 0) -> dict:
    key = jax.random.key(seed)
    ks = jax.random.split(key, 16)

    def nrm(k, shape, scale):
        return jax.random.normal(k, shape, jnp.float32) * scale

    return {
        "x": nrm(ks[0], (BATCH, SEQ, D_MODEL), 1.0),
        "norm1_g": 1.0 + nrm(ks[1], (DEPTH, D_MODEL), 0.02),
        "w_in": nrm(ks[2], (DEPTH, D_MODEL, D_IN), D_MODEL ** -0.5),
        "conv_w": nrm(ks[3], (DEPTH, CONV_WIDTH, D_CONV), CONV_WIDTH ** -0.5),
        "q_norm_g": 1.0 + nrm(ks[4], (DEPTH, HEAD_DIM), 0.02),
        "k_norm_g": 1.0 + nrm(ks[5], (DEPTH, HEAD_DIM), 0.02),
        "w_conv_out": nrm(ks[6], (DEPTH, D_CONV, D_MODEL), D_CONV ** -0.5),
        "w_attn_out": nrm(ks[7], (DEPTH, D_Q, D_MODEL), D_Q ** -0.5),
        "w_o": nrm(ks[8], (DEPTH, D_MODEL, D_MODEL), D_MODEL ** -0.5),
        "norm2_g": 1.0 + nrm(ks[9], (DEPTH, D_MODEL), 0.02),
        "w_group": nrm(ks[10], (DEPTH, D_MODEL, N_GROUPS), D_MODEL ** -0.5),
        "w_router": nrm(ks[11], (DEPTH, D_MODEL, N_EXPERTS), D_MODEL ** -0.5),
        "w1": nrm(ks[12], (DEPTH, N_EXPERTS, D_MODEL, D_EXPERT), D_MODEL ** -0.5),
        "w3": nrm(ks[13], (DEPTH, N_EXPERTS, D_MODEL, D_EXPERT), D_MODEL ** -0.5),
        "w2": nrm(ks[14], (DEPTH, N_EXPERTS, D_EXPERT, D_MODEL), D_EXPERT ** -0.5),
    }


def reference(x, norm1_g, w_in, conv_w, q_norm_g, k_norm_g, w_conv_out, w_attn_out, w_o,
              norm2_g, w_group, w_router, w1, w3, w2):
    S = x.shape[1]
    cos, sin = axial_rope_tables(S)
    h = x
    for l in range(DEPTH):
        h = h + mixer_block(rmsnorm(h, norm1_g[l]), cos, sin, w_in[l], conv_w[l],
                            q_norm_g[l], k_norm_g[l], w_conv_out[l], w_attn_out[l], w_o[l])
        h = h + hier_moe(rmsnorm(h, norm2_g[l]), w_group[l], w_router[l], w1[l], w3[l], w2[l])
    return h
```

```python
import contextlib
import numpy as np
import concourse.bass as bass
import concourse.mybir as mybir
from concourse.bass_utils import run_bass_kernel_spmd

F32 = mybir.dt.float32
BF16 = mybir.dt.bfloat16
I32 = mybir.dt.int32
U8 = mybir.dt.uint8
AF = mybir.ActivationFunctionType
ALU = mybir.AluOpType
AX = mybir.AxisListType

NT_OWN = 16
NT_ALL = 32
CAP = 128
NEXP = 64
EPS = 1e-6


class Op:
    __slots__ = ("eng", "fn", "idx", "waits", "inc", "count", "is_dma", "semkey")


class Sched:
    ENGS = ("sync", "scalar", "vector", "gpsimd", "tensor")

    def __init__(self, nc):
        self.nc = nc
        self.ops = {e: [] for e in self.ENGS}
        self.tiles = {}
        self.dma_count = {}

    def add(self, eng, fn, reads=(), writes=(), semkey=None):
        op = Op()
        op.eng = eng; op.fn = fn; op.idx = len(self.ops[eng]); op.inc = False
        op.is_dma = semkey is not None; op.semkey = semkey; op.count = None
        deps = []
        for k in reads:
            st = self.tiles.get(k)
            if st and st[0] is not None:
                deps.append(st[0])
        for k in writes:
            st = self.tiles.get(k)
            if st:
                if st[0] is not None:
                    deps.append(st[0])
                deps.extend(st[1])
        waits = {}
        for d in deps:
            if d.is_dma:
                key = ("dma", d.semkey); val = self.dma_count[d.semkey]
                waits[key] = max(waits.get(key, 0), val)
            else:
                if d.eng == "tensor" and eng == "tensor" and not op.is_dma:
                    continue
                d.inc = True
                key = ("eng", d.eng)
                cur = waits.get(key)
                if cur is None or d.idx > cur.idx:
                    waits[key] = d
        op.waits = waits
        if op.is_dma:
            self.dma_count[semkey] = self.dma_count.get(semkey, 0) + 16
        for k in reads:
            st = self.tiles.setdefault(k, [None, []])
            st[1].append(op)
        for k in writes:
            self.tiles[k] = [op, []]
        self.ops[eng].append(op)
        return op

    def barrier(self):
        last = {}
        for e in self.ENGS:
            for op in reversed(self.ops[e]):
                if not op.is_dma and op.fn is not None:
                    last[e] = op
                    break
        dma_snapshot = dict(self.dma_count)
        for e in self.ENGS:
            op = Op()
            op.eng = e; op.fn = None; op.idx = len(self.ops[e]); op.inc = False
            op.is_dma = False; op.semkey = None; op.count = None
            waits = {}
            for f, lo in last.items():
                if f == "tensor" and e == "tensor":
                    continue
                lo.inc = True
                waits[("eng", f)] = lo
            for k, v in dma_snapshot.items():
                waits[("dma", k)] = v
            op.waits = waits
            self.ops[e].append(op)
        self.tiles.clear()

    def emit(self, final_eng="gpsimd"):
        nc = self.nc
        for e in self.ENGS:
            c = 0
            for op in self.ops[e]:
                if op.inc and not op.is_dma:
                    c += 1
                    op.count = c
        semkeys = sorted(self.dma_count.keys())
        with contextlib.ExitStack() as st:
            esem = {e: st.enter_context(nc.semaphore("es_" + e)) for e in self.ENGS}
            dsem = {k: st.enter_context(nc.semaphore("ds_" + str(k))) for k in semkeys}
            block = st.enter_context(nc.Block())

            def run(e, eng):
                known = {}
                for op in self.ops[e]:
                    for key, val in op.waits.items():
                        if key[0] == "dma":
                            sem = dsem[key[1]]; v = val
                        else:
                            sem = esem[key[1]]; v = val.count
                        if known.get(key, 0) >= v:
                            continue
                        known[key] = v
                        eng.wait_ge(sem, v)
                    if op.fn is None:
                        continue
                    ins = op.fn(eng)
                    if op.is_dma:
                        ins.then_inc(dsem[op.semkey], 16)
                    elif op.inc:
                        ins.then_inc(esem[e], 1)
                if e == final_eng:
                    for k in semkeys:
                        if known.get(("dma", k), 0) < self.dma_count[k]:
                            eng.wait_ge(dsem[k], self.dma_count[k])

            @block.sync
            def _(eng): run("sync", eng)

            @block.scalar
            def _(eng): run("scalar", eng)

            @block.vector
            def _(eng): run("vector", eng)

            @block.gpsimd
            def _(eng): run("gpsimd", eng)

            @block.tensor
            def _(eng): run("tensor", eng)


def build(stop=99, taps=()):
    nc = bass.Bass("TRN2", target_bir_lowering=False)

    def din(name, shape, dt=F32):
        return nc.dram_tensor(name, list(shape), dt, kind="ExternalInput").ap()

    xa = din("xa", [4224, 1024])
    cs = din("cs", [4096, 128])
    norm1_g = din("norm1_g", [1024])
    norm2_g = din("norm2_g", [1024])
    w_in = din("w_in", [1024, 6656])
    conv_w = din("conv_w", [3, 1024])
    q_norm_g = din("q_norm_g", [128])
    k_norm_g = din("k_norm_g", [128])
    w_conv_out = din("w_conv_out", [1024, 1024])
    w_attn_out = din("w_attn_out", [1024, 1024])
    w_o = din("w_o", [1024, 1024])
    w_gr = din("w_gr", [1024, 72])
    w1 = din("w1", [NEXP, 1024, 512])
    w3 = din("w3", [NEXP, 1024, 512])
    w2 = din("w2", [NEXP, 512, 1024])
    consts = din("consts", [128, 512])
    out = nc.dram_tensor("out", [2048, 1024], F32, kind="ExternalOutput").ap()
    XS = nc.dram_tensor("XS", [NEXP * CAP, 1024], BF16).ap()
    YS = nc.dram_tensor("YS", [NEXP * CAP, 1024], F32).ap()
    tap_out = {}
    for (name, shape, dt) in taps:
        tap_out[name] = nc.dram_tensor("tap_" + name, list(shape), dt, kind="ExternalOutput").ap()

    with contextlib.ExitStack() as st:
        TOTAL = 212000
        arena = st.enter_context(nc.sbuf_tensor("arena", [128, TOTAL], U8))
        PS = st.enter_context(nc.psum_tensor("ps", [128, 8, 512], F32))
        PSb = PS[:].bitcast(BF16) if hasattr(PS[:], "bitcast") else None

        def region(off, nbytes, dt):
            assert off % 4 == 0 and off + nbytes <= TOTAL, (off, nbytes)
            return arena[:, off:off + nbytes].bitcast(dt)

        S = Sched(nc)

        def V(fn, r=(), w=()): return S.add("vector", fn, r, w)
        def A(fn, r=(), w=()): return S.add("scalar", fn, r, w)
        def G(fn, r=(), w=()): return S.add("gpsimd", fn, r, w)
        def T(fn, r=(), w=()): return S.add("tensor", fn, r, w)
        def D(q, key, fn, r=(), w=()): return S.add(q, fn, r, w, semkey=key)

        def psb(bank):
            return PSb[:, bank, :]

        P0 = 198000
        p = [P0]

        def palloc(nbytes, dt):
            r = region(p[0], nbytes, dt); p[0] += (nbytes + 3) // 4 * 4
            return r
        cst = palloc(512 * 4, F32)
        id_f = cst[:, 0:128]; ebase = cst[:, 384:448]
        cstb = palloc(384 * 2, BF16)
        id_b = cstb[:, 0:128]; tri_b = cstb[:, 128:256]; ones_b = cstb[:, 256:384]
        g1T = palloc(8 * 4, F32)
        cw = palloc(24 * 4, F32).rearrange("p (k c) -> p k c", k=3)
        gq_bc = palloc(128 * 4, F32)
        gk_bc = palloc(128 * 4, F32)
        negc = palloc(4 * 4, F32)
        wgr = palloc(8 * 72 * 4, F32).rearrange("p (c n) -> p c n", c=8)
        Wk = palloc(2 * 16 * 4, F32).rearrange("p (k t) -> p k t", k=2)
        idx = palloc(2 * 16 * 4, I32).rearrange("p (k t) -> p k t", k=2)
        Mb = palloc(16 * 64 * 2, BF16).rearrange("p (t e) -> p t e", t=16)
        g2_bc = palloc(1024 * 4, F32)
        assert p[0] <= TOTAL, p[0]

        OFF_A, OFF_B, OFF_QT, OFF_KT, OFF_VX, OFF_D, OFF_W = 0, 34816, 67584, 100352, 116736, 133376, 166144
        uT = region(OFF_A, 34816, BF16).rearrange("p (c t) -> p c t", c=8)
        Bbuf = region(OFF_B, 32768, BF16).rearrange("p (c t) -> p c t", c=8)
        QT = region(OFF_QT, 32768, BF16).rearrange("p (t h q) -> p t h q", t=16, h=8)
        KT = region(OFF_KT, 16384, BF16).rearrange("p (g t) -> p g t", g=2)
        VX = region(OFF_VX, 16640, BF16).rearrange("p (t g d) -> p t g d", t=32, g=2)
        OT = region(OFF_D, 32768, BF16).rearrange("p (c t) -> p c t", c=8)
        mT = region(OFF_QT, 32768, BF16).rearrange("p (c t) -> p c t", c=8)
        Wreg = OFF_W

        D("sync", "const", lambda e: e.dma_start(out=cst, in_=consts), w=["cst"])
        D("sync", "const", lambda e: e.dma_start(out=g1T, in_=norm1_g.rearrange("(c p) -> p c", p=128), allow_slow_non_contiguous=True), w=["g1T"])
        D("sync", "const", lambda e: e.dma_start(out=cw, in_=conv_w.rearrange("k (c p) -> p k c", p=128), allow_slow_non_contiguous=True), w=["cw"])
        D("sync", "const", lambda e: e.dma_start(out=gq_bc, in_=q_norm_g.partition_broadcast(128)), w=["gq"])
        D("sync", "const", lambda e: e.dma_start(out=gk_bc, in_=k_norm_g.partition_broadcast(128)), w=["gk"])
        D("sync", "const", lambda e: e.dma_start(out=g2_bc, in_=norm2_g.partition_broadcast(128)), w=["g2"])
        D("sync", "const", lambda e: e.dma_start(out=wgr, in_=w_gr.rearrange("(c p) n -> p c n", p=128)), w=["wgr"])
        V(lambda e: e.tensor_copy(out=cstb, in_=cst[:, 0:384]), ["cst"], ["cstb"])
        V(lambda e: e.tensor_reduce(out=negc[:, 0:1], in_=gq_bc, axis=AX.X, op=ALU.max, apply_absolute_value=True), ["gq"], ["negc0"])
        V(lambda e: e.tensor_reduce(out=negc[:, 1:2], in_=gk_bc, axis=AX.X, op=ALU.max, apply_absolute_value=True), ["gk"], ["negc1"])
        V(lambda e: e.tensor_tensor(out=negc[:, 2:3], in0=negc[:, 0:1], in1=negc[:, 1:2], op=ALU.mult), ["negc0", "negc1"], ["negc2"])
        V(lambda e: e.tensor_scalar(out=negc[:, 3:4], in0=negc[:, 2:3], scalar1=-(128.0 ** 0.5), scalar2=None, op0=ALU.mult), ["negc2"], ["negc"])
        V(lambda e: e.tensor_scalar(out=gq_bc, in0=gq_bc, scalar1=128.0 ** -0.5, scalar2=None, op0=ALU.mult), ["gq", "negc0"], ["gq"])
        bnd = {}

        def mk_bnd(e):
            bnd['r'] = e.alloc_register("bnd")
            return e.reg_mov(bnd['r'], NEXP * CAP - 1)
        G(mk_bnd)
        G(lambda e: e.memset(VX[:, :, :, 128:130], 1.0), [], ["vx1"])

        def tap(name, src_ap, keys):
            if name in tap_out:
                D("gpsimd", "tap_" + name, lambda e: e.dma_start(out=tap_out[name], in_=src_ap), r=keys)

        zt = region(OFF_B, 2048, BF16)
        G(lambda e: e.memset(zt, 0.0), [], ["zt"])
        for i in range(8):
            D("gpsimd", "xsz", lambda e, i=i: e.dma_start(out=XS[i * 1024:(i + 1) * 1024, :].rearrange("(a p) n -> p a n", p=128), in_=zt.unsqueeze(1).to_broadcast([128, 8, 1024])), r=["zt"], w=["XS"])

        wqkv = region(Wreg, 24576, BF16).rearrange("p (c n) -> p c n", c=8)
        o = [OFF_B + 2048]

        def salloc(nbytes, dt, lim=OFF_B + 32768):
            r = region(o[0], nbytes, dt); o[0] += (nbytes + 3) // 4 * 4
            assert o[0] <= lim, (o[0], lim)
            return r
        stg = [salloc(6144, F32) for _ in range(2)]
        xt = [salloc(4096, F32) for _ in range(3)]
        sqb = salloc(5120, F32)
        o = [OFF_D]

        def salloc2(nbytes, dt):
            return salloc(nbytes, dt, lim=OFF_D + 32768)
        xn = [salloc2(2048, BF16) for _ in range(2)]
        qn = salloc2(5120, F32)
        tmp4 = [salloc2(2560, F32) for _ in range(4)]
        qr = salloc2(2560, BF16)
        cst_t = [salloc2(512, F32) for _ in range(3)]
        st1 = salloc2(64 * 4, F32)
        uTo = [salloc2(2048, BF16).rearrange("p (c t) -> p c t", c=8) for _ in range(2)]

        for kc in range(8):
            s = kc % 2
            D("sync", f"stg{s}", lambda e, kc=kc, s=s: e.dma_start(out=stg[s], in_=w_in[kc * 128:(kc + 1) * 128, 3072:4608]), w=[f"stg{s}"])
            G(lambda e, kc=kc, s=s: e.tensor_copy(out=wqkv[:, kc, :], in_=stg[s]), [f"stg{s}"], [f"wqkv{kc}"])
        wq_keys = [f"wqkv{kc}" for kc in range(8)]

        tile_order = []
        for i in range(16):
            tile_order += [i, 16 + i]
        tile_order.append(32)

        def load_x(n):
            if n >= len(tile_order):
                return
            t = tile_order[n]; s = n % 3
            D("sync", f"xt{s}", lambda e: e.dma_start(out=xt[s], in_=xa[t * 128:(t + 1) * 128, :]), w=[f"xt{s}"])
            if t < 32:
                D("sync", f"cs{s}", lambda e: e.dma_start(out=cst_t[s], in_=cs[t * 128:(t + 1) * 128, :]), w=[f"cs{s}"])

        load_x(0); load_x(1)

        def p1_tile(n, t):
            load_x(n + 2)
            s = n % 3; s2 = n % 2
            own = t < 16; halo = t == 32
            xts = xt[s]; xns = xn[s2]
            ss = st1[:, 0:1]; rstd = st1[:, 1:2]
            V(lambda e: e.memset(ss, 0.0), [], ["ss"])
            A(lambda e: e.activation(out=sqb[:, 0:1024], in_=xts, func=AF.Square, accum_out=ss), [f"xt{s}", "ss"], ["sqb", "ss"])
            V(lambda e: e.tensor_scalar(out=rstd, in0=ss, scalar1=1.0 / 1024, scalar2=EPS, op0=ALU.mult, op1=ALU.add), ["ss"], ["rstd"])
            A(lambda e: e.activation(out=rstd, in_=rstd, func=AF.Sqrt), ["rstd"], ["rstd"])
            V(lambda e: e.reciprocal(out=rstd, in_=rstd), ["rstd"], ["rstd"])
            A(lambda e: e.activation(out=xns, in_=xts, func=AF.Copy, scale=rstd), [f"xt{s}", "rstd"], [f"xn{s2}"])
            pt = psb(s2).rearrange("p (c q) -> p c q", c=8)
            for c in range(8):
                T(lambda e, c=c: e.transpose(out=pt[:, c, :], in_=xns[:, c * 128:(c + 1) * 128], identity=id_b), [f"xn{s2}", "cstb"], [("ps", s2)])
            if own or halo:
                col = t * 128 if own else 2048
                udst = uT[:, :, col:col + 128]; ukey = ("uT", col // 128)
            else:
                udst = uTo[s2]; ukey = ("uTo", s2)
            V(lambda e: e.tensor_tensor(out=udst, in0=pt, in1=g1T.unsqueeze(2).to_broadcast([128, 8, 128]), op=ALU.mult), [("ps", s2), "g1T"], [ukey])
            if halo:
                return
            nq = 10 if own else 2
            h0 = 0 if own else 8
            for kc in range(8):
                lhs = udst[:, kc, :]
                if own:
                    T(lambda e, kc=kc, lhs=lhs: e.matmul(PS[:, 2, :], lhsT=lhs, rhs=wqkv[:, kc, 0:512], start=(kc == 0), stop=(kc == 7)), [ukey] + wq_keys, [("ps", 2)])
                    T(lambda e, kc=kc, lhs=lhs: e.matmul(PS[:, 3, :], lhsT=lhs, rhs=wqkv[:, kc, 512:1024], start=(kc == 0), stop=(kc == 7)), [ukey], [("ps", 3)])
                T(lambda e, kc=kc, lhs=lhs: e.matmul(PS[:, 4, :], lhsT=lhs, rhs=wqkv[:, kc, 1024:1536], start=(kc == 0), stop=(kc == 7)), [ukey] + wq_keys, [("ps", 4)])
            qkps = PS[:, 2:5, :].rearrange("p b n -> p (b n)")
            c0 = h0 * 128; c1 = 1280
            pkeys = [("ps", 2), ("ps", 3), ("ps", 4)] if own else [("ps", 4)]
            A(lambda e: e.activation(out=sqb[:, c0:c1], in_=qkps[:, c0:c1], func=AF.Square), pkeys, ["sqb"])
            ssq = st1[:, 16 + h0:26]; rs = st1[:, 32 + h0:42]
            V(lambda e: e.tensor_reduce(out=ssq, in_=sqb[:, c0:c1].rearrange("p (h d) -> p h d", d=128), axis=AX.X, op=ALU.add), ["sqb"], ["ssq"])
            V(lambda e: e.tensor_scalar(out=rs, in0=ssq, scalar1=1.0 / 128, scalar2=EPS, op0=ALU.mult, op1=ALU.add), ["ssq"], ["rs"])
            A(lambda e: e.activation(out=rs, in_=rs, func=AF.Sqrt), ["rs"], ["rs"])
            V(lambda e: e.reciprocal(out=rs, in_=rs), ["rs"], ["rs"])
            qn3 = qn[:, c0:c1].rearrange("p (h d) -> p h d", d=128)
            V(lambda e: e.tensor_tensor(out=qn3, in0=qkps[:, c0:c1].rearrange("p (h d) -> p h d", d=128), in1=rs.unsqueeze(2).to_broadcast([128, nq, 128]), op=ALU.mult), pkeys + ["rs"], ["qn"])
            A(lambda e: e.activation(out=VX[:, t, :, 0:128], in_=PS[:, 4, 256:512].rearrange("p (g d) -> p g d", g=2), func=AF.Copy), [("ps", 4)], [("vx", t)])
            if own:
                G(lambda e: e.tensor_tensor(out=qn[:, 0:1024].rearrange("p (h d) -> p h d", d=128), in0=qn[:, 0:1024].rearrange("p (h d) -> p h d", d=128), in1=gq_bc.unsqueeze(1).to_broadcast([128, 8, 128]), op=ALU.mult), ["qn", "gq"], ["qn"])
            G(lambda e: e.tensor_tensor(out=qn[:, 1024:1280].rearrange("p (h d) -> p h d", d=128), in0=qn[:, 1024:1280].rearrange("p (h d) -> p h d", d=128), in1=gk_bc.unsqueeze(1).to_broadcast([128, 2, 128]), op=ALU.mult), ["qn", "gk"], ["qn"])
            q4 = qn[:, c0:c1].rearrange("p (h i two) -> p h i two", i=64, two=2)
            x0 = q4[:, :, :, 0]; x1 = q4[:, :, :, 1]
            r4 = qr[:, c0:c1].rearrange("p (h i two) -> p h i two", i=64, two=2)
            cosb = cst_t[s][:, 0:64].unsqueeze(1).to_broadcast([128, nq, 64])
            sinb = cst_t[s][:, 64:128].unsqueeze(1).to_broadcast([128, nq, 64])
            tt = [tmp4[i][:, 0:nq * 64].rearrange("p (h i) -> p h i", i=64) for i in range(4)]
            ck = f"cs{s}"
            V(lambda e: e.tensor_tensor(out=tt[0], in0=x0, in1=cosb, op=ALU.mult), ["qn", ck], ["t0"])
            G(lambda e: e.tensor_tensor(out=tt[1], in0=x1, in1=sinb, op=ALU.mult), ["qn", ck], ["t1"])
            V(lambda e: e.tensor_tensor(out=r4[:, :, :, 0], in0=tt[0], in1=tt[1], op=ALU.subtract), ["t0", "t1"], ["qr0"])
            G(lambda e: e.tensor_tensor(out=tt[2], in0=x0, in1=sinb, op=ALU.mult), ["qn", ck], ["t2"])
            V(lambda e: e.tensor_tensor(out=tt[3], in0=x1, in1=cosb, op=ALU.mult), ["qn", ck], ["t3"])
            G(lambda e: e.tensor_tensor(out=r4[:, :, :, 1], in0=tt[2], in1=tt[3], op=ALU.add), ["t2", "t3"], ["qr1"])
            pq = psb(5).rearrange("p (h q) -> p h q", h=8)
            pk = psb(6).rearrange("p (h q) -> p h q", h=8)
            if own:
                for h in range(8):
                    T(lambda e, h=h: e.transpose(out=pq[:, h, :], in_=qr[:, h * 128:(h + 1) * 128], identity=id_b), ["qr0", "qr1"], [("ps", 5)])
                A(lambda e: e.activation(out=QT[:, t, :, :], in_=pq, func=AF.Copy), [("ps", 5)], [("QT", t)])
            for g in range(2):
                T(lambda e, g=g: e.transpose(out=pk[:, g, :], in_=qr[:, (8 + g) * 128:(9 + g) * 128], identity=id_b), ["qr0", "qr1"], [("ps", 6)])
            V(lambda e: e.tensor_copy(out=KT[:, :, t * 128:(t + 1) * 128], in_=pk[:, 0:2, :]), [("ps", 6)], [("KT", t)])

        for n_, t_ in enumerate(tile_order):
            p1_tile(n_, t_)

        tap("uT", uT, [("uT", i) for i in range(17)])
        tap("QT", QT, [("QT", i) for i in range(16)])
        tap("KT", KT, [("KT", i) for i in range(32)])
        tap("VX", VX, [("vx", i) for i in range(32)] + ["vx1"])
        S.barrier()
        if stop <= 1:
            S.emit(); return nc

        o = [OFF_B]
        PTs = [salloc(1024, BF16) for _ in range(4)]
        Otok = [salloc(2048, BF16).rearrange("p (h d) -> p h d", h=8) for _ in range(2)]
        rd = salloc(64, F32)
        SB = [0, 1, 6]
        seq = [(qt, g, kt) for qt in range(16) for g in range(2) for kt in range(32)]
        nbias = negc[:, 3:4]

        def issue_S(i):
            qt, g, kt = seq[i]
            b = SB[i % 3]
            T(lambda e: e.matmul(PS[:, b, :].rearrange("p (h q) -> p h q", h=4), lhsT=KT[:, g, kt * 128:(kt + 1) * 128], rhs=QT[:, qt, 4 * g:4 * g + 4, :], start=True, stop=True), [], [("ps", b)])

        def oacc(qt, g, h):
            sset = (qt * 2 + g) % 2
            bank = 2 + 2 * sset + h // 2
            return PS[:, bank, :].rearrange("p (j n) -> p j n", j=2), bank, h % 2

        issue_S(0); issue_S(1)

        def p2_step(i, qt, g, kt):
            if i + 2 < len(seq):
                issue_S(i + 2)
            b = SB[i % 3]; ps_ = PTs[i % 4]; pk_ = ("PT", i % 4)
            A(lambda e: e.activation(out=ps_, in_=PS[:, b, :], func=AF.Exp, bias=nbias, scale=1.0), [("ps", b), "negc"], [pk_])
            for h in range(4):
                ov, bank, j = oacc(qt, g, h)
                T(lambda e, h=h, ov=ov, j=j: e.matmul(ov[:, j, 0:129], lhsT=ps_[:, h * 128:(h + 1) * 128], rhs=VX[:, kt, g, 0:129], start=(kt == 0 and j == 0), stop=(kt == 31), skip_group_check=True), [pk_], [("ps", bank)])
            if kt == 31:
                os_ = Otok[qt % 2]; ok_ = ("Otok", qt % 2)
                for hb in range(2):
                    ov, bank, _ = oacc(qt, g, hb * 2)
                    V(lambda e, ov=ov, hb=hb: e.reciprocal(out=rd[:, 2 * hb:2 * hb + 2], in_=ov[:, :, 128]), [("ps", bank)], [("rd", hb)])
                    V(lambda e, ov=ov, hb=hb: e.tensor_tensor(out=os_[:, 4 * g + 2 * hb:4 * g + 2 * hb + 2, :], in0=ov[:, :, 0:128], in1=rd[:, 2 * hb:2 * hb + 2].unsqueeze(2).to_broadcast([128, 2, 128]), op=ALU.mult), [("ps", bank), ("rd", hb)], [ok_])
                if g == 1:
                    pt = psb(7).rearrange("p (c q) -> p c q", c=8)
                    for c in range(8):
                        T(lambda e, c=c: e.transpose(out=pt[:, c, :], in_=os_[:, c, :], identity=id_b), [ok_], [("ps", 7)])
                    V(lambda e: e.tensor_copy(out=OT[:, :, qt * 128:(qt + 1) * 128], in_=pt), [("ps", 7)], [("OT", qt)])

        for i_, (qt_, g_, kt_) in enumerate(seq):
            p2_step(i_, qt_, g_, kt_)
        tap("OT", OT, [("OT", i) for i in range(16)])
        S.barrier()
        if stop <= 2:
            S.emit(); return nc

        o = [OFF_B]
        ybuf = salloc(2052 * 4, F32)
        zb = [salloc(2048, F32) for _ in range(2)]
        ccs = [salloc(2048, F32) for _ in range(2)]
        o = [OFF_KT]
        cstg3 = [salloc(4096, F32, lim=OFF_D).rearrange("p (c n) -> p c n", c=8) for _ in range(3)]
        wcb3 = [salloc(2048, BF16, lim=OFF_D).rearrange("p (c n) -> p c n", c=8) for _ in range(6)]
        nld = [0]

        def load_chunk(col0):
            s = nld[0] % 3; sb_ = nld[0] % 6; nld[0] += 1
            D("sync", f"cstg{s}", lambda e: e.dma_start(out=cstg3[s], in_=w_in[:, col0:col0 + 128].rearrange("(c p) n -> p c n", p=128)), w=[f"cstg{s}"])
            G(lambda e: e.tensor_copy(out=wcb3[sb_], in_=cstg3[s]), [f"cstg{s}"], [f"wcb{sb_}"])
            return wcb3[sb_], f"wcb{sb_}"

        def mm8(bank, wch, wkey, rhs_fn, rkeys, n=512):
            for kc in range(8):
                T(lambda e, kc=kc: e.matmul(PS[:, bank, 0:n], lhsT=wch[:, kc, :], rhs=rhs_fn(kc), start=(kc == 0), stop=(kc == 7)), [wkey] + rkeys, [("ps", bank)])

        ukeys_all = [("uT", i) for i in range(17)]
        chunks = [load_chunk(1024 + 0), load_chunk(2048 + 0), load_chunk(0)]
        bk = [0]

        def nb():
            b = bk[0] % 8; bk[0] += 1
            return b
        for j in range(8):
            wcc, wcx, wcb_ = chunks
            if j + 1 < 8:
                chunks = [load_chunk(1024 + (j + 1) * 128), load_chunk(2048 + (j + 1) * 128), load_chunk((j + 1) * 128)]
            for nt in range(5):
                if nt < 4:
                    rf = lambda kc, nt=nt: uT[:, kc, nt * 512:(nt + 1) * 512]; n = 512; ycol = 1 + nt * 512
                else:
                    rf = lambda kc: uT[:, kc, 2048:2050]; n = 2
                b1 = nb(); b2 = nb()
                mm8(b1, wcc[0], wcc[1], rf, [], n)
                mm8(b2, wcx[0], wcx[1], rf, [], n)
                cs_ = ccs[nt % 2]; ck = ("ccs", nt % 2)
                A(lambda e, b1=b1, n=n, cs_=cs_: e.activation(out=cs_[:, 0:n], in_=PS[:, b1, 0:n], func=AF.Copy), [("ps", b1)], [ck])
                if nt < 4:
                    V(lambda e, b2=b2, cs_=cs_, ycol=ycol: e.tensor_tensor(out=ybuf[:, ycol:ycol + 512], in0=PS[:, b2, :], in1=cs_, op=ALU.mult), [("ps", b2), ck], [("y", nt)])
                else:
                    V(lambda e, b2=b2, cs_=cs_: e.tensor_tensor(out=ybuf[:, 0:1], in0=PS[:, b2, 0:1], in1=cs_[:, 0:1], op=ALU.mult), [("ps", b2), ck], [("y", 4)])
                    V(lambda e, b2=b2, cs_=cs_: e.tensor_tensor(out=ybuf[:, 2049:2050], in0=PS[:, b2, 1:2], in1=cs_[:, 1:2], op=ALU.mult), [("ps", b2), ck], [("y", 5)])
            ykeys = [("y", i) for i in range(6)]
            for nt in range(4):
                z = zb[nt % 2]; zk = ("z", nt % 2)
                c0 = nt * 512
                G(lambda e, z=z, c0=c0, j=j: e.tensor_scalar(out=z, in0=ybuf[:, c0:c0 + 512], scalar1=cw[:, 0, j:j + 1], scalar2=None, op0=ALU.mult), ykeys + ["cw"], [zk])
                V(lambda e, z=z, c0=c0, j=j: e.scalar_tensor_tensor(out=z, in0=ybuf[:, c0 + 1:c0 + 513], scalar=cw[:, 1, j:j + 1], in1=z, op0=ALU.mult, op1=ALU.add), ykeys + [zk], [zk])
                V(lambda e, z=z, c0=c0, j=j: e.scalar_tensor_tensor(out=z, in0=ybuf[:, c0 + 2:c0 + 514], scalar=cw[:, 2, j:j + 1], in1=z, op0=ALU.mult, op1=ALU.add), ykeys + [zk], [zk])
                b3 = nb()
                mm8(b3, wcb_[0], wcb_[1], lambda kc, nt=nt: uT[:, kc, nt * 512:(nt + 1) * 512], [])
                V(lambda e, b3=b3, z=z, nt=nt, j=j: e.tensor_tensor(out=mT[:, j, nt * 512:(nt + 1) * 512], in0=PS[:, b3, :], in1=z, op=ALU.mult), [("ps", b3), zk], [("mT", j, nt)])
        tap("mT", mT, [("mT", j, nt) for j in range(8) for nt in range(4)])
        S.barrier()
        if stop <= 3:
            S.emit(); return nc

        wco = region(Wreg, 16384, BF16).rearrange("p (c n) -> p c n", c=8)
        wao = region(OFF_KT, 16384, BF16).rearrange("p (c n) -> p c n", c=8)
        o = [OFF_VX]
        wstg4 = [salloc(4096, F32, lim=OFF_D) for _ in range(2)]
        sg = [salloc(2048, F32, lim=OFF_D) for _ in range(2)]
        tm = [salloc(2048, F32, lim=OFF_D) for _ in range(2)]
        o = [Wreg + 16384]
        cstg4 = [salloc(4096, F32, lim=P0).rearrange("p (c n) -> p c n", c=8) for _ in range(2)]
        wcb4 = [salloc(2048, BF16, lim=P0).rearrange("p (c n) -> p c n", c=8) for _ in range(2)]
        nld[0] = 0

        def load_chunk2(col0):
            s = nld[0] % 2; nld[0] += 1
            D("sync", f"cstg{s}", lambda e: e.dma_start(out=cstg4[s], in_=w_in[:, col0:col0 + 128].rearrange("(c p) n -> p c n", p=128)), w=[f"cstg{s}"])
            G(lambda e: e.tensor_copy(out=wcb4[s], in_=cstg4[s]), [f"cstg{s}"], [f"wcb{s}"])
            return wcb4[s], f"wcb{s}"

        def load_big(wdst, wsrc, name, ceng, wstg):
            for kc in range(8):
                s = kc % 2
                D("sync", f"wstg{s}", lambda e, kc=kc, s=s: e.dma_start(out=wstg[s], in_=wsrc[kc * 128:(kc + 1) * 128, :]), w=[f"wstg{s}"])
                S.add(ceng, lambda e, kc=kc, s=s: e.tensor_copy(out=wdst[:, kc, :], in_=wstg[s]), [f"wstg{s}"], [(name, kc)])
            return [(name, kc) for kc in range(8)]
        kco = load_big(wco, w_conv_out, "wco", "vector", wstg4)
        kao = load_big(wao, w_attn_out, "wao", "gpsimd", wstg4)
        gch = [load_chunk2(4608), load_chunk2(5632)]
        def p4_step(oc, nt, wgc, wga):
            if True:
                tk = slice(nt * 512, (nt + 1) * 512)
                byc, bgc, bya, bga = nb(), nb(), nb(), nb()
                for kc in range(8):
                    T(lambda e, kc=kc, byc=byc: e.matmul(PS[:, byc, :], lhsT=wco[:, kc, oc * 128:(oc + 1) * 128], rhs=mT[:, kc, tk], start=(kc == 0), stop=(kc == 7)), kco, [("ps", byc)])
                mm8(bgc, wgc[0], wgc[1], lambda kc: uT[:, kc, tk], [])
                for kc in range(8):
                    T(lambda e, kc=kc, bya=bya: e.matmul(PS[:, bya, :], lhsT=wao[:, kc, oc * 128:(oc + 1) * 128], rhs=OT[:, kc, tk], start=(kc == 0), stop=(kc == 7)), kao, [("ps", bya)])
                mm8(bga, wga[0], wga[1], lambda kc: uT[:, kc, tk], [])
                A(lambda e, bgc=bgc: e.activation(out=sg[0], in_=PS[:, bgc, :], func=AF.Sigmoid), [("ps", bgc)], ["sg0"])
                A(lambda e, bga=bga: e.activation(out=sg[1], in_=PS[:, bga, :], func=AF.Sigmoid), [("ps", bga)], ["sg1"])
                V(lambda e, byc=byc: e.tensor_tensor(out=tm[0], in0=PS[:, byc, :], in1=sg[0], op=ALU.mult), [("ps", byc), "sg0"], ["tm0"])
                V(lambda e, bya=bya: e.tensor_tensor(out=tm[1], in0=PS[:, bya, :], in1=sg[1], op=ALU.mult), [("ps", bya), "sg1"], ["tm1"])
                G(lambda e: e.tensor_tensor(out=Bbuf[:, oc, tk], in0=tm[0], in1=tm[1], op=ALU.add), ["tm0", "tm1"], [("mg", oc, nt)])

        for oc in range(8):
            wgc, wga = gch
            for nt in range(4):
                p4_step(oc, nt, wgc, wga)
            if oc + 1 < 8:
                gch = [load_chunk2(4608 + (oc + 1) * 128), load_chunk2(5632 + (oc + 1) * 128)]
        tap("mg", Bbuf, [("mg", oc, nt) for oc in range(8) for nt in range(4)])
        S.barrier()
        if stop <= 4:
            S.emit(); return nc

        wo = region(Wreg, 16384, BF16).rearrange("p (c n) -> p c n", c=8)
        o = [OFF_A]
        wstg5 = [salloc(4096, F32, lim=OFF_B) for _ in range(2)]
        xt5 = [salloc(4096, F32, lim=OFF_B) for _ in range(2)]
        x1t = [salloc(4096, F32, lim=OFF_B) for _ in range(2)]
        u2f = salloc(4096, F32, lim=OFF_B)
        sq2 = salloc(4096, F32, lim=OFF_B)
        o = [OFF_QT]
        u2b = [salloc(2048, BF16, lim=OFF_D) for _ in range(2)]
        u2T = salloc(4096, F32, lim=OFF_D).rearrange("p (c q) -> p c q", c=8)
        lg = salloc(72 * 4, F32, lim=OFF_D)
        rt = salloc(256 * 4, F32, lim=OFF_D)
        M1 = salloc(64 * 4, F32, lim=OFF_D); M2 = salloc(64 * 4, F32, lim=OFF_D)
        Rk = salloc(64 * 4, F32, lim=OFF_D); tmpM = salloc(64 * 4, F32, lim=OFF_D)
        kwo = load_big(wo, w_o, "wo", "vector", wstg5)

        def ld_x5(t):
            if t < 16:
                D("sync", f"x5{t % 2}", lambda e: e.dma_start(out=xt5[t % 2], in_=xa[t * 128:(t + 1) * 128, :]), w=[("x5", t % 2)])
        ld_x5(0)

        def p5_tile(t):
            ld_x5(t + 1)
            s = t % 2
            for half in range(2):
                for kc in range(8):
                    T(lambda e, kc=kc, half=half: e.matmul(PS[:, half, :], lhsT=Bbuf[:, kc, t * 128:(t + 1) * 128], rhs=wo[:, kc, half * 512:(half + 1) * 512], start=(kc == 0), stop=(kc == 7)), kwo, [("ps", half)])
            x1 = x1t[s]; xk = ("x1", s)
            V(lambda e: e.tensor_tensor(out=x1.rearrange("p (b n) -> p b n", b=2), in0=PS[:, 0:2, :], in1=xt5[s].rearrange("p (b n) -> p b n", b=2), op=ALU.add), [("ps", 0), ("ps", 1), ("x5", s)], [xk])
            D("gpsimd", f"x1o{s}", lambda e: e.dma_start(out=out[t * 128:(t + 1) * 128, :], in_=x1), r=[xk], w=[("out", t)])
            ss = rt[:, 0:1]; rstd = rt[:, 1:2]
            V(lambda e: e.memset(ss, 0.0), [], ["ss"])
            A(lambda e: e.activation(out=sq2, in_=x1, func=AF.Square, accum_out=ss), [xk, "ss"], ["sq2", "ss"])
            V(lambda e: e.tensor_scalar(out=rstd, in0=ss, scalar1=1.0 / 1024, scalar2=EPS, op0=ALU.mult, op1=ALU.add), ["ss"], ["rstd"])
            A(lambda e: e.activation(out=rstd, in_=rstd, func=AF.Sqrt), ["rstd"], ["rstd"])
            V(lambda e: e.reciprocal(out=rstd, in_=rstd), ["rstd"], ["rstd"])
            A(lambda e: e.activation(out=sq2, in_=x1, func=AF.Copy, scale=rstd), [xk, "rstd", "sq2"], ["sq2"])
            G(lambda e: e.tensor_tensor(out=u2f, in0=sq2, in1=g2_bc, op=ALU.mult), ["sq2", "g2"], ["u2f"])
            ub = u2b[s]; ubk = ("u2b", s)
            A(lambda e: e.activation(out=ub, in_=u2f, func=AF.Copy), ["u2f"], [ubk])
            for c in range(8):
                T(lambda e, c=c: e.transpose(out=PS[:, 2 + c // 4, (c % 4) * 128:(c % 4 + 1) * 128], in_=u2f[:, c * 128:(c + 1) * 128], identity=id_f), ["u2f", "cst"], [("ps", 2 + c // 4)])
            V(lambda e: e.tensor_copy(out=u2T, in_=PS[:, 2:4, :].rearrange("p b (c q) -> p (b c) q", c=4)), [("ps", 2), ("ps", 3)], ["u2T"])
            for kc in range(8):
                T(lambda e, kc=kc: e.matmul(PS[:, 4, 0:72], lhsT=u2T[:, kc, :], rhs=wgr[:, kc, :], start=(kc == 0), stop=(kc == 7)), ["u2T", "wgr"], [("ps", 4)])
            V(lambda e: e.tensor_copy(out=lg, in_=PS[:, 4, 0:72]), [("ps", 4)], ["lg"])
            lgg = lg[:, 0:8]; lge = lg[:, 8:72].rearrange("p (g x) -> p g x", g=8)
            m8 = rt[:, 8:16]; ohg = rt[:, 16:24]; nm = rt[:, 2:3]; se = rt[:, 3:4]; pg = rt[:, 4:5]
            eg = rt[:, 24:32]; t64 = rt[:, 64:128]; sel = rt[:, 32:40]; s8 = rt[:, 40:48]
            oh1 = rt[:, 48:56]; oh2 = rt[:, 56:64]; dd = rt[:, 5:6]; ed = rt[:, 6:7]
            V(lambda e: e.max(out=m8, in_=lgg), ["lg"], ["m8"])
            V(lambda e: e.tensor_scalar(out=ohg, in0=lgg, scalar1=m8[:, 0:1], scalar2=None, op0=ALU.is_equal), ["lg", "m8"], ["ohg"])
            V(lambda e: e.tensor_scalar(out=nm, in0=m8[:, 0:1], scalar1=-1.0, scalar2=None, op0=ALU.mult), ["m8"], ["nm"])
            V(lambda e: e.memset(se, 0.0), [], ["se"])
            A(lambda e: e.activation(out=eg, in_=lgg, func=AF.Exp, bias=nm, scale=1.0, accum_out=se), ["lg", "nm", "se"], ["eg", "se"])
            V(lambda e: e.reciprocal(out=pg, in_=se), ["se"], ["pg"])
            V(lambda e: e.tensor_tensor(out=t64.rearrange("p (g x) -> p g x", g=8), in0=lge, in1=ohg.unsqueeze(2).to_broadcast([128, 8, 8]), op=ALU.mult), ["lg", "ohg"], ["t64"])
            V(lambda e: e.tensor_reduce(out=sel, in_=t64.rearrange("p (g x) -> p x g", g=8), axis=AX.X, op=ALU.add), ["t64"], ["sel"])
            V(lambda e: e.max(out=s8, in_=sel), ["sel"], ["s8"])
            V(lambda e: e.tensor_scalar(out=oh1, in0=sel, scalar1=s8[:, 0:1], scalar2=None, op0=ALU.is_equal), ["sel", "s8"], ["oh1"])
            V(lambda e: e.tensor_scalar(out=oh2, in0=sel, scalar1=s8[:, 1:2], scalar2=None, op0=ALU.is_equal), ["sel", "s8"], ["oh2"])
            V(lambda e: e.tensor_tensor(out=dd, in0=s8[:, 1:2], in1=s8[:, 0:1], op=ALU.subtract), ["s8"], ["dd"])
            A(lambda e: e.activation(out=ed, in_=dd, func=AF.Exp), ["dd"], ["ed"])
            V(lambda e: e.tensor_scalar(out=dd, in0=ed, scalar1=1.0, scalar2=None, op0=ALU.add), ["ed", "dd"], ["dd"])
            V(lambda e: e.reciprocal(out=dd, in_=dd), ["dd"], ["dd"])
            V(lambda e: e.tensor_tensor(out=Wk[:, 0, t:t + 1], in0=dd, in1=pg, op=ALU.mult), ["dd", "pg"], [("Wk0", t)])
            V(lambda e: e.tensor_tensor(out=Wk[:, 1, t:t + 1], in0=Wk[:, 0, t:t + 1], in1=ed, op=ALU.mult), [("Wk0", t), "ed"], [("Wk1", t)])
            M1v = M1.rearrange("p (g x) -> p g x", g=8); M2v = M2.rearrange("p (g x) -> p g x", g=8)
            V(lambda e: e.tensor_tensor(out=M1v, in0=ohg.unsqueeze(2).to_broadcast([128, 8, 8]), in1=oh1.unsqueeze(1).to_broadcast([128, 8, 8]), op=ALU.mult), ["ohg", "oh1"], ["M1"])
            V(lambda e: e.tensor_tensor(out=M2v, in0=ohg.unsqueeze(2).to_broadcast([128, 8, 8]), in1=oh2.unsqueeze(1).to_broadcast([128, 8, 8]), op=ALU.mult), ["ohg", "oh2"], ["M2"])
            V(lambda e: e.tensor_tensor(out=Mb[:, t, :], in0=M1, in1=M2, op=ALU.add), ["M1", "M2"], [("Mb", t)])
            T(lambda e: e.matmul(PS[:, 5, 0:64], lhsT=tri_b, rhs=Mb[:, t, :], start=True, stop=(t == 0)), [("Mb", t), "cstb"], [("ps", 5)])
            for i in range(t):
                T(lambda e, i=i: e.matmul(PS[:, 5, 0:64], lhsT=ones_b, rhs=Mb[:, i, :], start=False, stop=(i == t - 1)), [("Mb", i), "cstb"], [("ps", 5)])
            V(lambda e: e.tensor_copy(out=Rk, in_=PS[:, 5, 0:64]), [("ps", 5)], ["Rk"])
            for k, Mk in enumerate((M1, M2)):
                mk_ = "M1" if k == 0 else "M2"
                rsel = rt[:, 128 + 4 * k:129 + 4 * k]; bsel = rt[:, 129 + 4 * k:130 + 4 * k]; ov = rt[:, 130 + 4 * k:131 + 4 * k]
                V(lambda e, Mk=Mk: e.tensor_tensor(out=tmpM, in0=Rk, in1=Mk, op=ALU.mult), ["Rk", mk_], ["tmpM"])
                V(lambda e, rsel=rsel: e.tensor_reduce(out=rsel, in_=tmpM, axis=AX.X, op=ALU.add), ["tmpM"], [("rsel", k)])
                V(lambda e, Mk=Mk: e.tensor_tensor(out=tmpM, in0=ebase, in1=Mk, op=ALU.mult), ["cst", mk_, ("rsel", k)], ["tmpM"])
                V(lambda e, bsel=bsel: e.tensor_reduce(out=bsel, in_=tmpM, axis=AX.X, op=ALU.add), ["tmpM"], [("bsel", k)])
                V(lambda e, ov=ov, rsel=rsel: e.tensor_scalar(out=ov, in0=rsel, scalar1=float(CAP) - 0.5, scalar2=100000.0, op0=ALU.is_ge, op1=ALU.mult), [("rsel", k)], [("ov", k)])
                V(lambda e, bsel=bsel, rsel=rsel: e.tensor_tensor(out=bsel, in0=bsel, in1=rsel, op=ALU.add), [("bsel", k), ("rsel", k)], [("bsel", k)])
                V(lambda e, bsel=bsel, ov=ov: e.tensor_tensor(out=bsel, in0=bsel, in1=ov, op=ALU.add), [("bsel", k), ("ov", k)], [("bsel", k)])
                V(lambda e, bsel=bsel, k=k: e.tensor_copy(out=idx[:, k, t:t + 1], in_=bsel), [("bsel", k)], [("idx", k, t)])
                D("gpsimd", "scat", lambda e, k=k: e.indirect_dma_start(out=XS, out_offset=bass.IndirectOffsetOnAxis(ap=idx[:, k, t:t + 1], axis=0), in_=ub, in_offset=None, bounds_check=bnd['r'], oob_is_err=False), r=[("idx", k, t), ubk, "XS"], w=["XSs"])
        for t_ in range(16):
            p5_tile(t_)
        tap("idx", idx, [("idx", k, t) for k in range(2) for t in range(16)])
        tap("Wk", Wk, [("Wk0", t) for t in range(16)] + [("Wk1", t) for t in range(16)])
        S.barrier()
        if stop <= 5:
            S.emit(); return nc

        o = [0]
        LIM = P0
        wst = [salloc(16384, F32, lim=LIM) for _ in range(4)]
        wb1 = [salloc(8192, BF16, lim=LIM).rearrange("p (c n) -> p c n", c=8) for _ in range(2)]
        wb3 = [salloc(8192, BF16, lim=LIM).rearrange("p (c n) -> p c n", c=8) for _ in range(2)]
        wb2 = [salloc(8192, BF16, lim=LIM).rearrange("p (c n) -> p c n", c=4) for _ in range(2)]
        xs_sb = [salloc(2048, BF16, lim=LIM) for _ in range(2)]
        xsT = [salloc(2048, BF16, lim=LIM).rearrange("p (c q) -> p c q", c=8) for _ in range(2)]
        sh = salloc(2048, F32, lim=LIM)
        act = salloc(1024, BF16, lim=LIM)
        aT = salloc(1024, BF16, lim=LIM).rearrange("p (c q) -> p c q", c=4)
        ysb = [salloc(4096, F32, lim=LIM) for _ in range(2)]
        nst = [0]

        def load_w(e_):
            keys = []
            for (wsrc, wdst, ceng, name) in ((w1, wb1, "vector", "wb1"), (w3, wb3, "gpsimd", "wb3"), (w2, wb2, "scalar", "wb2")):
                s = nst[0] % 4; nst[0] += 1
                d = wdst[e_ % 2]
                if name == "wb2":
                    src = wsrc[e_].rearrange("(c p) n -> p c n", p=128)
                    stv = wst[s].rearrange("p (c n) -> p c n", c=4)
                else:
                    src = wsrc[e_].rearrange("(c p) n -> p c n", p=128)
                    stv = wst[s].rearrange("p (c n) -> p c n", c=8)
                D("sync", f"wst{s}", lambda e, stv=stv, src=src: e.dma_start(out=stv, in_=src), w=[f"wst{s}"])
                if ceng == "scalar":
                    S.add(ceng, lambda e, d=d, stv=stv: e.activation(out=d, in_=stv, func=AF.Copy), [f"wst{s}"], [(name, e_ % 2)])
                else:
                    S.add(ceng, lambda e, d=d, stv=stv: e.tensor_copy(out=d, in_=stv), [f"wst{s}"], [(name, e_ % 2)])

        def load_xs(e_):
            s = e_ % 2
            D("sync", f"xs{s}", lambda e: e.dma_start(out=xs_sb[s], in_=XS[e_ * CAP:(e_ + 1) * CAP, :]), r=["XSs", "XS"], w=[("xs", s)])

        load_w(0); load_xs(0)

        def p6_expert(e_):
            if e_ + 1 < NEXP:
                load_w(e_ + 1); load_xs(e_ + 1)
            s = e_ % 2
            pt = psb(0).rearrange("p (c q) -> p c q", c=8)
            for c in range(8):
                T(lambda e, c=c: e.transpose(out=pt[:, c, :], in_=xs_sb[s][:, c * 128:(c + 1) * 128], identity=id_b), [("xs", s)], [("ps", 0)])
            V(lambda e: e.tensor_copy(out=xsT[s], in_=pt), [("ps", 0)], [("xsT", s)])
            for kc in range(8):
                T(lambda e, kc=kc: e.matmul(PS[:, 1, :], lhsT=xsT[s][:, kc, :], rhs=wb1[s][:, kc, :], start=(kc == 0), stop=(kc == 7)), [("xsT", s), ("wb1", s)], [("ps", 1)])
            for kc in range(8):
                T(lambda e, kc=kc: e.matmul(PS[:, 2, :], lhsT=xsT[s][:, kc, :], rhs=wb3[s][:, kc, :], start=(kc == 0), stop=(kc == 7)), [("xsT", s), ("wb3", s)], [("ps", 2)])
            A(lambda e: e.activation(out=sh, in_=PS[:, 1, :], func=AF.Silu), [("ps", 1)], ["sh"])
            V(lambda e: e.tensor_tensor(out=act, in0=PS[:, 2, :], in1=sh, op=ALU.mult), [("ps", 2), "sh"], ["act"])
            pa = psb(3).rearrange("p (c q) -> p c q", c=8)
            for c in range(4):
                T(lambda e, c=c: e.transpose(out=pa[:, c, :], in_=act[:, c * 128:(c + 1) * 128], identity=id_b), ["act"], [("ps", 3)])
            V(lambda e: e.tensor_copy(out=aT, in_=pa[:, 0:4, :]), [("ps", 3)], ["aT"])
            for half in range(2):
                for c in range(4):
                    T(lambda e, c=c, half=half: e.matmul(PS[:, 4 + half, :], lhsT=aT[:, c, :], rhs=wb2[s][:, c, half * 512:(half + 1) * 512], start=(c == 0), stop=(c == 3)), ["aT", ("wb2", s)], [("ps", 4 + half)])
            yk = ("ysb", s)
            A(lambda e: e.activation(out=ysb[s][:, 0:512], in_=PS[:, 4, :], func=AF.Copy), [("ps", 4)], [(yk, 0)])
            V(lambda e: e.tensor_copy(out=ysb[s][:, 512:1024], in_=PS[:, 5, :]), [("ps", 5)], [(yk, 1)])
            D("gpsimd", f"ys{s}", lambda e: e.dma_start(out=YS[e_ * CAP:(e_ + 1) * CAP, :], in_=ysb[s]), r=[(yk, 0), (yk, 1)], w=["YS"])

        for e2 in range(NEXP):
            p6_expert(e2)
        S.barrier()
        if stop <= 6:
            S.emit(); return nc

        o = [0]
        y0 = [salloc(4096, F32, lim=LIM) for _ in range(2)]
        y1 = [salloc(4096, F32, lim=LIM) for _ in range(2)]
        x1b = [salloc(4096, F32, lim=LIM) for _ in range(2)]
        def p7_tile(t):
            s = t % 2
            D("sync", f"x7{s}", lambda e: e.dma_start(out=x1b[s], in_=out[t * 128:(t + 1) * 128, :]), w=[("x7", s)])
            for k, yb in enumerate((y0, y1)):
                G(lambda e, yb=yb: e.memset(yb[s], 0.0), [], [("y", k, s)])
                D("gpsimd", f"gat{k}{s}", lambda e, k=k, yb=yb: e.indirect_dma_start(out=yb[s], out_offset=None, in_=YS, in_offset=bass.IndirectOffsetOnAxis(ap=idx[:, k, t:t + 1], axis=0), bounds_check=bnd['r'], oob_is_err=False), r=[("y", k, s)], w=[("y", k, s)])
            V(lambda e: e.scalar_tensor_tensor(out=x1b[s], in0=y0[s], scalar=Wk[:, 0, t:t + 1], in1=x1b[s], op0=ALU.mult, op1=ALU.add), [("y", 0, s), ("x7", s)], [("x7", s)])
            V(lambda e: e.scalar_tensor_tensor(out=x1b[s], in0=y1[s], scalar=Wk[:, 1, t:t + 1], in1=x1b[s], op0=ALU.mult, op1=ALU.add), [("y", 1, s), ("x7", s)], [("x7", s)])
            D("sync", f"fo{s}", lambda e: e.dma_start(out=out[t * 128:(t + 1) * 128, :], in_=x1b[s]), r=[("x7", s)])

        for t_ in range(16):
            p7_tile(t_)
        S.emit()
    return nc


def _rope_tables():
    S = 4096; GW = 64
    rows = S // GW
    row = np.repeat(np.arange(rows, dtype=np.float32), GW)
    col = np.tile(np.arange(GW, dtype=np.float32), rows)
    inv = (np.float32(10000.0) ** (-np.arange(0, 64, 2, dtype=np.float32) / np.float32(64))).astype(np.float32)
    ang = np.concatenate([row[:, None] * inv, col[:, None] * inv], axis=-1).astype(np.float32)
    return np.concatenate([np.cos(ang), np.sin(ang)], axis=-1).astype(np.float32)


def _consts():
    c = np.zeros((128, 512), np.float32)
    c[:, 0:128] = np.eye(128, dtype=np.float32)
    c[:, 128:256] = np.triu(np.ones((128, 128), np.float32), k=1)
    c[:, 256:384] = 1.0
    c[:, 384:448] = (np.arange(64, dtype=np.float32) * CAP)[None, :]
    return c


def core_inputs(inputs, c):
    b, hq = c // 2, c % 2
    x = np.asarray(inputs["x"], np.float32)
    t0 = hq * 2048
    xa = np.zeros((4224, 1024), np.float32)
    xa[0:2048] = x[b, t0:t0 + 2048]
    xa[2048:4096] = x[b, (1 - hq) * 2048:(1 - hq) * 2048 + 2048]
    if t0 > 0:
        xa[4096] = x[b, t0 - 1]
    if t0 + 2048 < 4096:
        xa[4097] = x[b, t0 + 2048]
    tab = _rope_tables()
    cs = np.concatenate([tab[t0:t0 + 2048], tab[(1 - hq) * 2048:(1 - hq) * 2048 + 2048]], axis=0)
    f = lambda k: np.ascontiguousarray(np.asarray(inputs[k], np.float32)[0])
    return {
        "xa": xa, "cs": np.ascontiguousarray(cs),
        "norm1_g": f("norm1_g"), "norm2_g": f("norm2_g"), "w_in": f("w_in"), "conv_w": f("conv_w"),
        "q_norm_g": f("q_norm_g"), "k_norm_g": f("k_norm_g"), "w_conv_out": f("w_conv_out"),
        "w_attn_out": f("w_attn_out"), "w_o": f("w_o"),
        "w_gr": np.ascontiguousarray(np.concatenate([f("w_group"), f("w_router")], axis=1)),
        "w1": f("w1"), "w3": f("w3"), "w2": f("w2"), "consts": _consts(),
    }


def kernel(**inputs):
    nc = build()
    shared = core_inputs(inputs, 0)
    in_maps = []
    for c in range(8):
        m = dict(shared)
        if c:
            ci = core_inputs_light(inputs, c)
            m.update(ci)
        in_maps.append(m)
    res = run_bass_kernel_spmd(nc, in_maps, core_ids=list(range(8)))
    outp = np.zeros((4, 4096, 1024), np.float32)
    for c in range(8):
        b, hq = c // 2, c % 2
        outp[b, hq * 2048:(hq + 1) * 2048] = res.results[c]["out"]
    return outp


def core_inputs_light(inputs, c):
    b, hq = c // 2, c % 2
    x = np.asarray(inputs["x"], np.float32)
    t0 = hq * 2048
    xa = np.zeros((4224, 1024), np.float32)
    xa[0:2048] = x[b, t0:t0 + 2048]
    xa[2048:4096] = x[b, (1 - hq) * 2048:(1 - hq) * 2048 + 2048]
    if t0 > 0:
        xa[4096] = x[b, t0 - 1]
    if t0 + 2048 < 4096:
        xa[4097] = x[b, t0 + 2048]
    tab = _rope_tables()
    cs = np.concatenate([tab[t0:t0 + 2048], tab[(1 - hq) * 2048:(1 - hq) * 2048 + 2048]], axis=0)
    return {"xa": xa, "cs": np.ascontiguousarray(cs)}
```

```python
import contextlib
import numpy as np
import concourse.bass as bass
import concourse.mybir as mybir
from concourse.bass_utils import run_bass_kernel_spmd

F32 = mybir.dt.float32
BF16 = mybir.dt.bfloat16
I32 = mybir.dt.int32
U8 = mybir.dt.uint8
AF = mybir.ActivationFunctionType
ALU = mybir.AluOpType
AX = mybir.AxisListType

NT_OWN = 16
NT_ALL = 32
CAP = 128
NEXP = 64
EPS = 1e-6


class Op:
    __slots__ = ("eng", "fn", "idx", "waits", "inc", "count", "is_dma", "semkey")


class Sched:
    ENGS = ("sync", "scalar", "vector", "gpsimd", "tensor")

    def __init__(self, nc):
        self.nc = nc
        self.ops = {e: [] for e in self.ENGS}
        self.tiles = {}
        self.dma_count = {}

    def add(self, eng, fn, reads=(), writes=(), semkey=None):
        op = Op()
        op.eng = eng; op.fn = fn; op.idx = len(self.ops[eng]); op.inc = False
        op.is_dma = semkey is not None; op.semkey = semkey; op.count = None
        deps = []
        for k in reads:
            st = self.tiles.get(k)
            if st and st[0] is not None:
                deps.append(st[0])
        for k in writes:
            st = self.tiles.get(k)
            if st:
                if st[0] is not None:
                    deps.append(st[0])
                deps.extend(st[1])
        waits = {}
        for d in deps:
            if d.is_dma:
                key = ("dma", d.semkey); val = self.dma_count[d.semkey]
                waits[key] = max(waits.get(key, 0), val)
            else:
                if d.eng == "tensor" and eng == "tensor" and not op.is_dma:
                    continue
                d.inc = True
                key = ("eng", d.eng)
                cur = waits.get(key)
                if cur is None or d.idx > cur.idx:
                    waits[key] = d
        op.waits = waits
        if op.is_dma:
            self.dma_count[semkey] = self.dma_count.get(semkey, 0) + 16
        for k in reads:
            st = self.tiles.setdefault(k, [None, []])
            st[1].append(op)
        for k in writes:
            self.tiles[k] = [op, []]
        self.ops[eng].append(op)
        return op

    def barrier(self):
        last = {}
        for e in self.ENGS:
            for op in reversed(self.ops[e]):
                if not op.is_dma and op.fn is not None:
                    last[e] = op
                    break
        dma_snapshot = dict(self.dma_count)
        for e in self.ENGS:
            op = Op()
            op.eng = e; op.fn = None; op.idx = len(self.ops[e]); op.inc = False
            op.is_dma = False; op.semkey = None; op.count = None
            waits = {}
            for f, lo in last.items():
                if f == "tensor" and e == "tensor":
                    continue
                lo.inc = True
                waits[("eng", f)] = lo
            for k, v in dma_snapshot.items():
                waits[("dma", k)] = v
            op.waits = waits
            self.ops[e].append(op)
        self.tiles.clear()

    def emit(self, final_eng="gpsimd"):
        nc = self.nc
        for e in self.ENGS:
            c = 0
            for op in self.ops[e]:
                if op.inc and not op.is_dma:
                    c += 1
                    op.count = c
        semkeys = sorted(self.dma_count.keys())
        with contextlib.ExitStack() as st:
            esem = {e: st.enter_context(nc.semaphore("es_" + e)) for e in self.ENGS}
            dsem = {k: st.enter_context(nc.semaphore("ds_" + str(k))) for k in semkeys}
            block = st.enter_context(nc.Block())

            def run(e, eng):
                known = {}
                for op in self.ops[e]:
                    for key, val in op.waits.items():
                        if key[0] == "dma":
                            sem = dsem[key[1]]; v = val
                        else:
                            sem = esem[key[1]]; v = val.count
                        if known.get(key, 0) >= v:
                            continue
                        known[key] = v
                        eng.wait_ge(sem, v)
                    if op.fn is None:
                        continue
                    ins = op.fn(eng)
                    if op.is_dma:
                        ins.then_inc(dsem[op.semkey], 16)
                    elif op.inc:
                        ins.then_inc(esem[e], 1)
                if e == final_eng:
                    for k in semkeys:
                        if known.get(("dma", k), 0) < self.dma_count[k]:
                            eng.wait_ge(dsem[k], self.dma_count[k])

            @block.sync
            def _(eng): run("sync", eng)

            @block.scalar
            def _(eng): run("scalar", eng)

            @block.vector
            def _(eng): run("vector", eng)

            @block.gpsimd
            def _(eng): run("gpsimd", eng)

            @block.tensor
            def _(eng): run("tensor", eng)


def build(stop=99, taps=()):
    nc = bass.Bass("TRN2", target_bir_lowering=False)

    def din(name, shape, dt=F32):
        return nc.dram_tensor(name, list(shape), dt, kind="ExternalInput").ap()

    xa = din("xa", [4224, 1024])
    cs = din("cs", [4096, 128])
    norm1_g = din("norm1_g", [1024])
    norm2_g = din("norm2_g", [1024])
    w_in = din("w_in", [1024, 6656])
    conv_w = din("conv_w", [3, 1024])
    q_norm_g = din("q_norm_g", [128])
    k_norm_g = din("k_norm_g", [128])
    w_conv_out = din("w_conv_out", [1024, 1024])
    w_attn_out = din("w_attn_out", [1024, 1024])
    w_o = din("w_o", [1024, 1024])
    w_gr = din("w_gr", [1024, 72])
    w1 = din("w1", [NEXP, 1024, 512])
    w3 = din("w3", [NEXP, 1024, 512])
    w2 = din("w2", [NEXP, 512, 1024])
    consts = din("consts", [128, 512])
    out = nc.dram_tensor("out", [2048, 1024], F32, kind="ExternalOutput").ap()
    XS = nc.dram_tensor("XS", [NEXP * CAP, 1024], BF16).ap()
    YS = nc.dram_tensor("YS", [NEXP * CAP, 1024], F32).ap()
    tap_out = {}
    for (name, shape, dt) in taps:
        tap_out[name] = nc.dram_tensor("tap_" + name, list(shape), dt, kind="ExternalOutput").ap()

    with contextlib.ExitStack() as st:
        TOTAL = 212000
        arena = st.enter_context(nc.sbuf_tensor("arena", [128, TOTAL], U8))
        PS = st.enter_context(nc.psum_tensor("ps", [128, 8, 512], F32))
        PSb = PS[:].bitcast(BF16) if hasattr(PS[:], "bitcast") else None

        def region(off, nbytes, dt):
            assert off % 4 == 0 and off + nbytes <= TOTAL, (off, nbytes)
            return arena[:, off:off + nbytes].bitcast(dt)

        S = Sched(nc)

        def V(fn, r=(), w=()): return S.add("vector", fn, r, w)
        def A(fn, r=(), w=()): return S.add("scalar", fn, r, w)
        def G(fn, r=(), w=()): return S.add("gpsimd", fn, r, w)
        def T(fn, r=(), w=()): return S.add("tensor", fn, r, w)
        def D(q, key, fn, r=(), w=()): return S.add(q, fn, r, w, semkey=key)

        def psb(bank):
            return PSb[:, bank, :]

        P0 = 198000
        p = [P0]

        def palloc(nbytes, dt):
            r = region(p[0], nbytes, dt); p[0] += (nbytes + 3) // 4 * 4
            return r
        cst = palloc(512 * 4, F32)
        id_f = cst[:, 0:128]; ebase = cst[:, 384:448]
        cstb = palloc(384 * 2, BF16)
        id_b = cstb[:, 0:128]; tri_b = cstb[:, 128:256]; ones_b = cstb[:, 256:384]
        g1T = palloc(8 * 4, F32)
        cw = palloc(24 * 4, F32).rearrange("p (k c) -> p k c", k=3)
        gq_bc = palloc(128 * 4, F32)
        gk_bc = palloc(128 * 4, F32)
        negc = palloc(4 * 4, F32)
        wgr = palloc(8 * 72 * 4, F32).rearrange("p (c n) -> p c n", c=8)
        Wk = palloc(2 * 16 * 4, F32).rearrange("p (k t) -> p k t", k=2)
        idx = palloc(2 * 16 * 4, I32).rearrange("p (k t) -> p k t", k=2)
        Mb = palloc(16 * 64 * 2, BF16).rearrange("p (t e) -> p t e", t=16)
        g2_bc = palloc(1024 * 4, F32)
        gq_p = palloc(128 * 4, F32)
        gk_p = palloc(128 * 4, F32)
        assert p[0] <= TOTAL, p[0]

        OFF_A, OFF_B, OFF_QT, OFF_KT, OFF_VX, OFF_D, OFF_W = 0, 34816, 67584, 100352, 116736, 133376, 166144
        uT = region(OFF_A, 34816, BF16).rearrange("p (c t) -> p c t", c=8)
        Bbuf = region(OFF_B, 32768, BF16).rearrange("p (c t) -> p c t", c=8)
        QT = region(OFF_QT, 32768, BF16).rearrange("p (t h q) -> p t h q", t=16, h=8)
        KT = region(OFF_KT, 16384, BF16).rearrange("p (g t) -> p g t", g=2)
        VX = region(OFF_VX, 16640, BF16).rearrange("p (t g d) -> p t g d", t=32, g=2)
        OT = region(OFF_D, 32768, BF16).rearrange("p (c t) -> p c t", c=8)
        mT = region(OFF_QT, 32768, BF16).rearrange("p (c t) -> p c t", c=8)
        Wreg = OFF_W

        D("sync", "const", lambda e: e.dma_start(out=cst, in_=consts), w=["cst"])
        D("sync", "const", lambda e: e.dma_start(out=g1T, in_=norm1_g.rearrange("(c p) -> p c", p=128), allow_slow_non_contiguous=True), w=["g1T"])
        D("sync", "const", lambda e: e.dma_start(out=cw, in_=conv_w.rearrange("k (c p) -> p k c", p=128), allow_slow_non_contiguous=True), w=["cw"])
        D("sync", "const", lambda e: e.dma_start(out=gq_bc, in_=q_norm_g.partition_broadcast(128)), w=["gq"])
        D("sync", "const", lambda e: e.dma_start(out=gk_bc, in_=k_norm_g.partition_broadcast(128)), w=["gk"])
        D("sync", "const", lambda e: e.dma_start(out=g2_bc, in_=norm2_g.partition_broadcast(128)), w=["g2"])
        D("sync", "const", lambda e: e.dma_start(out=wgr, in_=w_gr.rearrange("(c p) n -> p c n", p=128)), w=["wgr"])
        V(lambda e: e.tensor_copy(out=cstb, in_=cst[:, 0:384]), ["cst"], ["cstb"])
        V(lambda e: e.tensor_reduce(out=negc[:, 0:1], in_=gq_bc, axis=AX.X, op=ALU.max, apply_absolute_value=True), ["gq"], ["negc0"])
        V(lambda e: e.tensor_reduce(out=negc[:, 1:2], in_=gk_bc, axis=AX.X, op=ALU.max, apply_absolute_value=True), ["gk"], ["negc1"])
        V(lambda e: e.tensor_tensor(out=negc[:, 2:3], in0=negc[:, 0:1], in1=negc[:, 1:2], op=ALU.mult), ["negc0", "negc1"], ["negc2"])
        V(lambda e: e.tensor_scalar(out=negc[:, 3:4], in0=negc[:, 2:3], scalar1=-(128.0 ** 0.5), scalar2=None, op0=ALU.mult), ["negc2"], ["negc"])
        bnd = {}

        def mk_bnd(e):
            bnd['r'] = e.alloc_register("bnd")
            return e.reg_mov(bnd['r'], NEXP * CAP - 1)
        G(mk_bnd)
        G(lambda e: e.memset(VX[:, :, :, 128:130], 1.0), [], ["vx1"])

        def tap(name, src_ap, keys):
            if name in tap_out:
                D("gpsimd", "tap_" + name, lambda e: e.dma_start(out=tap_out[name], in_=src_ap), r=keys)

        wqkv = region(Wreg, 24576, BF16).rearrange("p (c n) -> p c n", c=8)
        o = [OFF_B]

        def salloc(nbytes, dt, lim=OFF_B + 32768):
            r = region(o[0], nbytes, dt); o[0] += (nbytes + 3) // 4 * 4
            assert o[0] <= lim, (o[0], lim)
            return r
        xt = [salloc(4096, F32) for _ in range(3)]
        xn = [salloc(2048, BF16) for _ in range(2)]
        junk = salloc(4096, F32)
        uTo = [salloc(2048, BF16).rearrange("p (c t) -> p c t", c=8) for _ in range(2)]
        qkc = salloc(5120, F32)
        qkc_o = salloc(1024, F32)
        o = [OFF_D]

        def salloc2(nbytes, dt):
            return salloc(nbytes, dt, lim=OFF_D + 32768)
        qn = [salloc2(5120, F32) for _ in range(2)]
        qn_o = [salloc2(1024, F32) for _ in range(2)]
        tmp4 = [salloc2(2560, F32) for _ in range(4)]
        qr = [salloc2(2560, BF16) for _ in range(2)]
        cst_t = [salloc2(512, F32) for _ in range(3)]
        st1 = salloc2(4 * 32 * 4, F32).rearrange("p (s k) -> p s k", s=4)
        epsb = salloc2(4, F32)
        stg = [region(OFF_D + 12288 + i * 6144, 6144, F32) for i in range(2)]

        V(lambda e: e.memset(epsb, EPS), [], ["eps"])
        wq_keys = [f"wqkv{kc}" for kc in range(8)]
        V(lambda e: e.tensor_scalar(out=gq_p.rearrange("p (two i) -> p two i", two=2), in0=gq_bc.rearrange("p (i two) -> p two i", two=2), scalar1=128.0 ** -0.5, scalar2=None, op0=ALU.mult), ["gq"], ["gqp"])
        V(lambda e: e.tensor_copy(out=gk_p.rearrange("p (two i) -> p two i", two=2), in_=gk_bc.rearrange("p (i two) -> p two i", two=2)), ["gk"], ["gkp"])

        tile_order = []
        for i in range(16):
            tile_order += [i, 16 + i]
        tile_order.append(32)
        NTL = len(tile_order)

        def load_x(n):
            if n >= NTL:
                return
            t = tile_order[n]; s = n % 3
            D("sync", f"xt{s}", lambda e: e.dma_start(out=xt[s], in_=xa[t * 128:(t + 1) * 128, :]), w=[f"xt{s}"])

        def udst_of(n):
            t = tile_order[n]
            if t < 16:
                return uT[:, :, t * 128:(t + 1) * 128], ("uT", t)
            if t == 32:
                return uT[:, :, 2048:2176], ("uT", 16)
            return uTo[(n // 2) % 2], ("uTo", (n // 2) % 2)

        def tinfo(n):
            t = tile_order[n]
            own = t < 16
            nq = 10 if own else 2
            return t, own, nq

        def st_S0(n):
            s = n % 3; s2 = n % 2
            xts = xt[s]; xns = xn[s2]
            ss = st1[:, n % 4, 0:1]; rstd = st1[:, n % 4, 1:2]
            sk = ("ss", n % 4)
            A(lambda e: e.activation(out=junk[:, 0:1024], in_=xts, func=AF.Square, accum_out=ss), [f"xt{s}"], [sk])
            A(lambda e: e.activation(out=rstd, in_=ss, func=AF.Ln, scale=1.0 / 1024, bias=epsb), [sk, "eps"], [sk])
            A(lambda e: e.activation(out=rstd, in_=rstd, func=AF.Exp, scale=-0.5), [sk], [sk])
            A(lambda e: e.activation(out=xns, in_=xts, func=AF.Copy, scale=rstd), [f"xt{s}", sk], [f"xn{s2}"])
            load_x(n + 2)

        def st_S1(n):
            s2 = n % 2
            xns = xn[s2]
            pt = psb(s2).rearrange("p (c q) -> p c q", c=8)
            for c in range(8):
                T(lambda e, c=c: e.transpose(out=pt[:, c, :], in_=xns[:, c * 128:(c + 1) * 128], identity=id_b), [f"xn{s2}", "cstb"], [("ps", s2)])

        def st_S2(n):
            s2 = n % 2
            pt = psb(s2).rearrange("p (c q) -> p c q", c=8)
            udst, ukey = udst_of(n)
            V(lambda e: e.tensor_tensor(out=udst, in0=pt, in1=g1T.unsqueeze(2).to_broadcast([128, 8, 128]), op=ALU.mult), [("ps", s2), "g1T"], [ukey])

        def st_S3(n):
            t, own, nq = tinfo(n)
            if t == 32:
                return
            udst, ukey = udst_of(n)
            kvb = 4 if own else 5
            for kc in range(8):
                lhs = udst[:, kc, :]
                if own:
                    T(lambda e, kc=kc, lhs=lhs: e.matmul(PS[:, 2, :], lhsT=lhs, rhs=wqkv[:, kc, 0:512], start=(kc == 0), stop=(kc == 7)), [ukey] + wq_keys, [("ps", 2)])
                    T(lambda e, kc=kc, lhs=lhs: e.matmul(PS[:, 3, :], lhsT=lhs, rhs=wqkv[:, kc, 512:1024], start=(kc == 0), stop=(kc == 7)), [ukey], [("ps", 3)])
                T(lambda e, kc=kc, lhs=lhs: e.matmul(PS[:, kvb, :], lhsT=lhs, rhs=wqkv[:, kc, 1024:1536], start=(kc == 0), stop=(kc == 7)), [ukey] + wq_keys, [("ps", kvb)])

        def st_S4(n):
            t, own, nq = tinfo(n)
            if t == 32:
                return
            s3 = n % 3
            D("sync", f"cs{s3}", lambda e: e.dma_start(out=cst_t[s3], in_=cs[t * 128:(t + 1) * 128, :]), w=[f"cs{s3}"])
            if own:
                qkps = PS[:, 2:5, :].rearrange("p b n -> p (b n)")[:, 0:1280]
                pkeys = [("ps", 2), ("ps", 3), ("ps", 4)]; kvb = 4
                cdst = qkc; ckey = "qkc"
            else:
                qkps = PS[:, 5, 0:256]
                pkeys = [("ps", 5)]; kvb = 5
                cdst = qkc_o[:, 0:256]; ckey = "qkco"
            ssq = st1[:, n % 4, 8:8 + nq]; rs = st1[:, n % 4, 20:20 + nq]
            rk = ("rs", n % 4)
            for h in range(nq):
                A(lambda e, h=h: e.activation(out=junk[:, 0:128], in_=qkps[:, h * 128:(h + 1) * 128], func=AF.Square, accum_out=ssq[:, h:h + 1]), pkeys, [rk])
            A(lambda e: e.activation(out=rs, in_=ssq, func=AF.Ln, scale=1.0 / 128, bias=epsb), [rk, "eps"], [rk])
            A(lambda e: e.activation(out=rs, in_=rs, func=AF.Exp, scale=-0.5), [rk], [rk])
            A(lambda e: e.activation(out=cdst, in_=qkps, func=AF.Copy), pkeys, [ckey])
            A(lambda e: e.activation(out=VX[:, t, :, 0:128], in_=PS[:, kvb, 256:512].rearrange("p (g d) -> p g d", g=2), func=AF.Copy), [("ps", kvb)], [("vx", t)])

        def st_S5(n):
            t, own, nq = tinfo(n)
            if t == 32:
                return
            rs = st1[:, n % 4, 20:20 + nq]; rk = ("rs", n % 4)
            if own:
                src = qkc; ckey = "qkc"; qs = qn[(n // 2) % 2]
            else:
                src = qkc_o[:, 0:256]; ckey = "qkco"; qs = qn_o[(n // 2) % 2]
            qkey = ("qn", (n // 2) % 2, own)
            src3 = src.rearrange("p (h i two) -> p h two i", i=64, two=2)
            for h in range(nq):
                gp = gq_p if (own and h < 8) else gk_p
                dst = qs[:, h * 128:(h + 1) * 128].rearrange("p (two i) -> p two i", two=2)
                V(lambda e, h=h, gp=gp, dst=dst: e.scalar_tensor_tensor(out=dst, in0=src3[:, h, :, :], scalar=rs[:, h:h + 1], in1=gp.rearrange("p (two i) -> p two i", two=2), op0=ALU.mult, op1=ALU.mult), [ckey, rk, "gqp", "gkp"], [qkey])

        def rope_views(n):
            t, own, nq = tinfo(n)
            qs = qn[(n // 2) % 2] if own else qn_o[(n // 2) % 2]
            q3 = qs[:, 0:nq * 128].rearrange("p (h d) -> p h d", d=128)
            tt = [tmp4[i][:, 0:nq * 64].rearrange("p (h i) -> p h i", i=64) for i in range(4)]
            return t, own, nq, q3, tt

        def st_S6(n):
            t, own, nq, q3, tt = rope_views(n)
            if t == 32:
                return
            s3 = n % 3
            qkey = ("qn", (n // 2) % 2, own)
            x0 = q3[:, :, 0:64]; x1 = q3[:, :, 64:128]
            cosb = cst_t[s3][:, 0:64].unsqueeze(1).to_broadcast([128, nq, 64])
            sinb = cst_t[s3][:, 64:128].unsqueeze(1).to_broadcast([128, nq, 64])
            ck = f"cs{s3}"
            V(lambda e: e.tensor_tensor(out=tt[0], in0=x0, in1=cosb, op=ALU.mult), [qkey, ck], ["t0"])
            G(lambda e: e.tensor_tensor(out=tt[1], in0=x1, in1=sinb, op=ALU.mult), [qkey, ck], ["t1"])
            G(lambda e: e.tensor_tensor(out=tt[2], in0=x0, in1=sinb, op=ALU.mult), [qkey, ck], ["t2"])
            V(lambda e: e.tensor_tensor(out=tt[3], in0=x1, in1=cosb, op=ALU.mult), [qkey, ck], ["t3"])

        def st_S7(n):
            t, own, nq, q3, tt = rope_views(n)
            if t == 32:
                return
            h0 = 10 - nq
            qrs = qr[n % 2]; qk_ = ("qr", n % 2)
            r3 = qrs[:, h0 * 128:1280].rearrange("p (h d) -> p h d", d=128)
            V(lambda e: e.tensor_tensor(out=r3[:, :, 0:64], in0=tt[0], in1=tt[1], op=ALU.subtract), ["t0", "t1"], [(qk_, 0)])
            G(lambda e: e.tensor_tensor(out=r3[:, :, 64:128], in0=tt[2], in1=tt[3], op=ALU.add), ["t2", "t3"], [(qk_, 1)])

        def st_S8(n):
            t, own, nq = tinfo(n)
            if t == 32:
                return
            qrs = qr[n % 2]; qk_ = ("qr", n % 2)
            pq = psb(6).rearrange("p (h q) -> p h q", h=8)
            pk = psb(7).rearrange("p (h q) -> p h q", h=8)
            if own:
                for h in range(8):
                    T(lambda e, h=h: e.transpose(out=pq[:, h, :], in_=qrs[:, h * 128:(h + 1) * 128], identity=id_b), [(qk_, 0), (qk_, 1)], [("ps", 6)])
            for g in range(2):
                T(lambda e, g=g: e.transpose(out=pk[:, g, :], in_=qrs[:, (8 + g) * 128:(9 + g) * 128], identity=id_b), [(qk_, 0), (qk_, 1)], [("ps", 7)])

        def st_S9(n):
            t, own, nq = tinfo(n)
            if t == 32:
                return
            pq = psb(6).rearrange("p (h q) -> p h q", h=8)
            pk = psb(7).rearrange("p (h q) -> p h q", h=8)
            if own:
                A(lambda e: e.activation(out=QT[:, t, :, :], in_=pq, func=AF.Copy), [("ps", 6)], [("QT", t)])
            V(lambda e: e.tensor_copy(out=KT[:, :, t * 128:(t + 1) * 128], in_=pk[:, 0:2, :]), [("ps", 7)], [("KT", t)])

        stages = [st_S0, st_S1, st_S2, st_S3, st_S4, st_S5, st_S6, st_S7, st_S8, st_S9]
        load_x(0); load_x(1)
        for kc in range(8):
            s = kc % 2
            D("sync", f"stg{s}", lambda e, kc=kc, s=s: e.dma_start(out=stg[s], in_=w_in[kc * 128:(kc + 1) * 128, 3072:4608]), w=[f"stg{s}"])
            if kc % 2 == 0:
                G(lambda e, kc=kc, s=s: e.tensor_copy(out=wqkv[:, kc, :], in_=stg[s]), [f"stg{s}"], [f"wqkv{kc}"])
            else:
                V(lambda e, kc=kc, s=s: e.tensor_copy(out=wqkv[:, kc, :], in_=stg[s]), [f"stg{s}"], [f"wqkv{kc}"])
        for it in range(NTL + 9):
            for off in range(9, -1, -1):
                n = it - off
                if 0 <= n < NTL:
                    stages[off](n)

        tap("uT", uT, [("uT", i) for i in range(17)])
        tap("QT", QT, [("QT", i) for i in range(16)])
        tap("KT", KT, [("KT", i) for i in range(32)])
        tap("VX", VX, [("vx", i) for i in range(32)] + ["vx1"])
        S.barrier()
        if stop <= 1:
            S.emit(); return nc

        zt = region(Wreg, 2048, BF16)
        G(lambda e: e.memset(zt, 0.0), [], ["zt"])
        for i in range(8):
            D("gpsimd", "xsz", lambda e, i=i: e.dma_start(out=XS[i * 1024:(i + 1) * 1024, :].rearrange("(a p) n -> p a n", p=128), in_=zt.unsqueeze(1).to_broadcast([128, 8, 1024])), r=["zt"], w=["XS"])

        o = [OFF_B]
        PTs = [salloc(1024, BF16) for _ in range(4)]
        Otok = [salloc(2048, BF16).rearrange("p (h d) -> p h d", h=8) for _ in range(2)]
        rd = salloc(64, F32)
        SB = [0, 1, 6]
        seq = [(qt, g, kt) for qt in range(16) for g in range(2) for kt in range(32)]
        nbias = negc[:, 3:4]

        def issue_S(i):
            qt, g, kt = seq[i]
            b = SB[i % 3]
            T(lambda e: e.matmul(PS[:, b, :].rearrange("p (h q) -> p h q", h=4), lhsT=KT[:, g, kt * 128:(kt + 1) * 128], rhs=QT[:, qt, 4 * g:4 * g + 4, :], start=True, stop=True), [], [("ps", b)])

        def oacc(qt, g, h):
            sset = (qt * 2 + g) % 2
            bank = 2 + 2 * sset + h // 2
            return PS[:, bank, :].rearrange("p (j n) -> p j n", j=2), bank, h % 2

        issue_S(0); issue_S(1)

        def p2_step(i, qt, g, kt):
            if i + 2 < len(seq):
                issue_S(i + 2)
            b = SB[i % 3]; ps_ = PTs[i % 4]; pk_ = ("PT", i % 4)
            A(lambda e: e.activation(out=ps_, in_=PS[:, b, :], func=AF.Exp, bias=nbias, scale=1.0), [("ps", b), "negc"], [pk_])
            for h in range(4):
                ov, bank, j = oacc(qt, g, h)
                T(lambda e, h=h, ov=ov, j=j: e.matmul(ov[:, j, 0:129], lhsT=ps_[:, h * 128:(h + 1) * 128], rhs=VX[:, kt, g, 0:129], start=(kt == 0 and j == 0), stop=(kt == 31), skip_group_check=True), [pk_], [("ps", bank)])
            if kt == 31:
                os_ = Otok[qt % 2]; ok_ = ("Otok", qt % 2)
                for hb in range(2):
                    ov, bank, _ = oacc(qt, g, hb * 2)
                    V(lambda e, ov=ov, hb=hb: e.reciprocal(out=rd[:, 2 * hb:2 * hb + 2], in_=ov[:, :, 128]), [("ps", bank)], [("rd", hb)])
                    V(lambda e, ov=ov, hb=hb: e.tensor_tensor(out=os_[:, 4 * g + 2 * hb:4 * g + 2 * hb + 2, :], in0=ov[:, :, 0:128], in1=rd[:, 2 * hb:2 * hb + 2].unsqueeze(2).to_broadcast([128, 2, 128]), op=ALU.mult), [("ps", bank), ("rd", hb)], [ok_])
                if g == 1:
                    pt = psb(7).rearrange("p (c q) -> p c q", c=8)
                    for c in range(8):
                        T(lambda e, c=c: e.transpose(out=pt[:, c, :], in_=os_[:, c, :], identity=id_b), [ok_], [("ps", 7)])
                    V(lambda e: e.tensor_copy(out=OT[:, :, qt * 128:(qt + 1) * 128], in_=pt), [("ps", 7)], [("OT", qt)])

        for i_, (qt_, g_, kt_) in enumerate(seq):
            p2_step(i_, qt_, g_, kt_)
        tap("OT", OT, [("OT", i) for i in range(16)])
        S.barrier()
        if stop <= 2:
            S.emit(); return nc

        o = [OFF_B]
        ybuf = salloc(2052 * 4, F32)
        zb = [salloc(2048, F32) for _ in range(2)]
        ccs = [salloc(2048, F32) for _ in range(2)]
        o = [OFF_KT]
        cstg3 = [salloc(4096, F32, lim=OFF_D).rearrange("p (c n) -> p c n", c=8) for _ in range(3)]
        wcb3 = [salloc(2048, BF16, lim=OFF_D).rearrange("p (c n) -> p c n", c=8) for _ in range(6)]
        nld = [0]

        def load_chunk(col0):
            s = nld[0] % 3; sb_ = nld[0] % 6; nld[0] += 1
            D("sync", f"cstg{s}", lambda e: e.dma_start(out=cstg3[s], in_=w_in[:, col0:col0 + 128].rearrange("(c p) n -> p c n", p=128)), w=[f"cstg{s}"])
            A(lambda e: e.activation(out=wcb3[sb_], in_=cstg3[s], func=AF.Copy), [f"cstg{s}"], [f"wcb{sb_}"])
            return wcb3[sb_], f"wcb{sb_}"

        def mm8(bank, wch, wkey, rhs_fn, rkeys, n=512):
            for kc in range(8):
                T(lambda e, kc=kc: e.matmul(PS[:, bank, 0:n], lhsT=wch[:, kc, :], rhs=rhs_fn(kc), start=(kc == 0), stop=(kc == 7)), [wkey] + rkeys, [("ps", bank)])

        ukeys_all = [("uT", i) for i in range(17)]
        chunks = [load_chunk(1024 + 0), load_chunk(2048 + 0), load_chunk(0)]
        bk = [0]

        def nb():
            b = bk[0] % 8; bk[0] += 1
            return b
        for j in range(8):
            wcc, wcx, wcb_ = chunks
            if j + 1 < 8:
                chunks = [load_chunk(1024 + (j + 1) * 128), load_chunk(2048 + (j + 1) * 128), load_chunk((j + 1) * 128)]
            for nt in range(5):
                if nt < 4:
                    rf = lambda kc, nt=nt: uT[:, kc, nt * 512:(nt + 1) * 512]; n = 512; ycol = 1 + nt * 512
                else:
                    rf = lambda kc: uT[:, kc, 2048:2050]; n = 2
                b1 = nb(); b2 = nb()
                mm8(b1, wcc[0], wcc[1], rf, [], n)
                mm8(b2, wcx[0], wcx[1], rf, [], n)
                cs_ = ccs[nt % 2]; ck = ("ccs", nt % 2)
                A(lambda e, b1=b1, n=n, cs_=cs_: e.activation(out=cs_[:, 0:n], in_=PS[:, b1, 0:n], func=AF.Copy), [("ps", b1)], [ck])
                if nt < 4:
                    V(lambda e, b2=b2, cs_=cs_, ycol=ycol: e.tensor_tensor(out=ybuf[:, ycol:ycol + 512], in0=PS[:, b2, :], in1=cs_, op=ALU.mult), [("ps", b2), ck], [("y", nt)])
                else:
                    V(lambda e, b2=b2, cs_=cs_: e.tensor_tensor(out=ybuf[:, 0:1], in0=PS[:, b2, 0:1], in1=cs_[:, 0:1], op=ALU.mult), [("ps", b2), ck], [("y", 4)])
                    V(lambda e, b2=b2, cs_=cs_: e.tensor_tensor(out=ybuf[:, 2049:2050], in0=PS[:, b2, 1:2], in1=cs_[:, 1:2], op=ALU.mult), [("ps", b2), ck], [("y", 5)])
            ykeys = [("y", i) for i in range(6)]
            for nt in range(4):
                z = zb[nt % 2]; zk = ("z", nt % 2)
                c0 = nt * 512
                V(lambda e, z=z, c0=c0, j=j: e.tensor_scalar(out=z, in0=ybuf[:, c0:c0 + 512], scalar1=cw[:, 0, j:j + 1], scalar2=None, op0=ALU.mult), ykeys + ["cw"], [zk])
                V(lambda e, z=z, c0=c0, j=j: e.scalar_tensor_tensor(out=z, in0=ybuf[:, c0 + 1:c0 + 513], scalar=cw[:, 1, j:j + 1], in1=z, op0=ALU.mult, op1=ALU.add), ykeys + [zk], [zk])
                V(lambda e, z=z, c0=c0, j=j: e.scalar_tensor_tensor(out=z, in0=ybuf[:, c0 + 2:c0 + 514], scalar=cw[:, 2, j:j + 1], in1=z, op0=ALU.mult, op1=ALU.add), ykeys + [zk], [zk])
                b3 = nb()
                mm8(b3, wcb_[0], wcb_[1], lambda kc, nt=nt: uT[:, kc, nt * 512:(nt + 1) * 512], [])
                V(lambda e, b3=b3, z=z, nt=nt, j=j: e.tensor_tensor(out=mT[:, j, nt * 512:(nt + 1) * 512], in0=PS[:, b3, :], in1=z, op=ALU.mult), [("ps", b3), zk], [("mT", j, nt)])
        tap("mT", mT, [("mT", j, nt) for j in range(8) for nt in range(4)])
        S.barrier()
        if stop <= 3:
            S.emit(); return nc

        wco = region(Wreg, 16384, BF16).rearrange("p (c n) -> p c n", c=8)
        wao = region(OFF_KT, 16384, BF16).rearrange("p (c n) -> p c n", c=8)
        o = [OFF_VX]
        wstg4 = [salloc(4096, F32, lim=OFF_D) for _ in range(2)]
        sg = [salloc(2048, F32, lim=OFF_D) for _ in range(2)]
        tm = [salloc(2048, F32, lim=OFF_D) for _ in range(2)]
        o = [Wreg + 16384]
        cstg4 = [salloc(4096, F32, lim=P0).rearrange("p (c n) -> p c n", c=8) for _ in range(2)]
        wcb4 = [salloc(2048, BF16, lim=P0).rearrange("p (c n) -> p c n", c=8) for _ in range(2)]
        nld[0] = 0

        def load_chunk2(col0):
            s = nld[0] % 2; nld[0] += 1
            D("sync", f"cstg{s}", lambda e: e.dma_start(out=cstg4[s], in_=w_in[:, col0:col0 + 128].rearrange("(c p) n -> p c n", p=128)), w=[f"cstg{s}"])
            G(lambda e: e.tensor_copy(out=wcb4[s], in_=cstg4[s]), [f"cstg{s}"], [f"wcb{s}"])
            return wcb4[s], f"wcb{s}"

        def load_big(wdst, wsrc, name, ceng, wstg):
            for kc in range(8):
                s = kc % 2
                D("sync", f"wstg{s}", lambda e, kc=kc, s=s: e.dma_start(out=wstg[s], in_=wsrc[kc * 128:(kc + 1) * 128, :]), w=[f"wstg{s}"])
                S.add(ceng, lambda e, kc=kc, s=s: e.tensor_copy(out=wdst[:, kc, :], in_=wstg[s]), [f"wstg{s}"], [(name, kc)])
            return [(name, kc) for kc in range(8)]
        kco = load_big(wco, w_conv_out, "wco", "vector", wstg4)
        kao = load_big(wao, w_attn_out, "wao", "gpsimd", wstg4)
        gch = [load_chunk2(4608), load_chunk2(5632)]
        def p4_step(oc, nt, wgc, wga):
            if True:
                tk = slice(nt * 512, (nt + 1) * 512)
                byc, bgc, bya, bga = nb(), nb(), nb(), nb()
                for kc in range(8):
                    T(lambda e, kc=kc, byc=byc: e.matmul(PS[:, byc, :], lhsT=wco[:, kc, oc * 128:(oc + 1) * 128], rhs=mT[:, kc, tk], start=(kc == 0), stop=(kc == 7)), kco, [("ps", byc)])
                mm8(bgc, wgc[0], wgc[1], lambda kc: uT[:, kc, tk], [])
                for kc in range(8):
                    T(lambda e, kc=kc, bya=bya: e.matmul(PS[:, bya, :], lhsT=wao[:, kc, oc * 128:(oc + 1) * 128], rhs=OT[:, kc, tk], start=(kc == 0), stop=(kc == 7)), kao, [("ps", bya)])
                mm8(bga, wga[0], wga[1], lambda kc: uT[:, kc, tk], [])
                A(lambda e, bgc=bgc: e.activation(out=sg[0], in_=PS[:, bgc, :], func=AF.Sigmoid), [("ps", bgc)], ["sg0"])
                A(lambda e, bga=bga: e.activation(out=sg[1], in_=PS[:, bga, :], func=AF.Sigmoid), [("ps", bga)], ["sg1"])
                V(lambda e, byc=byc: e.tensor_tensor(out=tm[0], in0=PS[:, byc, :], in1=sg[0], op=ALU.mult), [("ps", byc), "sg0"], ["tm0"])
                V(lambda e, bya=bya: e.tensor_tensor(out=tm[1], in0=PS[:, bya, :], in1=sg[1], op=ALU.mult), [("ps", bya), "sg1"], ["tm1"])
                G(lambda e: e.tensor_tensor(out=Bbuf[:, oc, tk], in0=tm[0], in1=tm[1], op=ALU.add), ["tm0", "tm1"], [("mg", oc, nt)])

        for oc in range(8):
            wgc, wga = gch
            for nt in range(4):
                p4_step(oc, nt, wgc, wga)
            if oc + 1 < 8:
                gch = [load_chunk2(4608 + (oc + 1) * 128), load_chunk2(5632 + (oc + 1) * 128)]
        tap("mg", Bbuf, [("mg", oc, nt) for oc in range(8) for nt in range(4)])
        S.barrier()
        if stop <= 4:
            S.emit(); return nc

        wo = region(Wreg, 16384, BF16).rearrange("p (c n) -> p c n", c=8)
        o = [OFF_A]
        wstg5 = [salloc(4096, F32, lim=OFF_B) for _ in range(2)]
        xt5 = [salloc(4096, F32, lim=OFF_B) for _ in range(3)]
        x1t = [salloc(4096, F32, lim=OFF_B) for _ in range(2)]
        junk5 = salloc(4096, F32, lim=OFF_B)
        st5 = salloc(16 * 4 * 4, F32, lim=OFF_B).rearrange("p (t k) -> p t k", t=16)
        eps5 = salloc(4, F32, lim=OFF_B)
        o = [OFF_QT]
        sq2 = [salloc(4096, F32, lim=OFF_D) for _ in range(2)]
        u2f = [salloc(4096, F32, lim=OFF_D) for _ in range(2)]
        u2T = [salloc(4096, F32, lim=OFF_D).rearrange("p (c q) -> p c q", c=8) for _ in range(2)]
        LG = salloc(16 * 72 * 4, F32, lim=OFF_D).rearrange("p (t n) -> p t n", t=16)
        MA = [salloc(16 * 64 * 4, F32, lim=OFF_D).rearrange("p (t e) -> p t e", t=16) for _ in range(2)]
        Mf = salloc(16 * 64 * 4, F32, lim=OFF_D).rearrange("p (t e) -> p t e", t=16)
        Mcum = salloc(16 * 64 * 4, F32, lim=OFF_D).rearrange("p (t e) -> p t e", t=16)
        RkA = salloc(16 * 64 * 4, F32, lim=OFF_D).rearrange("p (t e) -> p t e", t=16)
        T64 = salloc(16 * 64 * 4, F32, lim=OFF_D).rearrange("p (t e) -> p t e", t=16)
        rb = salloc(16 * 64 * 4, F32, lim=OFF_D).rearrange("p (k t x) -> p k t x", k=8, t=16)
        rs_ = salloc(16 * 16 * 4, F32, lim=OFF_D).rearrange("p (k t) -> p k t", k=16)
        U2B = region(OFF_D, 32768, BF16).rearrange("p (t n) -> p t n", t=16)
        kwo = load_big(wo, w_o, "wo", "vector", wstg5)
        V(lambda e: e.memset(eps5, EPS), [], ["eps5"])

        def ld_x5(t):
            if t < 16:
                D("sync", f"x5{t % 3}", lambda e: e.dma_start(out=xt5[t % 3], in_=xa[t * 128:(t + 1) * 128, :]), w=[("x5", t % 3)])

        def q0(t):
            pb = 2 * (t % 2)
            for half in range(2):
                for kc in range(8):
                    T(lambda e, kc=kc, half=half: e.matmul(PS[:, pb + half, :], lhsT=Bbuf[:, kc, t * 128:(t + 1) * 128], rhs=wo[:, kc, half * 512:(half + 1) * 512], start=(kc == 0), stop=(kc == 7)), kwo, [("ps", pb + half)])

        def q1(t):
            pb = 2 * (t % 2); s = t % 2
            V(lambda e: e.tensor_tensor(out=x1t[s].rearrange("p (b n) -> p b n", b=2), in0=PS[:, pb:pb + 2, :], in1=xt5[t % 3].rearrange("p (b n) -> p b n", b=2), op=ALU.add), [("ps", pb), ("ps", pb + 1), ("x5", t % 3)], [("x1", s)])
            D("sync", f"x1o{s}", lambda e: e.dma_start(out=out[t * 128:(t + 1) * 128, :], in_=x1t[s]), r=[("x1", s)], w=[("out", t)])
            ld_x5(t + 2)

        def q2(t):
            s = t % 2
            ss = st5[:, t, 0:1]; rstd = st5[:, t, 1:2]; sk = ("st5", t)
            A(lambda e: e.activation(out=junk5, in_=x1t[s], func=AF.Square, accum_out=ss), [("x1", s)], [sk])
            A(lambda e: e.activation(out=rstd, in_=ss, func=AF.Ln, scale=1.0 / 1024, bias=eps5), [sk, "eps5"], [sk])
            A(lambda e: e.activation(out=rstd, in_=rstd, func=AF.Exp, scale=-0.5), [sk], [sk])
            A(lambda e: e.activation(out=sq2[s], in_=x1t[s], func=AF.Copy, scale=rstd), [("x1", s), sk], [("sq2", s)])

        def q3(t):
            s = t % 2
            V(lambda e: e.tensor_tensor(out=u2f[s], in0=sq2[s], in1=g2_bc, op=ALU.mult), [("sq2", s), "g2"], [("u2f", s)])

        def q4(t):
            s = t % 2
            A(lambda e: e.activation(out=U2B[:, t, :], in_=u2f[s], func=AF.Copy), [("u2f", s)], [("u2b", t)])
            for c in range(8):
                T(lambda e, c=c: e.transpose(out=PS[:, 4 + c // 4, (c % 4) * 128:(c % 4 + 1) * 128], in_=u2f[s][:, c * 128:(c + 1) * 128], identity=id_f), [("u2f", s), "cst"], [("ps", 4 + c // 4)])

        def q5(t):
            s = t % 2
            V(lambda e: e.tensor_copy(out=u2T[s], in_=PS[:, 4:6, :].rearrange("p b (c q) -> p (b c) q", c=4)), [("ps", 4), ("ps", 5)], [("u2T", s)])

        def q6(t):
            s = t % 2; c6 = 128 * (t % 2)
            for kc in range(8):
                T(lambda e, kc=kc: e.matmul(PS[:, 6, c6:c6 + 72], lhsT=u2T[s][:, kc, :], rhs=wgr[:, kc, :], start=(kc == 0), stop=(kc == 7)), [("u2T", s), "wgr"], [("ps", 6)])

        def q7(t):
            c6 = 128 * (t % 2)
            V(lambda e: e.tensor_copy(out=LG[:, t, :], in_=PS[:, 6, c6:c6 + 72]), [("ps", 6)], [("lg", t)])

        tri_f = cst[:, 128:256]; ones_f = cst[:, 256:384]
        NBT = 4

        def route_batch(bi):
            t0 = bi * NBT; t1 = t0 + NBT; nb_ = NBT
            ts = slice(t0, t1)
            K = lambda name: (name, bi)
            lgk = [("lg", t) for t in range(t0, t1)]
            lgg = LG[:, ts, 0:8]
            lge = LG[:, ts, 8:72].rearrange("p t (g x) -> p t g x", g=8)
            gm, nv1, v2, se, pg, dd, ed, w1_ = (rs_[:, i, ts] for i in range(8))
            ohg, eg, sel, oh1, sel2, oh2 = (rb[:, i, ts, :] for i in range(6))

            def bc3(a):
                return a.unsqueeze(2).to_broadcast([128, nb_, 8])
            V(lambda e: e.tensor_reduce(out=gm, in_=lgg, axis=AX.X, op=ALU.max), lgk, [K("gm")])
            V(lambda e: e.tensor_tensor(out=ohg, in0=lgg, in1=bc3(gm), op=ALU.is_equal), lgk + [K("gm")], [K("ohg")])
            V(lambda e: e.tensor_tensor(out=eg, in0=lgg, in1=bc3(gm), op=ALU.subtract), lgk + [K("gm")], [K("eg")])
            A(lambda e: e.activation(out=eg, in_=eg, func=AF.Exp), [K("eg")], [K("eg")])
            V(lambda e: e.tensor_reduce(out=se, in_=eg, axis=AX.X, op=ALU.add), [K("eg")], [K("se")])
            V(lambda e: e.reciprocal(out=pg, in_=se), [K("se")], [K("pg")])
            T64b = T64[:, ts, :]
            V(lambda e: e.tensor_tensor(out=T64b.rearrange("p t (g x) -> p t g x", g=8), in0=lge, in1=ohg.unsqueeze(3).to_broadcast([128, nb_, 8, 8]), op=ALU.mult), lgk + [K("ohg")], [K("t64")])
            V(lambda e: e.tensor_reduce(out=sel, in_=T64b.rearrange("p t (g x) -> p t x g", g=8), axis=AX.X, op=ALU.add), [K("t64")], [K("sel")])
            V(lambda e: e.tensor_reduce(out=nv1, in_=sel, axis=AX.X, op=ALU.max), [K("sel")], [K("v1")])
            V(lambda e: e.tensor_tensor(out=oh1, in0=sel, in1=bc3(nv1), op=ALU.is_equal), [K("sel"), K("v1")], [K("oh1")])
            V(lambda e: e.scalar_tensor_tensor(out=sel2, in0=oh1, scalar=-1e30, in1=sel, op0=ALU.mult, op1=ALU.add), [K("oh1"), K("sel")], [K("sel2")])
            V(lambda e: e.tensor_reduce(out=v2, in_=sel2, axis=AX.X, op=ALU.max), [K("sel2")], [K("v2")])
            V(lambda e: e.tensor_tensor(out=oh2, in0=sel2, in1=bc3(v2), op=ALU.is_equal), [K("sel2"), K("v2")], [K("oh2")])
            V(lambda e: e.tensor_tensor(out=dd, in0=v2, in1=nv1, op=ALU.subtract), [K("v1"), K("v2")], [K("dd")])
            A(lambda e: e.activation(out=ed, in_=dd, func=AF.Exp), [K("dd")], [K("ed")])
            V(lambda e: e.tensor_scalar(out=w1_, in0=ed, scalar1=1.0, scalar2=None, op0=ALU.add), [K("ed")], [K("w1_")])
            V(lambda e: e.reciprocal(out=w1_, in_=w1_), [K("w1_")], [K("w1_")])
            V(lambda e: e.tensor_tensor(out=Wk[:, 0, ts], in0=w1_, in1=pg, op=ALU.mult), [K("w1_"), K("pg")], [K("Wk0")])
            V(lambda e: e.tensor_tensor(out=Wk[:, 1, ts], in0=Wk[:, 0, ts], in1=ed, op=ALU.mult), [K("Wk0"), K("ed")], [K("Wk1")])
            for k, ohk in enumerate((oh1, oh2)):
                V(lambda e, k=k, ohk=ohk: e.tensor_tensor(out=MA[k][:, ts, :].rearrange("p t (g x) -> p t g x", g=8), in0=ohg.unsqueeze(3).to_broadcast([128, nb_, 8, 8]), in1=ohk.unsqueeze(2).to_broadcast([128, nb_, 8, 8]), op=ALU.mult), [K("ohg"), K("oh1"), K("oh2")], [("MA", k, bi)])
            V(lambda e: e.tensor_tensor(out=Mb[:, ts, :], in0=MA[0][:, ts, :], in1=MA[1][:, ts, :], op=ALU.add), [("MA", 0, bi), ("MA", 1, bi)], [("Mf", bi)])
            mfk = [("Mf", j) for j in range(bi + 1)]
            first = True
            for t in range(t0, t1):
                c0 = (t - t0) * 64
                T(lambda e, t=t, c0=c0, first=first: e.matmul(PS[:, 7, c0:c0 + 64], lhsT=tri_b, rhs=Mb[:, t, :], start=first, stop=(t == 0), skip_group_check=True), mfk + ["cstb"], [("ps", 7)])
                first = False
                for i in range(t):
                    T(lambda e, i=i, c0=c0, t=t: e.matmul(PS[:, 7, c0:c0 + 64], lhsT=ones_b, rhs=Mb[:, i, :], start=False, stop=(i == t - 1), skip_group_check=True), mfk + ["cstb"], [("ps", 7)])
            RkB = RkA[:, ts, :]
            V(lambda e: e.tensor_copy(out=RkB.rearrange("p t e -> p (t e)"), in_=PS[:, 7, 0:nb_ * 64]), [("ps", 7)], [K("Rk")])
            rsel, bsel, ov = (rs_[:, 8 + i, ts] for i in range(3))
            for k in range(2):
                V(lambda e, k=k: e.tensor_tensor(out=T64b, in0=RkB, in1=MA[k][:, ts, :], op=ALU.mult), [K("Rk"), ("MA", k, bi)], [K("t64")])
                V(lambda e: e.tensor_reduce(out=rsel, in_=T64b, axis=AX.X, op=ALU.add), [K("t64")], [K("rsel")])
                V(lambda e, k=k: e.tensor_tensor(out=T64b, in0=MA[k][:, ts, :], in1=ebase.unsqueeze(1).to_broadcast([128, nb_, 64]), op=ALU.mult), [("MA", k, bi), "cst", K("rsel")], [K("t64")])
                V(lambda e: e.tensor_reduce(out=bsel, in_=T64b, axis=AX.X, op=ALU.add), [K("t64")], [K("bsel")])
                V(lambda e: e.tensor_scalar(out=ov, in0=rsel, scalar1=float(CAP) - 0.5, scalar2=100000.0, op0=ALU.is_ge, op1=ALU.mult), [K("rsel")], [K("ov")])
                V(lambda e: e.tensor_tensor(out=bsel, in0=bsel, in1=rsel, op=ALU.add), [K("bsel"), K("rsel")], [K("bsel")])
                V(lambda e: e.tensor_tensor(out=bsel, in0=bsel, in1=ov, op=ALU.add), [K("bsel"), K("ov")], [K("bsel")])
                V(lambda e, k=k: e.tensor_copy(out=idx[:, k, ts], in_=bsel), [K("bsel")], [("idx", k, bi)])
            for t in range(t0, t1):
                for k in range(2):
                    D("gpsimd", "scat", lambda e, k=k, t=t: e.indirect_dma_start(out=XS, out_offset=bass.IndirectOffsetOnAxis(ap=idx[:, k, t:t + 1], axis=0), in_=U2B[:, t, :], in_offset=None, bounds_check=bnd['r'], oob_is_err=False), r=[("idx", k, bi), ("u2b", t), "XS"], w=["XSs"])

        qst = [q0, q1, q2, q3, q4, q5, q6, q7]
        ld_x5(0); ld_x5(1)
        for it in range(16 + 7):
            for off in range(7, -1, -1):
                t_ = it - off
                if 0 <= t_ < 16:
                    qst[off](t_)
            tdone = it - 7
            if tdone >= 0 and (tdone + 1) % NBT == 0:
                route_batch(tdone // NBT)
        tap("idx", idx, [("idx", k, bi) for k in range(2) for bi in range(4)])
        tap("Wk", Wk, [(n_, bi) for n_ in ("Wk0", "Wk1") for bi in range(4)])
        S.barrier()
        if stop <= 5:
            S.emit(); return nc

        o = [0]
        LIM = P0
        wst = [salloc(16384, F32, lim=LIM) for _ in range(4)]
        wb1 = [salloc(8192, BF16, lim=LIM).rearrange("p (c n) -> p c n", c=8) for _ in range(2)]
        wb3 = [salloc(8192, BF16, lim=LIM).rearrange("p (c n) -> p c n", c=8) for _ in range(2)]
        wb2 = [salloc(8192, BF16, lim=LIM).rearrange("p (c n) -> p c n", c=4) for _ in range(2)]
        xs_sb = [salloc(2048, BF16, lim=LIM) for _ in range(2)]
        xsT = [salloc(2048, BF16, lim=LIM).rearrange("p (c q) -> p c q", c=8) for _ in range(2)]
        sh = salloc(2048, F32, lim=LIM)
        act = salloc(1024, BF16, lim=LIM)
        aT = salloc(1024, BF16, lim=LIM).rearrange("p (c q) -> p c q", c=4)
        ysb = [salloc(4096, F32, lim=LIM) for _ in range(2)]
        nst = [0]

        def load_w(e_):
            keys = []
            for (wsrc, wdst, ceng, name) in ((w1, wb1, "vector", "wb1"), (w3, wb3, "gpsimd", "wb3"), (w2, wb2, "scalar", "wb2")):
                s = nst[0] % 4; nst[0] += 1
                d = wdst[e_ % 2]
                if name == "wb2":
                    src = wsrc[e_].rearrange("(c p) n -> p c n", p=128)
                    stv = wst[s].rearrange("p (c n) -> p c n", c=4)
                else:
                    src = wsrc[e_].rearrange("(c p) n -> p c n", p=128)
                    stv = wst[s].rearrange("p (c n) -> p c n", c=8)
                D("sync", f"wst{s}", lambda e, stv=stv, src=src: e.dma_start(out=stv, in_=src), w=[f"wst{s}"])
                if ceng == "scalar":
                    S.add(ceng, lambda e, d=d, stv=stv: e.activation(out=d, in_=stv, func=AF.Copy), [f"wst{s}"], [(name, e_ % 2)])
                else:
                    S.add(ceng, lambda e, d=d, stv=stv: e.tensor_copy(out=d, in_=stv), [f"wst{s}"], [(name, e_ % 2)])

        def load_xs(e_):
            s = e_ % 2
            D("sync", f"xs{s}", lambda e: e.dma_start(out=xs_sb[s], in_=XS[e_ * CAP:(e_ + 1) * CAP, :]), r=["XSs", "XS"], w=[("xs", s)])

        load_w(0); load_xs(0)

        def p6_expert(e_):
            if e_ + 1 < NEXP:
                load_w(e_ + 1); load_xs(e_ + 1)
            s = e_ % 2
            pt = psb(0).rearrange("p (c q) -> p c q", c=8)
            for c in range(8):
                T(lambda e, c=c: e.transpose(out=pt[:, c, :], in_=xs_sb[s][:, c * 128:(c + 1) * 128], identity=id_b), [("xs", s)], [("ps", 0)])
            V(lambda e: e.tensor_copy(out=xsT[s], in_=pt), [("ps", 0)], [("xsT", s)])
            for kc in range(8):
                T(lambda e, kc=kc: e.matmul(PS[:, 1, :], lhsT=xsT[s][:, kc, :], rhs=wb1[s][:, kc, :], start=(kc == 0), stop=(kc == 7)), [("xsT", s), ("wb1", s)], [("ps", 1)])
            for kc in range(8):
                T(lambda e, kc=kc: e.matmul(PS[:, 2, :], lhsT=xsT[s][:, kc, :], rhs=wb3[s][:, kc, :], start=(kc == 0), stop=(kc == 7)), [("xsT", s), ("wb3", s)], [("ps", 2)])
            A(lambda e: e.activation(out=sh, in_=PS[:, 1, :], func=AF.Silu), [("ps", 1)], ["sh"])
            V(lambda e: e.tensor_tensor(out=act, in0=PS[:, 2, :], in1=sh, op=ALU.mult), [("ps", 2), "sh"], ["act"])
            pa = psb(3).rearrange("p (c q) -> p c q", c=8)
            for c in range(4):
                T(lambda e, c=c: e.transpose(out=pa[:, c, :], in_=act[:, c * 128:(c + 1) * 128], identity=id_b), ["act"], [("ps", 3)])
            V(lambda e: e.tensor_copy(out=aT, in_=pa[:, 0:4, :]), [("ps", 3)], ["aT"])
            for half in range(2):
                for c in range(4):
                    T(lambda e, c=c, half=half: e.matmul(PS[:, 4 + half, :], lhsT=aT[:, c, :], rhs=wb2[s][:, c, half * 512:(half + 1) * 512], start=(c == 0), stop=(c == 3)), ["aT", ("wb2", s)], [("ps", 4 + half)])
            yk = ("ysb", s)
            A(lambda e: e.activation(out=ysb[s][:, 0:512], in_=PS[:, 4, :], func=AF.Copy), [("ps", 4)], [(yk, 0)])
            V(lambda e: e.tensor_copy(out=ysb[s][:, 512:1024], in_=PS[:, 5, :]), [("ps", 5)], [(yk, 1)])
            D("gpsimd", f"ys{s}", lambda e: e.dma_start(out=YS[e_ * CAP:(e_ + 1) * CAP, :], in_=ysb[s]), r=[(yk, 0), (yk, 1)], w=["YS"])

        for e2 in range(NEXP):
            p6_expert(e2)
        S.barrier()
        if stop <= 6:
            S.emit(); return nc

        o = [0]
        y0 = [salloc(4096, F32, lim=LIM) for _ in range(2)]
        y1 = [salloc(4096, F32, lim=LIM) for _ in range(2)]
        x1b = [salloc(4096, F32, lim=LIM) for _ in range(2)]
        def p7_tile(t):
            s = t % 2
            D("sync", f"x7{s}", lambda e: e.dma_start(out=x1b[s], in_=out[t * 128:(t + 1) * 128, :]), w=[("x7", s)])
            for k, yb in enumerate((y0, y1)):
                G(lambda e, yb=yb: e.memset(yb[s], 0.0), [], [("y", k, s)])
                D("gpsimd", f"gat{k}{s}", lambda e, k=k, yb=yb: e.indirect_dma_start(out=yb[s], out_offset=None, in_=YS, in_offset=bass.IndirectOffsetOnAxis(ap=idx[:, k, t:t + 1], axis=0), bounds_check=bnd['r'], oob_is_err=False), r=[("y", k, s)], w=[("y", k, s)])
            V(lambda e: e.scalar_tensor_tensor(out=x1b[s], in0=y0[s], scalar=Wk[:, 0, t:t + 1], in1=x1b[s], op0=ALU.mult, op1=ALU.add), [("y", 0, s), ("x7", s)], [("x7", s)])
            V(lambda e: e.scalar_tensor_tensor(out=x1b[s], in0=y1[s], scalar=Wk[:, 1, t:t + 1], in1=x1b[s], op0=ALU.mult, op1=ALU.add), [("y", 1, s), ("x7", s)], [("x7", s)])
            D("sync", f"fo{s}", lambda e: e.dma_start(out=out[t * 128:(t + 1) * 128, :], in_=x1b[s]), r=[("x7", s)])

        for t_ in range(16):
            p7_tile(t_)
        S.emit()
    return nc


def _rope_tables():
    S = 4096; GW = 64
    rows = S // GW
    row = np.repeat(np.arange(rows, dtype=np.float32), GW)
    col = np.tile(np.arange(GW, dtype=np.float32), rows)
    inv = (np.float32(10000.0) ** (-np.arange(0, 64, 2, dtype=np.float32) / np.float32(64))).astype(np.float32)
    ang = np.concatenate([row[:, None] * inv, col[:, None] * inv], axis=-1).astype(np.float32)
    return np.concatenate([np.cos(ang), np.sin(ang)], axis=-1).astype(np.float32)


def _consts():
    c = np.zeros((128, 512), np.float32)
    c[:, 0:128] = np.eye(128, dtype=np.float32)
    c[:, 128:256] = np.triu(np.ones((128, 128), np.float32), k=1)
    c[:, 256:384] = 1.0
    c[:, 384:448] = (np.arange(64, dtype=np.float32) * CAP)[None, :]
    return c


def core_inputs(inputs, c):
    b, hq = c // 2, c % 2
    x = np.asarray(inputs["x"], np.float32)
    t0 = hq * 2048
    xa = np.zeros((4224, 1024), np.float32)
    xa[0:2048] = x[b, t0:t0 + 2048]
    xa[2048:4096] = x[b, (1 - hq) * 2048:(1 - hq) * 2048 + 2048]
    if t0 > 0:
        xa[4096] = x[b, t0 - 1]
    if t0 + 2048 < 4096:
        xa[4097] = x[b, t0 + 2048]
    tab = _rope_tables()
    cs = np.concatenate([tab[t0:t0 + 2048], tab[(1 - hq) * 2048:(1 - hq) * 2048 + 2048]], axis=0)
    f = lambda k: np.ascontiguousarray(np.asarray(inputs[k], np.float32)[0])
    return {
        "xa": xa, "cs": np.ascontiguousarray(cs),
        "norm1_g": f("norm1_g"), "norm2_g": f("norm2_g"), "w_in": f("w_in"), "conv_w": f("conv_w"),
        "q_norm_g": f("q_norm_g"), "k_norm_g": f("k_norm_g"), "w_conv_out": f("w_conv_out"),
        "w_attn_out": f("w_attn_out"), "w_o": f("w_o"),
        "w_gr": np.ascontiguousarray(np.concatenate([f("w_group"), f("w_router")], axis=1)),
        "w1": f("w1"), "w3": f("w3"), "w2": f("w2"), "consts": _consts(),
    }


def kernel(**inputs):
    nc = build()
    shared = core_inputs(inputs, 0)
    in_maps = []
    for c in range(8):
        m = dict(shared)
        if c:
            ci = core_inputs_light(inputs, c)
            m.update(ci)
        in_maps.append(m)
    res = run_bass_kernel_spmd(nc, in_maps, core_ids=list(range(8)))
    outp = np.zeros((4, 4096, 1024), np.float32)
    for c in range(8):
        b, hq = c // 2, c % 2
        outp[b, hq * 2048:(hq + 1) * 2048] = res.results[c]["out"]
    return outp


def core_inputs_light(inputs, c):
    b, hq = c // 2, c % 2
    x = np.asarray(inputs["x"], np.float32)
    t0 = hq * 2048
    xa = np.zeros((4224, 1024), np.float32)
    xa[0:2048] = x[b, t0:t0 + 2048]
    xa[2048:4096] = x[b, (1 - hq) * 2048:(1 - hq) * 2048 + 2048]
    if t0 > 0:
        xa[4096] = x[b, t0 - 1]
    if t0 + 2048 < 4096:
        xa[4097] = x[b, t0 + 2048]
    tab = _rope_tables()
    cs = np.concatenate([tab[t0:t0 + 2048], tab[(1 - hq) * 2048:(1 - hq) * 2048 + 2048]], axis=0)
    return {"xa": xa, "cs": np.ascontiguousarray(cs)}
```

```python
import contextlib
import numpy as np
import concourse.bass as bass
import concourse.mybir as mybir
from concourse.bass_utils import run_bass_kernel_spmd

F32 = mybir.dt.float32
BF16 = mybir.dt.bfloat16
I32 = mybir.dt.int32
U8 = mybir.dt.uint8
AF = mybir.ActivationFunctionType
ALU = mybir.AluOpType
AX = mybir.AxisListType

NT_OWN = 16
NT_ALL = 32
CAP = 128
NEXP = 64
EPS = 1e-6


class Op:
    __slots__ = ("eng", "fn", "idx", "waits", "inc", "count", "is_dma", "semkey")


class Sched:
    ENGS = ("sync", "scalar", "vector", "gpsimd", "tensor")

    def __init__(self, nc):
        self.nc = nc
        self.ops = {e: [] for e in self.ENGS}
        self.tiles = {}
        self.dma_count = {}
        self.seg_marks = []

    def core_barrier(self, prefetch=None):
        self.barrier()
        if prefetch is not None:
            prefetch()
        self.seg_marks.append({e: len(self.ops[e]) for e in self.ENGS})

    def add(self, eng, fn, reads=(), writes=(), semkey=None):
        op = Op()
        op.eng = eng; op.fn = fn; op.idx = len(self.ops[eng]); op.inc = False
        op.is_dma = semkey is not None; op.semkey = semkey; op.count = None
        deps = []
        for k in reads:
            st = self.tiles.get(k)
            if st and st[0] is not None:
                deps.append(st[0])
        for k in writes:
            st = self.tiles.get(k)
            if st:
                if st[0] is not None:
                    deps.append(st[0])
                deps.extend(st[1])
        waits = {}
        for d in deps:
            if d.is_dma:
                key = ("dma", d.semkey); val = self.dma_count[d.semkey]
                waits[key] = max(waits.get(key, 0), val)
            else:
                if d.eng == "tensor" and eng == "tensor" and not op.is_dma:
                    continue
                d.inc = True
                key = ("eng", d.eng)
                cur = waits.get(key)
                if cur is None or d.idx > cur.idx:
                    waits[key] = d
        op.waits = waits
        if op.is_dma:
            self.dma_count[semkey] = self.dma_count.get(semkey, 0) + 16
        for k in reads:
            st = self.tiles.setdefault(k, [None, []])
            st[1].append(op)
        for k in writes:
            self.tiles[k] = [op, []]
        self.ops[eng].append(op)
        return op

    def barrier(self):
        last = {}
        for e in self.ENGS:
            for op in reversed(self.ops[e]):
                if not op.is_dma and op.fn is not None:
                    last[e] = op
                    break
        dma_snapshot = dict(self.dma_count)
        for e in self.ENGS:
            op = Op()
            op.eng = e; op.fn = None; op.idx = len(self.ops[e]); op.inc = False
            op.is_dma = False; op.semkey = None; op.count = None
            waits = {}
            for f, lo in last.items():
                if f == "tensor" and e == "tensor":
                    continue
                lo.inc = True
                waits[("eng", f)] = lo
            for k, v in dma_snapshot.items():
                waits[("dma", k)] = v
            op.waits = waits
            self.ops[e].append(op)
        self.tiles.clear()

    def emit(self, final_eng="gpsimd"):
        nc = self.nc
        for e in self.ENGS:
            c = 0
            for op in self.ops[e]:
                if op.inc and not op.is_dma:
                    c += 1
                    op.count = c
        semkeys = sorted(self.dma_count.keys(), key=str)
        marks = self.seg_marks + [{e: len(self.ops[e]) for e in self.ENGS}]
        with contextlib.ExitStack() as st:
            esem = {e: st.enter_context(nc.semaphore("es_" + e)) for e in self.ENGS}
            dsem = {k: st.enter_context(nc.semaphore("ds_" + str(k))) for k in semkeys}
            known = {e: {} for e in self.ENGS}

            def run(e, eng, lo, hi, last):
                kn = known[e]
                for op in self.ops[e][lo:hi]:
                    for key, val in op.waits.items():
                        if key[0] == "dma":
                            sem = dsem[key[1]]; v = val
                        else:
                            sem = esem[key[1]]; v = val.count
                        if kn.get(key, 0) >= v:
                            continue
                        kn[key] = v
                        eng.wait_ge(sem, v)
                    if op.fn is None:
                        continue
                    ins = op.fn(eng)
                    if op.is_dma:
                        ins.then_inc(dsem[op.semkey], 16)
                    elif op.inc:
                        ins.then_inc(esem[e], 1)
                if last and e == final_eng:
                    for k in semkeys:
                        if kn.get(("dma", k), 0) < self.dma_count[k]:
                            eng.wait_ge(dsem[k], self.dma_count[k])

            prev = {e: 0 for e in self.ENGS}
            for si, mk in enumerate(marks):
                last = si == len(marks) - 1
                with nc.Block() as block:
                    @block.sync
                    def _(eng): run("sync", eng, prev["sync"], mk["sync"], last)

                    @block.scalar
                    def _(eng): run("scalar", eng, prev["scalar"], mk["scalar"], last)

                    @block.vector
                    def _(eng): run("vector", eng, prev["vector"], mk["vector"], last)

                    @block.gpsimd
                    def _(eng): run("gpsimd", eng, prev["gpsimd"], mk["gpsimd"], last)

                    @block.tensor
                    def _(eng): run("tensor", eng, prev["tensor"], mk["tensor"], last)
                prev = mk
                if not last:
                    nc.all_core_barrier()


def build(stop=99, taps=(), ncores=8):
    nc = bass.Bass("TRN2", target_bir_lowering=False, num_devices=ncores)

    def din(name, shape, dt=F32):
        return nc.dram_tensor(name, list(shape), dt, kind="ExternalInput").ap()

    xa = din("xa", [4224, 1024])
    cs = din("cs", [4096, 128])
    norm1_g = din("norm1_g", [1024])
    norm2_g = din("norm2_g", [1024])
    w_in = din("w_in", [1024, 6656])
    conv_w = din("conv_w", [3, 1024])
    q_norm_g = din("q_norm_g", [128])
    k_norm_g = din("k_norm_g", [128])
    w_conv_out = din("w_conv_out", [1024, 1024])
    w_attn_out = din("w_attn_out", [1024, 1024])
    w_o = din("w_o", [1024, 1024])
    w_gr = din("w_gr", [1024, 72])
    NEH = NEXP // 2
    w1 = din("w1", [NEH, 1024, 512])
    w3 = din("w3", [NEH, 1024, 512])
    w2 = din("w2", [NEH, 512, 1024])
    xblk_d = din("xblk", [128, 64], I32)
    consts = din("consts", [128, 512])
    out = nc.dram_tensor("out", [2048, 1024], F32, kind="ExternalOutput").ap()
    NROW = 2 * NEXP * CAP
    XS = nc.dram_tensor("XS", [NROW, 1024], BF16, addr_space="Shared").ap()
    YS = nc.dram_tensor("YS", [NROW, 1024], BF16, addr_space="Shared").ap()
    tap_out = {}
    for (name, shape, dt) in taps:
        tap_out[name] = nc.dram_tensor("tap_" + name, list(shape), dt, kind="ExternalOutput").ap()

    with contextlib.ExitStack() as st:
        TOTAL = 212000
        arena = st.enter_context(nc.sbuf_tensor("arena", [128, TOTAL], U8))
        PS = st.enter_context(nc.psum_tensor("ps", [128, 8, 512], F32))
        PSb = PS[:].bitcast(BF16) if hasattr(PS[:], "bitcast") else None

        def region(off, nbytes, dt):
            assert off % 4 == 0 and off + nbytes <= TOTAL, (off, nbytes)
            return arena[:, off:off + nbytes].bitcast(dt)

        S = Sched(nc)

        def V(fn, r=(), w=()): return S.add("vector", fn, r, w)
        def A(fn, r=(), w=()): return S.add("scalar", fn, r, w)
        def G(fn, r=(), w=()): return S.add("gpsimd", fn, r, w)
        def T(fn, r=(), w=()): return S.add("tensor", fn, r, w)
        def D(q, key, fn, r=(), w=()): return S.add(q, fn, r, w, semkey=key)

        def psb(bank):
            return PSb[:, bank, :]

        P0 = 198000
        p = [P0]

        def palloc(nbytes, dt):
            r = region(p[0], nbytes, dt); p[0] += (nbytes + 3) // 4 * 4
            return r
        cst = palloc(512 * 4, F32)
        id_f = cst[:, 0:128]; ebase = cst[:, 384:448]
        cstb = palloc(384 * 2, BF16)
        id_b = cstb[:, 0:128]; tri_b = cstb[:, 128:256]; ones_b = cstb[:, 256:384]
        g1T = palloc(8 * 4, F32)
        cw = palloc(24 * 4, F32).rearrange("p (k c) -> p k c", k=3)
        gq_bc = palloc(128 * 4, F32)
        gk_bc = palloc(128 * 4, F32)
        negc = palloc(4 * 4, F32)
        wgr = palloc(8 * 72 * 4, F32).rearrange("p (c n) -> p c n", c=8)
        Wk = palloc(2 * 16 * 4, F32).rearrange("p (k t) -> p k t", k=2)
        idx = palloc(2 * 16 * 4, I32).rearrange("p (k t) -> p k t", k=2)
        Mb = palloc(16 * 64 * 2, BF16).rearrange("p (t e) -> p t e", t=16)
        g2_bc = palloc(1024 * 4, F32)
        gq_p = palloc(128 * 4, F32)
        xblk = palloc(64 * 4, I32)
        gk_p = palloc(128 * 4, F32)
        assert p[0] <= TOTAL, p[0]

        OFF_A, OFF_B, OFF_QT, OFF_KT, OFF_VX, OFF_D, OFF_W = 0, 34816, 67584, 100352, 116736, 133376, 166144
        uT = region(OFF_A, 34816, BF16).rearrange("p (c t) -> p c t", c=8)
        Bbuf = region(OFF_B, 32768, BF16).rearrange("p (c t) -> p c t", c=8)
        QT = region(OFF_QT, 32768, BF16).rearrange("p (t h q) -> p t h q", t=16, h=8)
        KT = region(OFF_KT, 16384, BF16).rearrange("p (g t) -> p g t", g=2)
        VX = region(OFF_VX, 16640, BF16).rearrange("p (t g d) -> p t g d", t=32, g=2)
        OT = region(OFF_D, 32768, BF16).rearrange("p (c t) -> p c t", c=8)
        mT = region(OFF_QT, 32768, BF16).rearrange("p (c t) -> p c t", c=8)
        Wreg = OFF_W

        D("sync", "const", lambda e: e.dma_start(out=cst, in_=consts), w=["cst"])
        D("sync", "const", lambda e: e.dma_start(out=g1T, in_=norm1_g.rearrange("(c p) -> p c", p=128), allow_slow_non_contiguous=True), w=["g1T"])
        D("sync", "const", lambda e: e.dma_start(out=cw, in_=conv_w.rearrange("k (c p) -> p k c", p=128), allow_slow_non_contiguous=True), w=["cw"])
        D("sync", "const", lambda e: e.dma_start(out=gq_bc, in_=q_norm_g.partition_broadcast(128)), w=["gq"])
        D("sync", "const", lambda e: e.dma_start(out=gk_bc, in_=k_norm_g.partition_broadcast(128)), w=["gk"])
        D("sync", "const", lambda e: e.dma_start(out=g2_bc, in_=norm2_g.partition_broadcast(128)), w=["g2"])
        D("sync", "const", lambda e: e.dma_start(out=wgr, in_=w_gr.rearrange("(c p) n -> p c n", p=128)), w=["wgr"])
        D("sync", "const", lambda e: e.dma_start(out=xblk, in_=xblk_d), w=["xblk"])
        V(lambda e: e.tensor_copy(out=cstb, in_=cst[:, 0:384]), ["cst"], ["cstb"])
        V(lambda e: e.tensor_reduce(out=negc[:, 0:1], in_=gq_bc, axis=AX.X, op=ALU.max, apply_absolute_value=True), ["gq"], ["negc0"])
        V(lambda e: e.tensor_reduce(out=negc[:, 1:2], in_=gk_bc, axis=AX.X, op=ALU.max, apply_absolute_value=True), ["gk"], ["negc1"])
        V(lambda e: e.tensor_tensor(out=negc[:, 2:3], in0=negc[:, 0:1], in1=negc[:, 1:2], op=ALU.mult), ["negc0", "negc1"], ["negc2"])
        V(lambda e: e.tensor_scalar(out=negc[:, 3:4], in0=negc[:, 2:3], scalar1=-(128.0 ** 0.5), scalar2=None, op0=ALU.mult), ["negc2"], ["negc"])
        bnd = {}

        def mk_bnd(e):
            bnd['r'] = e.alloc_register("bnd")
            return e.reg_mov(bnd['r'], NROW - 1)
        G(mk_bnd)
        G(lambda e: e.memset(VX[:, :, :, 128:130], 1.0), [], ["vx1"])

        def tap(name, src_ap, keys):
            if name in tap_out:
                D("gpsimd", "tap_" + name, lambda e: e.dma_start(out=tap_out[name], in_=src_ap), r=keys)

        wqkv = region(Wreg, 24576, BF16).rearrange("p (c n) -> p c n", c=8)
        o = [OFF_B]

        def salloc(nbytes, dt, lim=OFF_B + 32768):
            r = region(o[0], nbytes, dt); o[0] += (nbytes + 3) // 4 * 4
            assert o[0] <= lim, (o[0], lim)
            return r
        xt = [salloc(4096, F32) for _ in range(3)]
        xn = [salloc(2048, BF16) for _ in range(2)]
        junk = salloc(4096, F32)
        uTo = [salloc(2048, BF16).rearrange("p (c t) -> p c t", c=8) for _ in range(2)]
        qkc = salloc(5120, F32)
        qkc_o = salloc(1024, F32)
        o = [OFF_D]

        def salloc2(nbytes, dt):
            return salloc(nbytes, dt, lim=OFF_D + 32768)
        qn = [salloc2(5120, F32) for _ in range(2)]
        qn_o = [salloc2(1024, F32) for _ in range(2)]
        tmp4 = [salloc2(2560, F32) for _ in range(4)]
        qr = [salloc2(2560, BF16) for _ in range(2)]
        cst_t = [salloc2(512, F32) for _ in range(3)]
        st1 = salloc2(4 * 32 * 4, F32).rearrange("p (s k) -> p s k", s=4)
        epsb = salloc2(4, F32)
        stg = [region(OFF_D + 12288 + i * 6144, 6144, F32) for i in range(2)]

        V(lambda e: e.memset(epsb, EPS), [], ["eps"])
        wq_keys = [f"wqkv{kc}" for kc in range(8)]
        V(lambda e: e.tensor_scalar(out=gq_p.rearrange("p (two i) -> p two i", two=2), in0=gq_bc.rearrange("p (i two) -> p two i", two=2), scalar1=128.0 ** -0.5, scalar2=None, op0=ALU.mult), ["gq"], ["gqp"])
        V(lambda e: e.tensor_copy(out=gk_p.rearrange("p (two i) -> p two i", two=2), in_=gk_bc.rearrange("p (i two) -> p two i", two=2)), ["gk"], ["gkp"])

        tile_order = []
        for i in range(16):
            tile_order += [i, 16 + i]
        tile_order.append(32)
        NTL = len(tile_order)

        def load_x(n):
            if n >= NTL:
                return
            t = tile_order[n]; s = n % 3
            D("sync", f"xt{s}", lambda e: e.dma_start(out=xt[s], in_=xa[t * 128:(t + 1) * 128, :]), w=[f"xt{s}"])

        def udst_of(n):
            t = tile_order[n]
            if t < 16:
                return uT[:, :, t * 128:(t + 1) * 128], ("uT", t)
            if t == 32:
                return uT[:, :, 2048:2176], ("uT", 16)
            return uTo[(n // 2) % 2], ("uTo", (n // 2) % 2)

        def tinfo(n):
            t = tile_order[n]
            own = t < 16
            nq = 10 if own else 2
            return t, own, nq

        def st_S0(n):
            s = n % 3; s2 = n % 2
            xts = xt[s]; xns = xn[s2]
            ss = st1[:, n % 4, 0:1]; rstd = st1[:, n % 4, 1:2]
            sk = ("ss", n % 4)
            A(lambda e: e.activation(out=junk[:, 0:1024], in_=xts, func=AF.Square, accum_out=ss), [f"xt{s}"], [sk])
            A(lambda e: e.activation(out=rstd, in_=ss, func=AF.Ln, scale=1.0 / 1024, bias=epsb), [sk, "eps"], [sk])
            A(lambda e: e.activation(out=rstd, in_=rstd, func=AF.Exp, scale=-0.5), [sk], [sk])
            A(lambda e: e.activation(out=xns, in_=xts, func=AF.Copy, scale=rstd), [f"xt{s}", sk], [f"xn{s2}"])
            load_x(n + 2)

        def st_S1(n):
            s2 = n % 2
            xns = xn[s2]
            pt = psb(s2).rearrange("p (c q) -> p c q", c=8)
            for c in range(8):
                T(lambda e, c=c: e.transpose(out=pt[:, c, :], in_=xns[:, c * 128:(c + 1) * 128], identity=id_b), [f"xn{s2}", "cstb"], [("ps", s2)])

        def st_S2(n):
            s2 = n % 2
            pt = psb(s2).rearrange("p (c q) -> p c q", c=8)
            udst, ukey = udst_of(n)
            V(lambda e: e.tensor_tensor(out=udst, in0=pt, in1=g1T.unsqueeze(2).to_broadcast([128, 8, 128]), op=ALU.mult), [("ps", s2), "g1T"], [ukey])

        def st_S3(n):
            t, own, nq = tinfo(n)
            if t == 32:
                return
            udst, ukey = udst_of(n)
            kvb = 4 if own else 5
            for kc in range(8):
                lhs = udst[:, kc, :]
                if own:
                    T(lambda e, kc=kc, lhs=lhs: e.matmul(PS[:, 2, :], lhsT=lhs, rhs=wqkv[:, kc, 0:512], start=(kc == 0), stop=(kc == 7)), [ukey] + wq_keys, [("ps", 2)])
                    T(lambda e, kc=kc, lhs=lhs: e.matmul(PS[:, 3, :], lhsT=lhs, rhs=wqkv[:, kc, 512:1024], start=(kc == 0), stop=(kc == 7)), [ukey], [("ps", 3)])
                T(lambda e, kc=kc, lhs=lhs: e.matmul(PS[:, kvb, :], lhsT=lhs, rhs=wqkv[:, kc, 1024:1536], start=(kc == 0), stop=(kc == 7)), [ukey] + wq_keys, [("ps", kvb)])

        def st_S4(n):
            t, own, nq = tinfo(n)
            if t == 32:
                return
            s3 = n % 3
            D("sync", f"cs{s3}", lambda e: e.dma_start(out=cst_t[s3], in_=cs[t * 128:(t + 1) * 128, :]), w=[f"cs{s3}"])
            if own:
                qkps = PS[:, 2:5, :].rearrange("p b n -> p (b n)")[:, 0:1280]
                pkeys = [("ps", 2), ("ps", 3), ("ps", 4)]; kvb = 4
                cdst = qkc; ckey = "qkc"
            else:
                qkps = PS[:, 5, 0:256]
                pkeys = [("ps", 5)]; kvb = 5
                cdst = qkc_o[:, 0:256]; ckey = "qkco"
            ssq = st1[:, n % 4, 8:8 + nq]; rs = st1[:, n % 4, 20:20 + nq]
            rk = ("rs", n % 4)
            for h in range(nq):
                A(lambda e, h=h: e.activation(out=junk[:, 0:128], in_=qkps[:, h * 128:(h + 1) * 128], func=AF.Square, accum_out=ssq[:, h:h + 1]), pkeys, [rk])
            A(lambda e: e.activation(out=rs, in_=ssq, func=AF.Ln, scale=1.0 / 128, bias=epsb), [rk, "eps"], [rk])
            A(lambda e: e.activation(out=rs, in_=rs, func=AF.Exp, scale=-0.5), [rk], [rk])
            A(lambda e: e.activation(out=cdst, in_=qkps, func=AF.Copy), pkeys, [ckey])
            A(lambda e: e.activation(out=VX[:, t, :, 0:128], in_=PS[:, kvb, 256:512].rearrange("p (g d) -> p g d", g=2), func=AF.Copy), [("ps", kvb)], [("vx", t)])

        def st_S5(n):
            t, own, nq = tinfo(n)
            if t == 32:
                return
            rs = st1[:, n % 4, 20:20 + nq]; rk = ("rs", n % 4)
            if own:
                src = qkc; ckey = "qkc"; qs = qn[(n // 2) % 2]
            else:
                src = qkc_o[:, 0:256]; ckey = "qkco"; qs = qn_o[(n // 2) % 2]
            qkey = ("qn", (n // 2) % 2, own)
            src3 = src.rearrange("p (h i two) -> p h two i", i=64, two=2)
            for h in range(nq):
                gp = gq_p if (own and h < 8) else gk_p
                dst = qs[:, h * 128:(h + 1) * 128].rearrange("p (two i) -> p two i", two=2)
                V(lambda e, h=h, gp=gp, dst=dst: e.scalar_tensor_tensor(out=dst, in0=src3[:, h, :, :], scalar=rs[:, h:h + 1], in1=gp.rearrange("p (two i) -> p two i", two=2), op0=ALU.mult, op1=ALU.mult), [ckey, rk, "gqp", "gkp"], [qkey])

        def rope_views(n):
            t, own, nq = tinfo(n)
            qs = qn[(n // 2) % 2] if own else qn_o[(n // 2) % 2]
            q3 = qs[:, 0:nq * 128].rearrange("p (h d) -> p h d", d=128)
            tt = [tmp4[i][:, 0:nq * 64].rearrange("p (h i) -> p h i", i=64) for i in range(4)]
            return t, own, nq, q3, tt

        def st_S6(n):
            t, own, nq, q3, tt = rope_views(n)
            if t == 32:
                return
            s3 = n % 3
            qkey = ("qn", (n // 2) % 2, own)
            x0 = q3[:, :, 0:64]; x1 = q3[:, :, 64:128]
            cosb = cst_t[s3][:, 0:64].unsqueeze(1).to_broadcast([128, nq, 64])
            sinb = cst_t[s3][:, 64:128].unsqueeze(1).to_broadcast([128, nq, 64])
            ck = f"cs{s3}"
            V(lambda e: e.tensor_tensor(out=tt[0], in0=x0, in1=cosb, op=ALU.mult), [qkey, ck], ["t0"])
            G(lambda e: e.tensor_tensor(out=tt[1], in0=x1, in1=sinb, op=ALU.mult), [qkey, ck], ["t1"])
            G(lambda e: e.tensor_tensor(out=tt[2], in0=x0, in1=sinb, op=ALU.mult), [qkey, ck], ["t2"])
            V(lambda e: e.tensor_tensor(out=tt[3], in0=x1, in1=cosb, op=ALU.mult), [qkey, ck], ["t3"])

        def st_S7(n):
            t, own, nq, q3, tt = rope_views(n)
            if t == 32:
                return
            h0 = 10 - nq
            qrs = qr[n % 2]; qk_ = ("qr", n % 2)
            r3 = qrs[:, h0 * 128:1280].rearrange("p (h d) -> p h d", d=128)
            V(lambda e: e.tensor_tensor(out=r3[:, :, 0:64], in0=tt[0], in1=tt[1], op=ALU.subtract), ["t0", "t1"], [(qk_, 0)])
            G(lambda e: e.tensor_tensor(out=r3[:, :, 64:128], in0=tt[2], in1=tt[3], op=ALU.add), ["t2", "t3"], [(qk_, 1)])

        def st_S8(n):
            t, own, nq = tinfo(n)
            if t == 32:
                return
            qrs = qr[n % 2]; qk_ = ("qr", n % 2)
            pq = psb(6).rearrange("p (h q) -> p h q", h=8)
            pk = psb(7).rearrange("p (h q) -> p h q", h=8)
            if own:
                for h in range(8):
                    T(lambda e, h=h: e.transpose(out=pq[:, h, :], in_=qrs[:, h * 128:(h + 1) * 128], identity=id_b), [(qk_, 0), (qk_, 1)], [("ps", 6)])
            for g in range(2):
                T(lambda e, g=g: e.transpose(out=pk[:, g, :], in_=qrs[:, (8 + g) * 128:(9 + g) * 128], identity=id_b), [(qk_, 0), (qk_, 1)], [("ps", 7)])

        def st_S9(n):
            t, own, nq = tinfo(n)
            if t == 32:
                return
            pq = psb(6).rearrange("p (h q) -> p h q", h=8)
            pk = psb(7).rearrange("p (h q) -> p h q", h=8)
            if own:
                A(lambda e: e.activation(out=QT[:, t, :, :], in_=pq, func=AF.Copy), [("ps", 6)], [("QT", t)])
            V(lambda e: e.tensor_copy(out=KT[:, :, t * 128:(t + 1) * 128], in_=pk[:, 0:2, :]), [("ps", 7)], [("KT", t)])

        stages = [st_S0, st_S1, st_S2, st_S3, st_S4, st_S5, st_S6, st_S7, st_S8, st_S9]
        load_x(0); load_x(1)
        for kc in range(8):
            s = kc % 2
            D("sync", f"stg{s}", lambda e, kc=kc, s=s: e.dma_start(out=stg[s], in_=w_in[kc * 128:(kc + 1) * 128, 3072:4608]), w=[f"stg{s}"])
            if kc % 2 == 0:
                G(lambda e, kc=kc, s=s: e.tensor_copy(out=wqkv[:, kc, :], in_=stg[s]), [f"stg{s}"], [f"wqkv{kc}"])
            else:
                V(lambda e, kc=kc, s=s: e.tensor_copy(out=wqkv[:, kc, :], in_=stg[s]), [f"stg{s}"], [f"wqkv{kc}"])
        for it in range(NTL + 9):
            for off in range(9, -1, -1):
                n = it - off
                if 0 <= n < NTL:
                    stages[off](n)

        tap("uT", uT, [("uT", i) for i in range(17)])
        tap("QT", QT, [("QT", i) for i in range(16)])
        tap("KT", KT, [("KT", i) for i in range(32)])
        tap("VX", VX, [("vx", i) for i in range(32)] + ["vx1"])
        S.barrier()
        if stop <= 1:
            S.emit(); return nc

        zt = region(Wreg, 2048, BF16)
        G(lambda e: e.memset(zt, 0.0), [], ["zt"])
        for i in range(16):
            D("gpsimd", "xsz", lambda e, i=i: e.dma_start(out=XS[i * 1024:(i + 1) * 1024, :].rearrange("(a p) n -> p a n", p=128), in_=zt.unsqueeze(1).to_broadcast([128, 8, 1024])), r=["zt"], w=["XS"])

        o = [OFF_B]
        PTs = [salloc(1024, BF16) for _ in range(4)]
        Otok = [salloc(2048, BF16).rearrange("p (h d) -> p h d", h=8) for _ in range(2)]
        rd = salloc(64, F32)
        SB = [0, 1, 6]
        seq = [(qt, g, kt) for qt in range(16) for g in range(2) for kt in range(32)]
        nbias = negc[:, 3:4]

        def issue_S(i):
            qt, g, kt = seq[i]
            b = SB[i % 3]
            T(lambda e: e.matmul(PS[:, b, :].rearrange("p (h q) -> p h q", h=4), lhsT=KT[:, g, kt * 128:(kt + 1) * 128], rhs=QT[:, qt, 4 * g:4 * g + 4, :], start=True, stop=True), [], [("ps", b)])

        def oacc(qt, g, h):
            sset = (qt * 2 + g) % 2
            bank = 2 + 2 * sset + h // 2
            return PS[:, bank, :].rearrange("p (j n) -> p j n", j=2), bank, h % 2

        issue_S(0); issue_S(1)

        def p2_step(i, qt, g, kt):
            if i + 2 < len(seq):
                issue_S(i + 2)
            b = SB[i % 3]; ps_ = PTs[i % 4]; pk_ = ("PT", i % 4)
            A(lambda e: e.activation(out=ps_, in_=PS[:, b, :], func=AF.Exp, bias=nbias, scale=1.0), [("ps", b), "negc"], [pk_])
            for h in range(4):
                ov, bank, j = oacc(qt, g, h)
                T(lambda e, h=h, ov=ov, j=j: e.matmul(ov[:, j, 0:129], lhsT=ps_[:, h * 128:(h + 1) * 128], rhs=VX[:, kt, g, 0:129], start=(kt == 0 and j == 0), stop=(kt == 31), skip_group_check=True), [pk_], [("ps", bank)])
            if kt == 31:
                os_ = Otok[qt % 2]; ok_ = ("Otok", qt % 2)
                for hb in range(2):
                    ov, bank, _ = oacc(qt, g, hb * 2)
                    V(lambda e, ov=ov, hb=hb: e.reciprocal(out=rd[:, 2 * hb:2 * hb + 2], in_=ov[:, :, 128]), [("ps", bank)], [("rd", hb)])
                    V(lambda e, ov=ov, hb=hb: e.tensor_tensor(out=os_[:, 4 * g + 2 * hb:4 * g + 2 * hb + 2, :], in0=ov[:, :, 0:128], in1=rd[:, 2 * hb:2 * hb + 2].unsqueeze(2).to_broadcast([128, 2, 128]), op=ALU.mult), [("ps", bank), ("rd", hb)], [ok_])
                if g == 1:
                    pt = psb(7).rearrange("p (c q) -> p c q", c=8)
                    for c in range(8):
                        T(lambda e, c=c: e.transpose(out=pt[:, c, :], in_=os_[:, c, :], identity=id_b), [ok_], [("ps", 7)])
                    V(lambda e: e.tensor_copy(out=OT[:, :, qt * 128:(qt + 1) * 128], in_=pt), [("ps", 7)], [("OT", qt)])

        for i_, (qt_, g_, kt_) in enumerate(seq):
            p2_step(i_, qt_, g_, kt_)
        tap("OT", OT, [("OT", i) for i in range(16)])
        S.core_barrier()
        if stop <= 2:
            S.emit(); return nc

        o = [OFF_B]
        ybuf = salloc(2052 * 4, F32)
        zb = [salloc(2048, F32) for _ in range(2)]
        ccs = [salloc(2048, F32) for _ in range(2)]
        o = [OFF_KT]
        cstg3 = [salloc(4096, F32, lim=OFF_D).rearrange("p (c n) -> p c n", c=8) for _ in range(3)]
        wcb3 = [salloc(2048, BF16, lim=OFF_D).rearrange("p (c n) -> p c n", c=8) for _ in range(6)]
        nld = [0]

        def load_chunk(col0):
            s = nld[0] % 3; sb_ = nld[0] % 6; nld[0] += 1
            D("sync", f"cstg{s}", lambda e: e.dma_start(out=cstg3[s], in_=w_in[:, col0:col0 + 128].rearrange("(c p) n -> p c n", p=128)), w=[f"cstg{s}"])
            A(lambda e: e.activation(out=wcb3[sb_], in_=cstg3[s], func=AF.Copy), [f"cstg{s}"], [f"wcb{sb_}"])
            return wcb3[sb_], f"wcb{sb_}"

        def mm8(bank, wch, wkey, rhs_fn, rkeys, n=512):
            for kc in range(8):
                T(lambda e, kc=kc: e.matmul(PS[:, bank, 0:n], lhsT=wch[:, kc, :], rhs=rhs_fn(kc), start=(kc == 0), stop=(kc == 7)), [wkey] + rkeys, [("ps", bank)])

        ukeys_all = [("uT", i) for i in range(17)]
        chunks = [load_chunk(1024 + 0), load_chunk(2048 + 0), load_chunk(0)]
        bk = [0]

        def nb():
            b = bk[0] % 8; bk[0] += 1
            return b
        for j in range(8):
            wcc, wcx, wcb_ = chunks
            if j + 1 < 8:
                chunks = [load_chunk(1024 + (j + 1) * 128), load_chunk(2048 + (j + 1) * 128), load_chunk((j + 1) * 128)]
            for nt in range(5):
                if nt < 4:
                    rf = lambda kc, nt=nt: uT[:, kc, nt * 512:(nt + 1) * 512]; n = 512; ycol = 1 + nt * 512
                else:
                    rf = lambda kc: uT[:, kc, 2048:2050]; n = 2
                b1 = nb(); b2 = nb()
                mm8(b1, wcc[0], wcc[1], rf, [], n)
                mm8(b2, wcx[0], wcx[1], rf, [], n)
                cs_ = ccs[nt % 2]; ck = ("ccs", nt % 2)
                A(lambda e, b1=b1, n=n, cs_=cs_: e.activation(out=cs_[:, 0:n], in_=PS[:, b1, 0:n], func=AF.Copy), [("ps", b1)], [ck])
                if nt < 4:
                    V(lambda e, b2=b2, cs_=cs_, ycol=ycol: e.tensor_tensor(out=ybuf[:, ycol:ycol + 512], in0=PS[:, b2, :], in1=cs_, op=ALU.mult), [("ps", b2), ck], [("y", nt)])
                else:
                    V(lambda e, b2=b2, cs_=cs_: e.tensor_tensor(out=ybuf[:, 0:1], in0=PS[:, b2, 0:1], in1=cs_[:, 0:1], op=ALU.mult), [("ps", b2), ck], [("y", 4)])
                    V(lambda e, b2=b2, cs_=cs_: e.tensor_tensor(out=ybuf[:, 2049:2050], in0=PS[:, b2, 1:2], in1=cs_[:, 1:2], op=ALU.mult), [("ps", b2), ck], [("y", 5)])
            ykeys = [("y", i) for i in range(6)]
            for nt in range(4):
                z = zb[nt % 2]; zk = ("z", nt % 2)
                c0 = nt * 512
                V(lambda e, z=z, c0=c0, j=j: e.tensor_scalar(out=z, in0=ybuf[:, c0:c0 + 512], scalar1=cw[:, 0, j:j + 1], scalar2=None, op0=ALU.mult), ykeys + ["cw"], [zk])
                V(lambda e, z=z, c0=c0, j=j: e.scalar_tensor_tensor(out=z, in0=ybuf[:, c0 + 1:c0 + 513], scalar=cw[:, 1, j:j + 1], in1=z, op0=ALU.mult, op1=ALU.add), ykeys + [zk], [zk])
                V(lambda e, z=z, c0=c0, j=j: e.scalar_tensor_tensor(out=z, in0=ybuf[:, c0 + 2:c0 + 514], scalar=cw[:, 2, j:j + 1], in1=z, op0=ALU.mult, op1=ALU.add), ykeys + [zk], [zk])
                b3 = nb()
                mm8(b3, wcb_[0], wcb_[1], lambda kc, nt=nt: uT[:, kc, nt * 512:(nt + 1) * 512], [])
                V(lambda e, b3=b3, z=z, nt=nt, j=j: e.tensor_tensor(out=mT[:, j, nt * 512:(nt + 1) * 512], in0=PS[:, b3, :], in1=z, op=ALU.mult), [("ps", b3), zk], [("mT", j, nt)])
        tap("mT", mT, [("mT", j, nt) for j in range(8) for nt in range(4)])
        S.barrier()
        if stop <= 3:
            S.emit(); return nc

        wco = region(Wreg, 16384, BF16).rearrange("p (c n) -> p c n", c=8)
        wao = region(OFF_KT, 16384, BF16).rearrange("p (c n) -> p c n", c=8)
        o = [OFF_VX]
        wstg4 = [salloc(4096, F32, lim=OFF_D) for _ in range(2)]
        sg = [salloc(2048, F32, lim=OFF_D) for _ in range(2)]
        tm = [salloc(2048, F32, lim=OFF_D) for _ in range(2)]
        o = [Wreg + 16384]
        cstg4 = [salloc(4096, F32, lim=P0).rearrange("p (c n) -> p c n", c=8) for _ in range(2)]
        wcb4 = [salloc(2048, BF16, lim=P0).rearrange("p (c n) -> p c n", c=8) for _ in range(2)]
        nld[0] = 0

        def load_chunk2(col0):
            s = nld[0] % 2; nld[0] += 1
            D("sync", f"cstg{s}", lambda e: e.dma_start(out=cstg4[s], in_=w_in[:, col0:col0 + 128].rearrange("(c p) n -> p c n", p=128)), w=[f"cstg{s}"])
            G(lambda e: e.tensor_copy(out=wcb4[s], in_=cstg4[s]), [f"cstg{s}"], [f"wcb{s}"])
            return wcb4[s], f"wcb{s}"

        def load_big(wdst, wsrc, name, ceng, wstg):
            for kc in range(8):
                s = kc % 2
                D("sync", f"wstg{s}", lambda e, kc=kc, s=s: e.dma_start(out=wstg[s], in_=wsrc[kc * 128:(kc + 1) * 128, :]), w=[f"wstg{s}"])
                S.add(ceng, lambda e, kc=kc, s=s: e.tensor_copy(out=wdst[:, kc, :], in_=wstg[s]), [f"wstg{s}"], [(name, kc)])
            return [(name, kc) for kc in range(8)]
        kco = load_big(wco, w_conv_out, "wco", "vector", wstg4)
        kao = load_big(wao, w_attn_out, "wao", "gpsimd", wstg4)
        gch = [load_chunk2(4608), load_chunk2(5632)]
        def p4_step(oc, nt, wgc, wga):
            if True:
                tk = slice(nt * 512, (nt + 1) * 512)
                byc, bgc, bya, bga = nb(), nb(), nb(), nb()
                for kc in range(8):
                    T(lambda e, kc=kc, byc=byc: e.matmul(PS[:, byc, :], lhsT=wco[:, kc, oc * 128:(oc + 1) * 128], rhs=mT[:, kc, tk], start=(kc == 0), stop=(kc == 7)), kco, [("ps", byc)])
                mm8(bgc, wgc[0], wgc[1], lambda kc: uT[:, kc, tk], [])
                for kc in range(8):
                    T(lambda e, kc=kc, bya=bya: e.matmul(PS[:, bya, :], lhsT=wao[:, kc, oc * 128:(oc + 1) * 128], rhs=OT[:, kc, tk], start=(kc == 0), stop=(kc == 7)), kao, [("ps", bya)])
                mm8(bga, wga[0], wga[1], lambda kc: uT[:, kc, tk], [])
                A(lambda e, bgc=bgc: e.activation(out=sg[0], in_=PS[:, bgc, :], func=AF.Sigmoid), [("ps", bgc)], ["sg0"])
                A(lambda e, bga=bga: e.activation(out=sg[1], in_=PS[:, bga, :], func=AF.Sigmoid), [("ps", bga)], ["sg1"])
                V(lambda e, byc=byc: e.tensor_tensor(out=tm[0], in0=PS[:, byc, :], in1=sg[0], op=ALU.mult), [("ps", byc), "sg0"], ["tm0"])
                V(lambda e, bya=bya: e.tensor_tensor(out=tm[1], in0=PS[:, bya, :], in1=sg[1], op=ALU.mult), [("ps", bya), "sg1"], ["tm1"])
                G(lambda e: e.tensor_tensor(out=Bbuf[:, oc, tk], in0=tm[0], in1=tm[1], op=ALU.add), ["tm0", "tm1"], [("mg", oc, nt)])

        for oc in range(8):
            wgc, wga = gch
            for nt in range(4):
                p4_step(oc, nt, wgc, wga)
            if oc + 1 < 8:
                gch = [load_chunk2(4608 + (oc + 1) * 128), load_chunk2(5632 + (oc + 1) * 128)]
        tap("mg", Bbuf, [("mg", oc, nt) for oc in range(8) for nt in range(4)])
        S.barrier()
        if stop <= 4:
            S.emit(); return nc

        wo = region(Wreg, 16384, BF16).rearrange("p (c n) -> p c n", c=8)
        o = [OFF_A]
        wstg5 = [salloc(4096, F32, lim=OFF_B) for _ in range(2)]
        xt5 = [salloc(4096, F32, lim=OFF_B) for _ in range(3)]
        x1t = [salloc(4096, F32, lim=OFF_B) for _ in range(2)]
        junk5 = salloc(4096, F32, lim=OFF_B)
        st5 = salloc(16 * 4 * 4, F32, lim=OFF_B).rearrange("p (t k) -> p t k", t=16)
        eps5 = salloc(4, F32, lim=OFF_B)
        o = [OFF_QT]
        sq2 = [salloc(4096, F32, lim=OFF_D) for _ in range(2)]
        u2f = [salloc(4096, F32, lim=OFF_D) for _ in range(2)]
        u2T = [salloc(4096, F32, lim=OFF_D).rearrange("p (c q) -> p c q", c=8) for _ in range(2)]
        LG = salloc(16 * 72 * 4, F32, lim=OFF_D).rearrange("p (t n) -> p t n", t=16)
        MA = [salloc(16 * 64 * 4, F32, lim=OFF_D).rearrange("p (t e) -> p t e", t=16) for _ in range(2)]
        Mf = salloc(16 * 64 * 4, F32, lim=OFF_D).rearrange("p (t e) -> p t e", t=16)
        Mcum = salloc(16 * 64 * 4, F32, lim=OFF_D).rearrange("p (t e) -> p t e", t=16)
        RkA = salloc(16 * 64 * 4, F32, lim=OFF_D).rearrange("p (t e) -> p t e", t=16)
        T64 = salloc(16 * 64 * 4, F32, lim=OFF_D).rearrange("p (t e) -> p t e", t=16)
        rb = salloc(16 * 64 * 4, F32, lim=OFF_D).rearrange("p (k t x) -> p k t x", k=8, t=16)
        rs_ = salloc(16 * 16 * 4, F32, lim=OFF_D).rearrange("p (k t) -> p k t", k=16)
        U2B = region(OFF_D, 32768, BF16).rearrange("p (t n) -> p t n", t=16)
        kwo = load_big(wo, w_o, "wo", "vector", wstg5)
        V(lambda e: e.memset(eps5, EPS), [], ["eps5"])

        def ld_x5(t):
            if t < 16:
                D("sync", f"x5{t % 3}", lambda e: e.dma_start(out=xt5[t % 3], in_=xa[t * 128:(t + 1) * 128, :]), w=[("x5", t % 3)])

        def q0(t):
            pb = 2 * (t % 2)
            for half in range(2):
                for kc in range(8):
                    T(lambda e, kc=kc, half=half: e.matmul(PS[:, pb + half, :], lhsT=Bbuf[:, kc, t * 128:(t + 1) * 128], rhs=wo[:, kc, half * 512:(half + 1) * 512], start=(kc == 0), stop=(kc == 7)), kwo, [("ps", pb + half)])

        def q1(t):
            pb = 2 * (t % 2); s = t % 2
            V(lambda e: e.tensor_tensor(out=x1t[s].rearrange("p (b n) -> p b n", b=2), in0=PS[:, pb:pb + 2, :], in1=xt5[t % 3].rearrange("p (b n) -> p b n", b=2), op=ALU.add), [("ps", pb), ("ps", pb + 1), ("x5", t % 3)], [("x1", s)])
            D("sync", f"x1o{s}", lambda e: e.dma_start(out=out[t * 128:(t + 1) * 128, :], in_=x1t[s]), r=[("x1", s)], w=[("out", t)])
            ld_x5(t + 2)

        def q2(t):
            s = t % 2
            ss = st5[:, t, 0:1]; rstd = st5[:, t, 1:2]; sk = ("st5", t)
            A(lambda e: e.activation(out=junk5, in_=x1t[s], func=AF.Square, accum_out=ss), [("x1", s)], [sk])
            A(lambda e: e.activation(out=rstd, in_=ss, func=AF.Ln, scale=1.0 / 1024, bias=eps5), [sk, "eps5"], [sk])
            A(lambda e: e.activation(out=rstd, in_=rstd, func=AF.Exp, scale=-0.5), [sk], [sk])
            A(lambda e: e.activation(out=sq2[s], in_=x1t[s], func=AF.Copy, scale=rstd), [("x1", s), sk], [("sq2", s)])

        def q3(t):
            s = t % 2
            V(lambda e: e.tensor_tensor(out=u2f[s], in0=sq2[s], in1=g2_bc, op=ALU.mult), [("sq2", s), "g2"], [("u2f", s)])

        def q4(t):
            s = t % 2
            A(lambda e: e.activation(out=U2B[:, t, :], in_=u2f[s], func=AF.Copy), [("u2f", s)], [("u2b", t)])
            for c in range(8):
                T(lambda e, c=c: e.transpose(out=PS[:, 4 + c // 4, (c % 4) * 128:(c % 4 + 1) * 128], in_=u2f[s][:, c * 128:(c + 1) * 128], identity=id_f), [("u2f", s), "cst"], [("ps", 4 + c // 4)])

        def q5(t):
            s = t % 2
            V(lambda e: e.tensor_copy(out=u2T[s], in_=PS[:, 4:6, :].rearrange("p b (c q) -> p (b c) q", c=4)), [("ps", 4), ("ps", 5)], [("u2T", s)])

        def q6(t):
            s = t % 2; c6 = 128 * (t % 2)
            for kc in range(8):
                T(lambda e, kc=kc: e.matmul(PS[:, 6, c6:c6 + 72], lhsT=u2T[s][:, kc, :], rhs=wgr[:, kc, :], start=(kc == 0), stop=(kc == 7)), [("u2T", s), "wgr"], [("ps", 6)])

        def q7(t):
            c6 = 128 * (t % 2)
            V(lambda e: e.tensor_copy(out=LG[:, t, :], in_=PS[:, 6, c6:c6 + 72]), [("ps", 6)], [("lg", t)])

        tri_f = cst[:, 128:256]; ones_f = cst[:, 256:384]
        NBT = 4

        def route_batch(bi):
            t0 = bi * NBT; t1 = t0 + NBT; nb_ = NBT
            ts = slice(t0, t1)
            K = lambda name: (name, bi)
            lgk = [("lg", t) for t in range(t0, t1)]
            lgg = LG[:, ts, 0:8]
            lge = LG[:, ts, 8:72].rearrange("p t (g x) -> p t g x", g=8)
            gm, nv1, v2, se, pg, dd, ed, w1_ = (rs_[:, i, ts] for i in range(8))
            ohg, eg, sel, oh1, sel2, oh2 = (rb[:, i, ts, :] for i in range(6))

            def bc3(a):
                return a.unsqueeze(2).to_broadcast([128, nb_, 8])
            V(lambda e: e.tensor_reduce(out=gm, in_=lgg, axis=AX.X, op=ALU.max), lgk, [K("gm")])
            V(lambda e: e.tensor_tensor(out=ohg, in0=lgg, in1=bc3(gm), op=ALU.is_equal), lgk + [K("gm")], [K("ohg")])
            V(lambda e: e.tensor_tensor(out=eg, in0=lgg, in1=bc3(gm), op=ALU.subtract), lgk + [K("gm")], [K("eg")])
            A(lambda e: e.activation(out=eg, in_=eg, func=AF.Exp), [K("eg")], [K("eg")])
            V(lambda e: e.tensor_reduce(out=se, in_=eg, axis=AX.X, op=ALU.add), [K("eg")], [K("se")])
            V(lambda e: e.reciprocal(out=pg, in_=se), [K("se")], [K("pg")])
            T64b = T64[:, ts, :]
            V(lambda e: e.tensor_tensor(out=T64b.rearrange("p t (g x) -> p t g x", g=8), in0=lge, in1=ohg.unsqueeze(3).to_broadcast([128, nb_, 8, 8]), op=ALU.mult), lgk + [K("ohg")], [K("t64")])
            V(lambda e: e.tensor_reduce(out=sel, in_=T64b.rearrange("p t (g x) -> p t x g", g=8), axis=AX.X, op=ALU.add), [K("t64")], [K("sel")])
            V(lambda e: e.tensor_reduce(out=nv1, in_=sel, axis=AX.X, op=ALU.max), [K("sel")], [K("v1")])
            V(lambda e: e.tensor_tensor(out=oh1, in0=sel, in1=bc3(nv1), op=ALU.is_equal), [K("sel"), K("v1")], [K("oh1")])
            V(lambda e: e.scalar_tensor_tensor(out=sel2, in0=oh1, scalar=-1e30, in1=sel, op0=ALU.mult, op1=ALU.add), [K("oh1"), K("sel")], [K("sel2")])
            V(lambda e: e.tensor_reduce(out=v2, in_=sel2, axis=AX.X, op=ALU.max), [K("sel2")], [K("v2")])
            V(lambda e: e.tensor_tensor(out=oh2, in0=sel2, in1=bc3(v2), op=ALU.is_equal), [K("sel2"), K("v2")], [K("oh2")])
            V(lambda e: e.tensor_tensor(out=dd, in0=v2, in1=nv1, op=ALU.subtract), [K("v1"), K("v2")], [K("dd")])
            A(lambda e: e.activation(out=ed, in_=dd, func=AF.Exp), [K("dd")], [K("ed")])
            V(lambda e: e.tensor_scalar(out=w1_, in0=ed, scalar1=1.0, scalar2=None, op0=ALU.add), [K("ed")], [K("w1_")])
            V(lambda e: e.reciprocal(out=w1_, in_=w1_), [K("w1_")], [K("w1_")])
            V(lambda e: e.tensor_tensor(out=Wk[:, 0, ts], in0=w1_, in1=pg, op=ALU.mult), [K("w1_"), K("pg")], [K("Wk0")])
            V(lambda e: e.tensor_tensor(out=Wk[:, 1, ts], in0=Wk[:, 0, ts], in1=ed, op=ALU.mult), [K("Wk0"), K("ed")], [K("Wk1")])
            for k, ohk in enumerate((oh1, oh2)):
                V(lambda e, k=k, ohk=ohk: e.tensor_tensor(out=MA[k][:, ts, :].rearrange("p t (g x) -> p t g x", g=8), in0=ohg.unsqueeze(3).to_broadcast([128, nb_, 8, 8]), in1=ohk.unsqueeze(2).to_broadcast([128, nb_, 8, 8]), op=ALU.mult), [K("ohg"), K("oh1"), K("oh2")], [("MA", k, bi)])
            V(lambda e: e.tensor_tensor(out=Mb[:, ts, :], in0=MA[0][:, ts, :], in1=MA[1][:, ts, :], op=ALU.add), [("MA", 0, bi), ("MA", 1, bi)], [("Mf", bi)])
            mfk = [("Mf", j) for j in range(bi + 1)]
            first = True
            for t in range(t0, t1):
                c0 = (t - t0) * 64
                T(lambda e, t=t, c0=c0, first=first: e.matmul(PS[:, 7, c0:c0 + 64], lhsT=tri_b, rhs=Mb[:, t, :], start=first, stop=(t == 0), skip_group_check=True), mfk + ["cstb"], [("ps", 7)])
                first = False
                for i in range(t):
                    T(lambda e, i=i, c0=c0, t=t: e.matmul(PS[:, 7, c0:c0 + 64], lhsT=ones_b, rhs=Mb[:, i, :], start=False, stop=(i == t - 1), skip_group_check=True), mfk + ["cstb"], [("ps", 7)])
            RkB = RkA[:, ts, :]
            V(lambda e: e.tensor_copy(out=RkB.rearrange("p t e -> p (t e)"), in_=PS[:, 7, 0:nb_ * 64]), [("ps", 7)], [K("Rk")])
            rsel, bsel, ov = (rs_[:, 8 + i, ts] for i in range(3))
            for k in range(2):
                V(lambda e, k=k: e.tensor_tensor(out=T64b, in0=RkB, in1=MA[k][:, ts, :], op=ALU.mult), [K("Rk"), ("MA", k, bi)], [K("t64")])
                V(lambda e: e.tensor_reduce(out=rsel, in_=T64b, axis=AX.X, op=ALU.add), [K("t64")], [K("rsel")])
                V(lambda e, k=k: e.tensor_tensor(out=T64b, in0=MA[k][:, ts, :], in1=ebase.unsqueeze(1).to_broadcast([128, nb_, 64]), op=ALU.mult), [("MA", k, bi), "cst", K("rsel")], [K("t64")])
                V(lambda e: e.tensor_reduce(out=bsel, in_=T64b, axis=AX.X, op=ALU.add), [K("t64")], [K("bsel")])
                V(lambda e: e.tensor_scalar(out=ov, in0=rsel, scalar1=float(CAP) - 0.5, scalar2=100000.0, op0=ALU.is_ge, op1=ALU.mult), [K("rsel")], [K("ov")])
                V(lambda e: e.tensor_tensor(out=bsel, in0=bsel, in1=rsel, op=ALU.add), [K("bsel"), K("rsel")], [K("bsel")])
                V(lambda e: e.tensor_tensor(out=bsel, in0=bsel, in1=ov, op=ALU.add), [K("bsel"), K("ov")], [K("bsel")])
                V(lambda e, k=k: e.tensor_copy(out=idx[:, k, ts], in_=bsel), [K("bsel")], [("idx", k, bi)])
            for t in range(t0, t1):
                for k in range(2):
                    D("gpsimd", "scat", lambda e, k=k, t=t: e.indirect_dma_start(out=XS, out_offset=bass.IndirectOffsetOnAxis(ap=idx[:, k, t:t + 1], axis=0), in_=U2B[:, t, :], in_offset=None, bounds_check=bnd['r'], oob_is_err=False), r=[("idx", k, bi), ("u2b", t), "XS"], w=["XSs"])

        qst = [q0, q1, q2, q3, q4, q5, q6, q7]
        ld_x5(0); ld_x5(1)
        for it in range(16 + 7):
            for off in range(7, -1, -1):
                t_ = it - off
                if 0 <= t_ < 16:
                    qst[off](t_)
            tdone = it - 7
            if tdone >= 0 and (tdone + 1) % NBT == 0:
                route_batch(tdone // NBT)
        tap("idx", idx, [("idx", k, bi) for k in range(2) for bi in range(4)])
        tap("Wk", Wk, [(n_, bi) for n_ in ("Wk0", "Wk1") for bi in range(4)])
        if stop <= 5:
            S.core_barrier(); S.emit(); return nc

        o = [0]
        LIM = P0
        wst = [salloc(16384, F32, lim=LIM) for _ in range(4)]
        wb1 = [salloc(8192, BF16, lim=LIM).rearrange("p (c n) -> p c n", c=8) for _ in range(2)]
        wb3 = [salloc(8192, BF16, lim=LIM).rearrange("p (c n) -> p c n", c=8) for _ in range(2)]
        wb2 = [salloc(8192, BF16, lim=LIM).rearrange("p (c n) -> p c n", c=4) for _ in range(2)]
        xs_sb = [salloc(2048, BF16, lim=LIM) for _ in range(3)]
        xsT = [salloc(2048, BF16, lim=LIM).rearrange("p (c q) -> p c q", c=8) for _ in range(2)]
        sh = [salloc(2048, F32, lim=LIM) for _ in range(2)]
        act = [salloc(1024, BF16, lim=LIM) for _ in range(2)]
        aT = [salloc(1024, BF16, lim=LIM).rearrange("p (c q) -> p c q", c=4) for _ in range(2)]
        ysb = [salloc(2048, BF16, lim=LIM) for _ in range(2)]
        nst = [0]

        def load_w(j):
            for (wsrc, wdst, ceng, name) in ((w1, wb1, "vector", "wb1"), (w3, wb3, "scalar", "wb3"), (w2, wb2, "vector", "wb2")):
                s = nst[0] % 4; nst[0] += 1
                d = wdst[j % 2]
                cc = 4 if name == "wb2" else 8
                src = wsrc[j].rearrange("(c p) n -> p c n", p=128)
                stv = wst[s].rearrange("p (c n) -> p c n", c=cc)
                D("sync", f"wst{s}", lambda e, stv=stv, src=src: e.dma_start(out=stv, in_=src), w=[f"wst{s}"])
                if ceng == "scalar":
                    S.add(ceng, lambda e, d=d, stv=stv: e.activation(out=d, in_=stv, func=AF.Copy), [f"wst{s}"], [(name, j % 2)])
                else:
                    S.add(ceng, lambda e, d=d, stv=stv: e.tensor_copy(out=d, in_=stv), [f"wst{s}"], [(name, j % 2)])

        def load_xs(k):
            if k >= 2 * NEH:
                return
            s = k % 3
            D("gpsimd", f"xs{s}", lambda e: e.indirect_dma_start(out=xs_sb[s], out_offset=None, in_=XS, in_offset=bass.IndirectOffsetOnAxis(ap=xblk[:, k:k + 1], axis=0), bounds_check=bnd['r'], oob_is_err=False), r=["xblk"], w=[("xs", s)])

        def p6_block(k):
            j = k // 2; ws = j % 2; s = k % 2; s3 = k % 3
            if k % 2 == 0 and j + 1 < NEH:
                load_w(j + 1)
            load_xs(k + 2)
            pt = psb(0).rearrange("p (c q) -> p c q", c=8)
            for c in range(8):
                T(lambda e, c=c: e.transpose(out=pt[:, c, :], in_=xs_sb[s3][:, c * 128:(c + 1) * 128], identity=id_b), [("xs", s3)], [("ps", 0)])
            V(lambda e: e.tensor_copy(out=xsT[s], in_=pt), [("ps", 0)], [("xsT", s)])
            for kc in range(8):
                T(lambda e, kc=kc: e.matmul(PS[:, 1, :], lhsT=xsT[s][:, kc, :], rhs=wb1[ws][:, kc, :], start=(kc == 0), stop=(kc == 7)), [("xsT", s), ("wb1", ws)], [("ps", 1)])
            for kc in range(8):
                T(lambda e, kc=kc: e.matmul(PS[:, 2, :], lhsT=xsT[s][:, kc, :], rhs=wb3[ws][:, kc, :], start=(kc == 0), stop=(kc == 7)), [("xsT", s), ("wb3", ws)], [("ps", 2)])
            A(lambda e: e.activation(out=sh[s], in_=PS[:, 1, :], func=AF.Silu), [("ps", 1)], [("sh", s)])
            V(lambda e: e.tensor_tensor(out=act[s], in0=PS[:, 2, :], in1=sh[s], op=ALU.mult), [("ps", 2), ("sh", s)], [("act", s)])
            pa = psb(3).rearrange("p (c q) -> p c q", c=8)
            for c in range(4):
                T(lambda e, c=c: e.transpose(out=pa[:, c, :], in_=act[s][:, c * 128:(c + 1) * 128], identity=id_b), [("act", s)], [("ps", 3)])
            V(lambda e: e.tensor_copy(out=aT[s], in_=pa[:, 0:4, :]), [("ps", 3)], [("aT", s)])
            for half in range(2):
                for c in range(4):
                    T(lambda e, c=c, half=half: e.matmul(PS[:, 4 + half, :], lhsT=aT[s][:, c, :], rhs=wb2[ws][:, c, half * 512:(half + 1) * 512], start=(c == 0), stop=(c == 3)), [("aT", s), ("wb2", ws)], [("ps", 4 + half)])
            yk = ("ysb", s)
            A(lambda e: e.activation(out=ysb[s][:, 0:512], in_=PS[:, 4, :], func=AF.Copy), [("ps", 4)], [(yk, 0)])
            A(lambda e: e.activation(out=ysb[s][:, 512:1024], in_=PS[:, 5, :], func=AF.Copy), [("ps", 5)], [(yk, 1)])
            D("gpsimd", f"ys{s}", lambda e: e.indirect_dma_start(out=YS, out_offset=bass.IndirectOffsetOnAxis(ap=xblk[:, k:k + 1], axis=0), in_=ysb[s], in_offset=None, bounds_check=bnd['r'], oob_is_err=False), r=[(yk, 0), (yk, 1), "xblk"], w=["YS"])

        S.core_barrier(prefetch=lambda: load_w(0))
        load_xs(0); load_xs(1)
        for k2 in range(2 * NEH):
            p6_block(k2)
        if stop <= 6:
            S.core_barrier(); S.emit(); return nc

        o = [0]
        y0 = [salloc(2048, BF16, lim=LIM) for _ in range(3)]
        y1 = [salloc(2048, BF16, lim=LIM) for _ in range(3)]
        x1b = [salloc(4096, F32, lim=LIM) for _ in range(3)]

        def ld_x7(t):
            if t < 16:
                D("sync", f"x7{t % 3}", lambda e: e.dma_start(out=x1b[t % 3], in_=out[t * 128:(t + 1) * 128, :]), w=[("x7", t % 3)])

        def gat7(t):
            if t >= 16:
                return
            s3 = t % 3
            for k, yb in enumerate((y0, y1)):
                (V if k == 0 else G)(lambda e, yb=yb: e.memset(yb[s3], 0.0), [], [("y", k, s3)])
                D("gpsimd", f"gat{k}{s3}", lambda e, k=k, yb=yb: e.indirect_dma_start(out=yb[s3], out_offset=None, in_=YS, in_offset=bass.IndirectOffsetOnAxis(ap=idx[:, k, t:t + 1], axis=0), bounds_check=bnd['r'], oob_is_err=False), r=[("y", k, s3)], w=[("y", k, s3)])

        def p7_tile(t):
            s3 = t % 3
            V(lambda e: e.scalar_tensor_tensor(out=x1b[s3], in0=y0[s3], scalar=Wk[:, 0, t:t + 1], in1=x1b[s3], op0=ALU.mult, op1=ALU.add), [("y", 0, s3), ("x7", s3)], [("x7", s3)])
            V(lambda e: e.scalar_tensor_tensor(out=x1b[s3], in0=y1[s3], scalar=Wk[:, 1, t:t + 1], in1=x1b[s3], op0=ALU.mult, op1=ALU.add), [("y", 1, s3), ("x7", s3)], [("x7", s3)])
            D("sync", f"fo{s3}", lambda e: e.dma_start(out=out[t * 128:(t + 1) * 128, :], in_=x1b[s3]), r=[("x7", s3)])
            ld_x7(t + 3)
            gat7(t + 3)

        S.core_barrier(prefetch=lambda: [ld_x7(t_) for t_ in range(3)])
        for t_ in range(3):
            gat7(t_)
        for t_ in range(16):
            p7_tile(t_)
        S.emit()
    return nc


def _rope_tables():
    S = 4096; GW = 64
    rows = S // GW
    row = np.repeat(np.arange(rows, dtype=np.float32), GW)
    col = np.tile(np.arange(GW, dtype=np.float32), rows)
    inv = (np.float32(10000.0) ** (-np.arange(0, 64, 2, dtype=np.float32) / np.float32(64))).astype(np.float32)
    ang = np.concatenate([row[:, None] * inv, col[:, None] * inv], axis=-1).astype(np.float32)
    return np.concatenate([np.cos(ang), np.sin(ang)], axis=-1).astype(np.float32)


def _consts(sp):
    c = np.zeros((128, 512), np.float32)
    c[:, 0:128] = np.eye(128, dtype=np.float32)
    c[:, 128:256] = np.triu(np.ones((128, 128), np.float32), k=1)
    c[:, 256:384] = 1.0
    c[:, 384:448] = ((sp * NEXP + np.arange(64, dtype=np.float32)) * CAP)[None, :]
    return c


def _xblk(sp):
    t = np.zeros((128, 64), np.int32)
    for k in range(64):
        j, src = k // 2, k % 2
        t[:, k] = (src * NEXP + sp * (NEXP // 2) + j) * CAP + np.arange(128)
    return t


def _core_private(inputs, c, tab):
    b, hq = c // 2, c % 2
    x = np.asarray(inputs["x"], np.float32)
    t0 = hq * 2048
    xa = np.zeros((4224, 1024), np.float32)
    xa[0:2048] = x[b, t0:t0 + 2048]
    xa[2048:4096] = x[b, (1 - hq) * 2048:(1 - hq) * 2048 + 2048]
    if t0 > 0:
        xa[4096] = x[b, t0 - 1]
    if t0 + 2048 < 4096:
        xa[4097] = x[b, t0 + 2048]
    cs = np.concatenate([tab[t0:t0 + 2048], tab[(1 - hq) * 2048:(1 - hq) * 2048 + 2048]], axis=0)
    return {"xa": xa, "cs": np.ascontiguousarray(cs)}


def make_in_maps(inputs, cores):
    f = lambda k: np.ascontiguousarray(np.asarray(inputs[k], np.float32)[0])
    shared = {
        "norm1_g": f("norm1_g"), "norm2_g": f("norm2_g"), "w_in": f("w_in"), "conv_w": f("conv_w"),
        "q_norm_g": f("q_norm_g"), "k_norm_g": f("k_norm_g"), "w_conv_out": f("w_conv_out"),
        "w_attn_out": f("w_attn_out"), "w_o": f("w_o"),
        "w_gr": np.ascontiguousarray(np.concatenate([f("w_group"), f("w_router")], axis=1)),
    }
    w1, w3, w2 = f("w1"), f("w3"), f("w2")
    halves = [{"w1": np.ascontiguousarray(w1[sp * 32:(sp + 1) * 32]), "w3": np.ascontiguousarray(w3[sp * 32:(sp + 1) * 32]),
               "w2": np.ascontiguousarray(w2[sp * 32:(sp + 1) * 32]), "consts": _consts(sp), "xblk": _xblk(sp)} for sp in range(2)]
    tab = _rope_tables()
    maps = []
    for c in cores:
        m = dict(shared)
        m.update(halves[c % 2])
        m.update(_core_private(inputs, c, tab))
        maps.append(m)
    return maps


def kernel(**inputs):
    nc = build()
    in_maps = make_in_maps(inputs, list(range(8)))
    res = run_bass_kernel_spmd(nc, in_maps, core_ids=list(range(8)))
    outp = np.zeros((4, 4096, 1024), np.float32)
    for c in range(8):
        b, hq = c // 2, c % 2
        outp[b, hq * 2048:(hq + 1) * 2048] = res.results[c]["out"]
    return outp
```

```python
import contextlib
import numpy as np
import concourse.bass as bass
import concourse.mybir as mybir
from concourse.bass_utils import run_bass_kernel_spmd

F32 = mybir.dt.float32
BF16 = mybir.dt.bfloat16
I32 = mybir.dt.int32
U8 = mybir.dt.uint8
AF = mybir.ActivationFunctionType
ALU = mybir.AluOpType
AX = mybir.AxisListType

NT_OWN = 16
NT_ALL = 32
CAP = 128
NEXP = 64
EPS = 1e-6


class Op:
    __slots__ = ("eng", "fn", "idx", "waits", "inc", "count", "is_dma", "semkey")


class Sched:
    ENGS = ("sync", "scalar", "vector", "gpsimd", "tensor")

    def __init__(self, nc):
        self.nc = nc
        self.ops = {e: [] for e in self.ENGS}
        self.tiles = {}
        self.dma_count = {}
        self.seg_marks = []

    def core_barrier(self, prefetch=None):
        self.barrier()
        if prefetch is not None:
            prefetch()
        self.seg_marks.append({e: len(self.ops[e]) for e in self.ENGS})

    def add(self, eng, fn, reads=(), writes=(), semkey=None):
        op = Op()
        op.eng = eng; op.fn = fn; op.idx = len(self.ops[eng]); op.inc = False
        op.is_dma = semkey is not None; op.semkey = semkey; op.count = None
        deps = []
        for k in reads:
            st = self.tiles.get(k)
            if st and st[0] is not None:
                deps.append(st[0])
        for k in writes:
            st = self.tiles.get(k)
            if st:
                if st[0] is not None:
                    deps.append(st[0])
                deps.extend(st[1])
        waits = {}
        for d in deps:
            if d.is_dma:
                key = ("dma", d.semkey); val = self.dma_count[d.semkey]
                waits[key] = max(waits.get(key, 0), val)
            else:
                if d.eng == "tensor" and eng == "tensor" and not op.is_dma:
                    continue
                d.inc = True
                key = ("eng", d.eng)
                cur = waits.get(key)
                if cur is None or d.idx > cur.idx:
                    waits[key] = d
        op.waits = waits
        if op.is_dma:
            self.dma_count[semkey] = self.dma_count.get(semkey, 0) + 16
        for k in reads:
            st = self.tiles.setdefault(k, [None, []])
            st[1].append(op)
        for k in writes:
            self.tiles[k] = [op, []]
        self.ops[eng].append(op)
        return op

    def barrier(self):
        last = {}
        for e in self.ENGS:
            for op in reversed(self.ops[e]):
                if not op.is_dma and op.fn is not None:
                    last[e] = op
                    break
        dma_snapshot = dict(self.dma_count)
        for e in self.ENGS:
            op = Op()
            op.eng = e; op.fn = None; op.idx = len(self.ops[e]); op.inc = False
            op.is_dma = False; op.semkey = None; op.count = None
            waits = {}
            for f, lo in last.items():
                if f == "tensor" and e == "tensor":
                    continue
                lo.inc = True
                waits[("eng", f)] = lo
            for k, v in dma_snapshot.items():
                waits[("dma", k)] = v
            op.waits = waits
            self.ops[e].append(op)
        self.tiles.clear()

    def emit(self, final_eng="gpsimd"):
        nc = self.nc
        for e in self.ENGS:
            c = 0
            for op in self.ops[e]:
                if op.inc and not op.is_dma:
                    c += 1
                    op.count = c
        semkeys = sorted(self.dma_count.keys(), key=str)
        marks = self.seg_marks + [{e: len(self.ops[e]) for e in self.ENGS}]
        with contextlib.ExitStack() as st:
            esem = {e: st.enter_context(nc.semaphore("es_" + e)) for e in self.ENGS}
            dsem = {k: st.enter_context(nc.semaphore("ds_" + str(k))) for k in semkeys}
            known = {e: {} for e in self.ENGS}

            def run(e, eng, lo, hi, last):
                kn = known[e]
                for op in self.ops[e][lo:hi]:
                    for key, val in op.waits.items():
                        if key[0] == "dma":
                            sem = dsem[key[1]]; v = val
                        else:
                            sem = esem[key[1]]; v = val.count
                        if kn.get(key, 0) >= v:
                            continue
                        kn[key] = v
                        eng.wait_ge(sem, v)
                    if op.fn is None:
                        continue
                    ins = op.fn(eng)
                    if op.is_dma:
                        ins.then_inc(dsem[op.semkey], 16)
                    elif op.inc:
                        ins.then_inc(esem[e], 1)
                if last and e == final_eng:
                    for k in semkeys:
                        if kn.get(("dma", k), 0) < self.dma_count[k]:
                            eng.wait_ge(dsem[k], self.dma_count[k])

            prev = {e: 0 for e in self.ENGS}
            for si, mk in enumerate(marks):
                last = si == len(marks) - 1
                with nc.Block() as block:
                    @block.sync
                    def _(eng): run("sync", eng, prev["sync"], mk["sync"], last)

                    @block.scalar
                    def _(eng): run("scalar", eng, prev["scalar"], mk["scalar"], last)

                    @block.vector
                    def _(eng): run("vector", eng, prev["vector"], mk["vector"], last)

                    @block.gpsimd
                    def _(eng): run("gpsimd", eng, prev["gpsimd"], mk["gpsimd"], last)

                    @block.tensor
                    def _(eng): run("tensor", eng, prev["tensor"], mk["tensor"], last)
                prev = mk
                if not last:
                    nc.all_core_barrier()


def build(stop=99, taps=(), ncores=8):
    nc = bass.Bass("TRN2", target_bir_lowering=False, num_devices=ncores)

    def din(name, shape, dt=F32):
        return nc.dram_tensor(name, list(shape), dt, kind="ExternalInput").ap()

    xa = din("xa", [4224, 1024])
    cs = din("cs", [4096, 128])
    norm1_g = din("norm1_g", [1024])
    norm2_g = din("norm2_g", [1024])
    w_in = din("w_in", [1024, 6656])
    conv_w = din("conv_w", [3, 1024])
    q_norm_g = din("q_norm_g", [128])
    k_norm_g = din("k_norm_g", [128])
    w_conv_out = din("w_conv_out", [1024, 1024])
    w_attn_out = din("w_attn_out", [1024, 1024])
    w_o = din("w_o", [1024, 1024])
    w_gr = din("w_gr", [1024, 72])
    NEH = NEXP // 2
    w1 = din("w1", [NEH, 1024, 512])
    w3 = din("w3", [NEH, 1024, 512])
    w2 = din("w2", [NEH, 512, 1024])
    xblk_d = din("xblk", [128, 64], I32)
    consts = din("consts", [128, 512])
    out = nc.dram_tensor("out", [2048, 1024], F32, kind="ExternalOutput").ap()
    NROW = 2 * NEXP * CAP
    XS = nc.dram_tensor("XS", [NROW, 1024], BF16, addr_space="Shared").ap()
    YS = nc.dram_tensor("YS", [NROW, 1024], BF16, addr_space="Shared").ap()
    tap_out = {}
    for (name, shape, dt) in taps:
        tap_out[name] = nc.dram_tensor("tap_" + name, list(shape), dt, kind="ExternalOutput").ap()

    with contextlib.ExitStack() as st:
        TOTAL = 212000
        arena = st.enter_context(nc.sbuf_tensor("arena", [128, TOTAL], U8))
        PS = st.enter_context(nc.psum_tensor("ps", [128, 8, 512], F32))
        PSb = PS[:].bitcast(BF16) if hasattr(PS[:], "bitcast") else None

        def region(off, nbytes, dt):
            assert off % 4 == 0 and off + nbytes <= TOTAL, (off, nbytes)
            return arena[:, off:off + nbytes].bitcast(dt)

        S = Sched(nc)

        def V(fn, r=(), w=()): return S.add("vector", fn, r, w)
        def A(fn, r=(), w=()): return S.add("scalar", fn, r, w)
        def G(fn, r=(), w=()): return S.add("gpsimd", fn, r, w)
        def T(fn, r=(), w=()): return S.add("tensor", fn, r, w)
        def D(q, key, fn, r=(), w=()): return S.add(q, fn, r, w, semkey=key)

        def psb(bank):
            return PSb[:, bank, :]

        P0 = 198000
        p = [P0]

        def palloc(nbytes, dt):
            r = region(p[0], nbytes, dt); p[0] += (nbytes + 3) // 4 * 4
            return r
        cst = palloc(512 * 4, F32)
        id_f = cst[:, 0:128]; ebase = cst[:, 384:448]
        cstb = palloc(384 * 2, BF16)
        id_b = cstb[:, 0:128]; tri_b = cstb[:, 128:256]; ones_b = cstb[:, 256:384]
        g1T = palloc(8 * 4, F32)
        cw = palloc(24 * 4, F32).rearrange("p (k c) -> p k c", k=3)
        gq_bc = palloc(128 * 4, F32)
        gk_bc = palloc(128 * 4, F32)
        negc = palloc(4 * 4, F32)
        wgr = palloc(8 * 72 * 4, F32).rearrange("p (c n) -> p c n", c=8)
        Wk = palloc(2 * 16 * 4, F32).rearrange("p (k t) -> p k t", k=2)
        idx = palloc(2 * 16 * 4, I32).rearrange("p (k t) -> p k t", k=2)
        Mb = palloc(16 * 64 * 2, BF16).rearrange("p (t e) -> p t e", t=16)
        g2_bc = palloc(1024 * 4, F32)
        gq_p = palloc(128 * 4, F32)
        xblk = palloc(64 * 4, I32)
        gk_p = palloc(128 * 4, F32)
        assert p[0] <= TOTAL, p[0]

        OFF_A, OFF_B, OFF_QT, OFF_KT, OFF_VX, OFF_D, OFF_W = 0, 34816, 67584, 100352, 116736, 133376, 166144
        uT = region(OFF_A, 34816, BF16).rearrange("p (c t) -> p c t", c=8)
        Bbuf = region(OFF_B, 32768, BF16).rearrange("p (c t) -> p c t", c=8)
        QT = region(OFF_QT, 32768, BF16).rearrange("p (t h q) -> p t h q", t=16, h=8)
        KT = region(OFF_KT, 16384, BF16).rearrange("p (g t) -> p g t", g=2)
        VX = region(OFF_VX, 16640, BF16).rearrange("p (t g d) -> p t g d", t=32, g=2)
        OT = region(OFF_D, 32768, BF16).rearrange("p (c t) -> p c t", c=8)
        mT = region(OFF_QT, 32768, BF16).rearrange("p (c t) -> p c t", c=8)
        Wreg = OFF_W

        D("sync", "const", lambda e: e.dma_start(out=cst, in_=consts), w=["cst"])
        D("sync", "const", lambda e: e.dma_start(out=g1T, in_=norm1_g.rearrange("(c p) -> p c", p=128), allow_slow_non_contiguous=True), w=["g1T"])
        D("sync", "const", lambda e: e.dma_start(out=cw, in_=conv_w.rearrange("k (c p) -> p k c", p=128), allow_slow_non_contiguous=True), w=["cw"])
        D("sync", "const", lambda e: e.dma_start(out=gq_bc, in_=q_norm_g.partition_broadcast(128)), w=["gq"])
        D("sync", "const", lambda e: e.dma_start(out=gk_bc, in_=k_norm_g.partition_broadcast(128)), w=["gk"])
        D("sync", "const", lambda e: e.dma_start(out=g2_bc, in_=norm2_g.partition_broadcast(128)), w=["g2"])
        D("sync", "const", lambda e: e.dma_start(out=wgr, in_=w_gr.rearrange("(c p) n -> p c n", p=128)), w=["wgr"])
        D("sync", "const", lambda e: e.dma_start(out=xblk, in_=xblk_d), w=["xblk"])
        V(lambda e: e.tensor_copy(out=cstb, in_=cst[:, 0:384]), ["cst"], ["cstb"])
        V(lambda e: e.tensor_reduce(out=negc[:, 0:1], in_=gq_bc, axis=AX.X, op=ALU.max, apply_absolute_value=True), ["gq"], ["negc0"])
        V(lambda e: e.tensor_reduce(out=negc[:, 1:2], in_=gk_bc, axis=AX.X, op=ALU.max, apply_absolute_value=True), ["gk"], ["negc1"])
        V(lambda e: e.tensor_tensor(out=negc[:, 2:3], in0=negc[:, 0:1], in1=negc[:, 1:2], op=ALU.mult), ["negc0", "negc1"], ["negc2"])
        V(lambda e: e.tensor_scalar(out=negc[:, 3:4], in0=negc[:, 2:3], scalar1=-(128.0 ** 0.5), scalar2=None, op0=ALU.mult), ["negc2"], ["negc"])
        bnd = {}

        def mk_bnd(e):
            bnd['r'] = e.alloc_register("bnd")
            return e.reg_mov(bnd['r'], NROW - 1)
        G(mk_bnd)
        G(lambda e: e.memset(VX[:, :, :, 128:130], 1.0), [], ["vx1"])

        def tap(name, src_ap, keys):
            if name in tap_out:
                D("gpsimd", "tap_" + name, lambda e: e.dma_start(out=tap_out[name], in_=src_ap), r=keys)

        wqkv = region(Wreg, 24576, BF16).rearrange("p (c n) -> p c n", c=8)
        o = [OFF_B]

        def salloc(nbytes, dt, lim=OFF_B + 32768):
            r = region(o[0], nbytes, dt); o[0] += (nbytes + 3) // 4 * 4
            assert o[0] <= lim, (o[0], lim)
            return r
        xt = [salloc(4096, F32) for _ in range(3)]
        xn = [salloc(2048, BF16) for _ in range(2)]
        junk = salloc(4096, F32)
        uTo = [salloc(2048, BF16).rearrange("p (c t) -> p c t", c=8) for _ in range(2)]
        qkc = salloc(5120, F32)
        qkc_o = salloc(1024, F32)
        o = [OFF_D]

        def salloc2(nbytes, dt):
            return salloc(nbytes, dt, lim=OFF_D + 32768)
        qn = [salloc2(5120, F32) for _ in range(2)]
        qn_o = [salloc2(1024, F32) for _ in range(2)]
        tmp4 = [salloc2(2560, F32) for _ in range(4)]
        qr = [salloc2(2560, BF16) for _ in range(2)]
        cst_t = [salloc2(512, F32) for _ in range(3)]
        st1 = salloc2(4 * 32 * 4, F32).rearrange("p (s k) -> p s k", s=4)
        epsb = salloc2(4, F32)
        stg = [region(OFF_D + 12288 + i * 6144, 6144, F32) for i in range(2)]

        V(lambda e: e.memset(epsb, EPS), [], ["eps"])
        wq_keys = [f"wqkv{kc}" for kc in range(8)]
        V(lambda e: e.tensor_scalar(out=gq_p.rearrange("p (two i) -> p two i", two=2), in0=gq_bc.rearrange("p (i two) -> p two i", two=2), scalar1=128.0 ** -0.5, scalar2=None, op0=ALU.mult), ["gq"], ["gqp"])
        V(lambda e: e.tensor_copy(out=gk_p.rearrange("p (two i) -> p two i", two=2), in_=gk_bc.rearrange("p (i two) -> p two i", two=2)), ["gk"], ["gkp"])

        tile_order = []
        for i in range(16):
            tile_order += [i, 16 + i]
        tile_order.append(32)
        NTL = len(tile_order)

        def load_x(n):
            if n >= NTL:
                return
            t = tile_order[n]; s = n % 3
            D("sync", f"xt{s}", lambda e: e.dma_start(out=xt[s], in_=xa[t * 128:(t + 1) * 128, :]), w=[f"xt{s}"])

        def udst_of(n):
            t = tile_order[n]
            if t < 16:
                return uT[:, :, t * 128:(t + 1) * 128], ("uT", t)
            if t == 32:
                return uT[:, :, 2048:2176], ("uT", 16)
            return uTo[(n // 2) % 2], ("uTo", (n // 2) % 2)

        def tinfo(n):
            t = tile_order[n]
            own = t < 16
            nq = 10 if own else 2
            return t, own, nq

        def st_S0(n):
            s = n % 3; s2 = n % 2
            xts = xt[s]; xns = xn[s2]
            ss = st1[:, n % 4, 0:1]; rstd = st1[:, n % 4, 1:2]
            sk = ("ss", n % 4)
            A(lambda e: e.activation(out=junk[:, 0:1024], in_=xts, func=AF.Square, accum_out=ss), [f"xt{s}"], [sk])
            A(lambda e: e.activation(out=rstd, in_=ss, func=AF.Ln, scale=1.0 / 1024, bias=epsb), [sk, "eps"], [sk])
            A(lambda e: e.activation(out=rstd, in_=rstd, func=AF.Exp, scale=-0.5), [sk], [sk])
            A(lambda e: e.activation(out=xns, in_=xts, func=AF.Copy, scale=rstd), [f"xt{s}", sk], [f"xn{s2}"])
            load_x(n + 2)

        def st_S1(n):
            s2 = n % 2
            xns = xn[s2]
            pt = psb(s2).rearrange("p (c q) -> p c q", c=8)
            for c in range(8):
                T(lambda e, c=c: e.transpose(out=pt[:, c, :], in_=xns[:, c * 128:(c + 1) * 128], identity=id_b), [f"xn{s2}", "cstb"], [("ps", s2)])

        def st_S2(n):
            s2 = n % 2
            pt = psb(s2).rearrange("p (c q) -> p c q", c=8)
            udst, ukey = udst_of(n)
            V(lambda e: e.tensor_tensor(out=udst, in0=pt, in1=g1T.unsqueeze(2).to_broadcast([128, 8, 128]), op=ALU.mult), [("ps", s2), "g1T"], [ukey])

        def st_S3(n):
            t, own, nq = tinfo(n)
            if t == 32:
                return
            udst, ukey = udst_of(n)
            kvb = 4 if own else 5
            for kc in range(8):
                lhs = udst[:, kc, :]
                if own:
                    T(lambda e, kc=kc, lhs=lhs: e.matmul(PS[:, 2, :], lhsT=lhs, rhs=wqkv[:, kc, 0:512], start=(kc == 0), stop=(kc == 7)), [ukey] + wq_keys, [("ps", 2)])
                    T(lambda e, kc=kc, lhs=lhs: e.matmul(PS[:, 3, :], lhsT=lhs, rhs=wqkv[:, kc, 512:1024], start=(kc == 0), stop=(kc == 7)), [ukey], [("ps", 3)])
                T(lambda e, kc=kc, lhs=lhs: e.matmul(PS[:, kvb, :], lhsT=lhs, rhs=wqkv[:, kc, 1024:1536], start=(kc == 0), stop=(kc == 7)), [ukey] + wq_keys, [("ps", kvb)])

        def st_S4(n):
            t, own, nq = tinfo(n)
            if t == 32:
                return
            s3 = n % 3
            D("sync", f"cs{s3}", lambda e: e.dma_start(out=cst_t[s3], in_=cs[t * 128:(t + 1) * 128, :]), w=[f"cs{s3}"])
            if own:
                qkps = PS[:, 2:5, :].rearrange("p b n -> p (b n)")[:, 0:1280]
                pkeys = [("ps", 2), ("ps", 3), ("ps", 4)]; kvb = 4
                cdst = qkc; ckey = "qkc"
            else:
                qkps = PS[:, 5, 0:256]
                pkeys = [("ps", 5)]; kvb = 5
                cdst = qkc_o[:, 0:256]; ckey = "qkco"
            ssq = st1[:, n % 4, 8:8 + nq]; rs = st1[:, n % 4, 20:20 + nq]
            rk = ("rs", n % 4)
            for h in range(nq):
                A(lambda e, h=h: e.activation(out=junk[:, 0:128], in_=qkps[:, h * 128:(h + 1) * 128], func=AF.Square, accum_out=ssq[:, h:h + 1]), pkeys, [rk])
            A(lambda e: e.activation(out=rs, in_=ssq, func=AF.Ln, scale=1.0 / 128, bias=epsb), [rk, "eps"], [rk])
            A(lambda e: e.activation(out=rs, in_=rs, func=AF.Exp, scale=-0.5), [rk], [rk])
            A(lambda e: e.activation(out=cdst, in_=qkps, func=AF.Copy), pkeys, [ckey])
            A(lambda e: e.activation(out=VX[:, t, :, 0:128], in_=PS[:, kvb, 256:512].rearrange("p (g d) -> p g d", g=2), func=AF.Copy), [("ps", kvb)], [("vx", t)])

        def st_S5(n):
            t, own, nq = tinfo(n)
            if t == 32:
                return
            rs = st1[:, n % 4, 20:20 + nq]; rk = ("rs", n % 4)
            if own:
                src = qkc; ckey = "qkc"; qs = qn[(n // 2) % 2]
            else:
                src = qkc_o[:, 0:256]; ckey = "qkco"; qs = qn_o[(n // 2) % 2]
            qkey = ("qn", (n // 2) % 2, own)
            src3 = src.rearrange("p (h i two) -> p h two i", i=64, two=2)
            for h in range(nq):
                gp = gq_p if (own and h < 8) else gk_p
                dst = qs[:, h * 128:(h + 1) * 128].rearrange("p (two i) -> p two i", two=2)
                V(lambda e, h=h, gp=gp, dst=dst: e.scalar_tensor_tensor(out=dst, in0=src3[:, h, :, :], scalar=rs[:, h:h + 1], in1=gp.rearrange("p (two i) -> p two i", two=2), op0=ALU.mult, op1=ALU.mult), [ckey, rk, "gqp", "gkp"], [qkey])

        def rope_views(n):
            t, own, nq = tinfo(n)
            qs = qn[(n // 2) % 2] if own else qn_o[(n // 2) % 2]
            q3 = qs[:, 0:nq * 128].rearrange("p (h d) -> p h d", d=128)
            tt = [tmp4[i][:, 0:nq * 64].rearrange("p (h i) -> p h i", i=64) for i in range(4)]
            return t, own, nq, q3, tt

        def st_S6(n):
            t, own, nq, q3, tt = rope_views(n)
            if t == 32:
                return
            s3 = n % 3
            qkey = ("qn", (n // 2) % 2, own)
            x0 = q3[:, :, 0:64]; x1 = q3[:, :, 64:128]
            cosb = cst_t[s3][:, 0:64].unsqueeze(1).to_broadcast([128, nq, 64])
            sinb = cst_t[s3][:, 64:128].unsqueeze(1).to_broadcast([128, nq, 64])
            ck = f"cs{s3}"
            V(lambda e: e.tensor_tensor(out=tt[0], in0=x0, in1=cosb, op=ALU.mult), [qkey, ck], ["t0"])
            G(lambda e: e.tensor_tensor(out=tt[1], in0=x1, in1=sinb, op=ALU.mult), [qkey, ck], ["t1"])
            G(lambda e: e.tensor_tensor(out=tt[2], in0=x0, in1=sinb, op=ALU.mult), [qkey, ck], ["t2"])
            V(lambda e: e.tensor_tensor(out=tt[3], in0=x1, in1=cosb, op=ALU.mult), [qkey, ck], ["t3"])

        def st_S7(n):
            t, own, nq, q3, tt = rope_views(n)
            if t == 32:
                return
            h0 = 10 - nq
            qrs = qr[n % 2]; qk_ = ("qr", n % 2)
            r3 = qrs[:, h0 * 128:1280].rearrange("p (h d) -> p h d", d=128)
            V(lambda e: e.tensor_tensor(out=r3[:, :, 0:64], in0=tt[0], in1=tt[1], op=ALU.subtract), ["t0", "t1"], [(qk_, 0)])
            G(lambda e: e.tensor_tensor(out=r3[:, :, 64:128], in0=tt[2], in1=tt[3], op=ALU.add), ["t2", "t3"], [(qk_, 1)])

        def st_S8(n):
            t, own, nq = tinfo(n)
            if t == 32:
                return
            qrs = qr[n % 2]; qk_ = ("qr", n % 2)
            pq = psb(6).rearrange("p (h q) -> p h q", h=8)
            pk = psb(7).rearrange("p (h q) -> p h q", h=8)
            if own:
                for h in range(8):
                    T(lambda e, h=h: e.transpose(out=pq[:, h, :], in_=qrs[:, h * 128:(h + 1) * 128], identity=id_b), [(qk_, 0), (qk_, 1)], [("ps", 6)])
            for g in range(2):
                T(lambda e, g=g: e.transpose(out=pk[:, g, :], in_=qrs[:, (8 + g) * 128:(9 + g) * 128], identity=id_b), [(qk_, 0), (qk_, 1)], [("ps", 7)])

        def st_S9(n):
            t, own, nq = tinfo(n)
            if t == 32:
                return
            pq = psb(6).rearrange("p (h q) -> p h q", h=8)
            pk = psb(7).rearrange("p (h q) -> p h q", h=8)
            if own:
                A(lambda e: e.activation(out=QT[:, t, :, :], in_=pq, func=AF.Copy), [("ps", 6)], [("QT", t)])
            V(lambda e: e.tensor_copy(out=KT[:, :, t * 128:(t + 1) * 128], in_=pk[:, 0:2, :]), [("ps", 7)], [("KT", t)])

        stages = [st_S0, st_S1, st_S2, st_S3, st_S4, st_S5, st_S6, st_S7, st_S8, st_S9]
        load_x(0); load_x(1)
        for kc in range(8):
            s = kc % 2
            D("sync", f"stg{s}", lambda e, kc=kc, s=s: e.dma_start(out=stg[s], in_=w_in[kc * 128:(kc + 1) * 128, 3072:4608]), w=[f"stg{s}"])
            if kc % 2 == 0:
                G(lambda e, kc=kc, s=s: e.tensor_copy(out=wqkv[:, kc, :], in_=stg[s]), [f"stg{s}"], [f"wqkv{kc}"])
            else:
                V(lambda e, kc=kc, s=s: e.tensor_copy(out=wqkv[:, kc, :], in_=stg[s]), [f"stg{s}"], [f"wqkv{kc}"])
        for it in range(NTL + 9):
            for off in range(9, -1, -1):
                n = it - off
                if 0 <= n < NTL:
                    stages[off](n)

        tap("uT", uT, [("uT", i) for i in range(17)])
        tap("QT", QT, [("QT", i) for i in range(16)])
        tap("KT", KT, [("KT", i) for i in range(32)])
        tap("VX", VX, [("vx", i) for i in range(32)] + ["vx1"])
        S.barrier()
        if stop <= 1:
            S.emit(); return nc

        zt = region(Wreg, 2048, BF16)
        G(lambda e: e.memset(zt, 0.0), [], ["zt"])
        for i in range(16):
            D("gpsimd", "xsz", lambda e, i=i: e.dma_start(out=XS[i * 1024:(i + 1) * 1024, :].rearrange("(a p) n -> p a n", p=128), in_=zt.unsqueeze(1).to_broadcast([128, 8, 1024])), r=["zt"], w=["XS"])

        o = [OFF_B]
        PTs = [salloc(1024, BF16) for _ in range(4)]
        Otok = [salloc(2048, BF16).rearrange("p (h d) -> p h d", h=8) for _ in range(2)]
        rd = salloc(64, F32)
        SB = [0, 1, 6]
        seq = [(qt, g, kt) for qt in range(16) for g in range(2) for kt in range(32)]
        nbias = negc[:, 3:4]

        def issue_S(i):
            qt, g, kt = seq[i]
            b = SB[i % 3]
            T(lambda e: e.matmul(PS[:, b, :].rearrange("p (h q) -> p h q", h=4), lhsT=KT[:, g, kt * 128:(kt + 1) * 128], rhs=QT[:, qt, 4 * g:4 * g + 4, :], start=True, stop=True), [], [("ps", b)])

        def oacc(qt, g, h):
            sset = (qt * 2 + g) % 2
            bank = 2 + 2 * sset + h // 2
            return PS[:, bank, :].rearrange("p (j n) -> p j n", j=2), bank, h % 2

        issue_S(0); issue_S(1)

        def p2_step(i, qt, g, kt):
            if i + 2 < len(seq):
                issue_S(i + 2)
            b = SB[i % 3]; ps_ = PTs[i % 4]; pk_ = ("PT", i % 4)
            A(lambda e: e.activation(out=ps_, in_=PS[:, b, :], func=AF.Exp, bias=nbias, scale=1.0), [("ps", b), "negc"], [pk_])
            for h in range(4):
                ov, bank, j = oacc(qt, g, h)
                T(lambda e, h=h, ov=ov, j=j: e.matmul(ov[:, j, 0:129], lhsT=ps_[:, h * 128:(h + 1) * 128], rhs=VX[:, kt, g, 0:129], start=(kt == 0 and j == 0), stop=(kt == 31), skip_group_check=True), [pk_], [("ps", bank)])
            if kt == 31:
                os_ = Otok[qt % 2]; ok_ = ("Otok", qt % 2)
                for hb in range(2):
                    ov, bank, _ = oacc(qt, g, hb * 2)
                    V(lambda e, ov=ov, hb=hb: e.reciprocal(out=rd[:, 2 * hb:2 * hb + 2], in_=ov[:, :, 128]), [("ps", bank)], [("rd", hb)])
                    V(lambda e, ov=ov, hb=hb: e.tensor_tensor(out=os_[:, 4 * g + 2 * hb:4 * g + 2 * hb + 2, :], in0=ov[:, :, 0:128], in1=rd[:, 2 * hb:2 * hb + 2].unsqueeze(2).to_broadcast([128, 2, 128]), op=ALU.mult), [("ps", bank), ("rd", hb)], [ok_])
                if g == 1:
                    pt = psb(7).rearrange("p (c q) -> p c q", c=8)
                    for c in range(8):
                        T(lambda e, c=c: e.transpose(out=pt[:, c, :], in_=os_[:, c, :], identity=id_b), [ok_], [("ps", 7)])
                    V(lambda e: e.tensor_copy(out=OT[:, :, qt * 128:(qt + 1) * 128], in_=pt), [("ps", 7)], [("OT", qt)])

        for i_, (qt_, g_, kt_) in enumerate(seq):
            p2_step(i_, qt_, g_, kt_)
        tap("OT", OT, [("OT", i) for i in range(16)])
        S.core_barrier()
        if stop <= 2:
            S.emit(); return nc

        o = [OFF_B]
        ybuf = salloc(2052 * 4, F32)
        zb = [salloc(2048, F32) for _ in range(2)]
        ccs = [salloc(2048, F32) for _ in range(2)]
        cstg3 = [salloc(4096, F32).rearrange("p (c n) -> p c n", c=8) for _ in range(3)]
        o = [Wreg + 16384]
        wcb3 = [salloc(2048, BF16, lim=P0).rearrange("p (c n) -> p c n", c=8) for _ in range(6)]
        wco = region(Wreg, 16384, BF16).rearrange("p (c n) -> p c n", c=8)
        wao = region(OFF_KT, 16384, BF16).rearrange("p (c n) -> p c n", c=8)
        wstg4 = [region(OFF_VX + i * 4096, 4096, F32) for i in range(2)]

        def load_big(wdst, wsrc, name, ceng, wstg):
            for kc in range(8):
                s = kc % 2
                D("sync", f"wstg{s}", lambda e, kc=kc, s=s: e.dma_start(out=wstg[s], in_=wsrc[kc * 128:(kc + 1) * 128, :]), w=[f"wstg{s}"])
                S.add(ceng, lambda e, kc=kc, s=s: e.tensor_copy(out=wdst[:, kc, :], in_=wstg[s]), [f"wstg{s}"], [(name, kc)])
            return [(name, kc) for kc in range(8)]
        nld = [0]

        def load_chunk(col0):
            s = nld[0] % 3; sb_ = nld[0] % 6; nld[0] += 1
            D("sync", f"cstg{s}", lambda e: e.dma_start(out=cstg3[s], in_=w_in[:, col0:col0 + 128].rearrange("(c p) n -> p c n", p=128)), w=[f"cstg{s}"])
            A(lambda e: e.activation(out=wcb3[sb_], in_=cstg3[s], func=AF.Copy), [f"cstg{s}"], [f"wcb{sb_}"])
            return wcb3[sb_], f"wcb{sb_}"

        def mm8(bank, wch, wkey, rhs_fn, rkeys, n=512):
            for kc in range(8):
                T(lambda e, kc=kc: e.matmul(PS[:, bank, 0:n], lhsT=wch[:, kc, :], rhs=rhs_fn(kc), start=(kc == 0), stop=(kc == 7)), [wkey] + rkeys, [("ps", bank)])

        ukeys_all = [("uT", i) for i in range(17)]
        chunks = [load_chunk(1024 + 0), load_chunk(2048 + 0), load_chunk(0)]
        big_steps = [(wco, w_conv_out, "wco", kc) for kc in range(8)] + [(wao, w_attn_out, "wao", kc) for kc in range(8)]

        def big_step():
            if not big_steps:
                return
            wdst, wsrc, name, kc = big_steps.pop(0)
            s = kc % 2
            D("sync", f"wstg{s}", lambda e: e.dma_start(out=wstg4[s], in_=wsrc[kc * 128:(kc + 1) * 128, :]), w=[f"wstg{s}"])
            G(lambda e: e.tensor_copy(out=wdst[:, kc, :], in_=wstg4[s]), [f"wstg{s}"], [(name, kc)])
        bk = [0]

        def nb():
            b = bk[0] % 8; bk[0] += 1
            return b
        for j in range(8):
            wcc, wcx, wcb_ = chunks
            if j + 1 < 8:
                chunks = [load_chunk(1024 + (j + 1) * 128), load_chunk(2048 + (j + 1) * 128), load_chunk((j + 1) * 128)]
            big_step(); big_step()
            for nt in range(5):
                if nt < 4:
                    rf = lambda kc, nt=nt: uT[:, kc, nt * 512:(nt + 1) * 512]; n = 512; ycol = 1 + nt * 512
                else:
                    rf = lambda kc: uT[:, kc, 2048:2050]; n = 2
                b1 = nb(); b2 = nb()
                mm8(b1, wcc[0], wcc[1], rf, [], n)
                mm8(b2, wcx[0], wcx[1], rf, [], n)
                cs_ = ccs[nt % 2]; ck = ("ccs", nt % 2)
                A(lambda e, b1=b1, n=n, cs_=cs_: e.activation(out=cs_[:, 0:n], in_=PS[:, b1, 0:n], func=AF.Copy), [("ps", b1)], [ck])
                if nt < 4:
                    V(lambda e, b2=b2, cs_=cs_, ycol=ycol: e.tensor_tensor(out=ybuf[:, ycol:ycol + 512], in0=PS[:, b2, :], in1=cs_, op=ALU.mult), [("ps", b2), ck], [("y", nt)])
                else:
                    V(lambda e, b2=b2, cs_=cs_: e.tensor_tensor(out=ybuf[:, 0:1], in0=PS[:, b2, 0:1], in1=cs_[:, 0:1], op=ALU.mult), [("ps", b2), ck], [("y", 4)])
                    V(lambda e, b2=b2, cs_=cs_: e.tensor_tensor(out=ybuf[:, 2049:2050], in0=PS[:, b2, 1:2], in1=cs_[:, 1:2], op=ALU.mult), [("ps", b2), ck], [("y", 5)])
            ykeys = [("y", i) for i in range(6)]
            for nt in range(4):
                z = zb[nt % 2]; zk = ("z", nt % 2)
                c0 = nt * 512
                V(lambda e, z=z, c0=c0, j=j: e.tensor_scalar(out=z, in0=ybuf[:, c0:c0 + 512], scalar1=cw[:, 0, j:j + 1], scalar2=None, op0=ALU.mult), ykeys + ["cw"], [zk])
                V(lambda e, z=z, c0=c0, j=j: e.scalar_tensor_tensor(out=z, in0=ybuf[:, c0 + 1:c0 + 513], scalar=cw[:, 1, j:j + 1], in1=z, op0=ALU.mult, op1=ALU.add), ykeys + [zk], [zk])
                V(lambda e, z=z, c0=c0, j=j: e.scalar_tensor_tensor(out=z, in0=ybuf[:, c0 + 2:c0 + 514], scalar=cw[:, 2, j:j + 1], in1=z, op0=ALU.mult, op1=ALU.add), ykeys + [zk], [zk])
                b3 = nb()
                mm8(b3, wcb_[0], wcb_[1], lambda kc, nt=nt: uT[:, kc, nt * 512:(nt + 1) * 512], [])
                V(lambda e, b3=b3, z=z, nt=nt, j=j: e.tensor_tensor(out=mT[:, j, nt * 512:(nt + 1) * 512], in0=PS[:, b3, :], in1=z, op=ALU.mult), [("ps", b3), zk], [("mT", j, nt)])
        tap("mT", mT, [("mT", j, nt) for j in range(8) for nt in range(4)])
        S.barrier()
        if stop <= 3:
            S.emit(); return nc

        o = [OFF_VX + 8192]
        sg = [salloc(2048, F32, lim=OFF_D) for _ in range(2)]
        tm = [salloc(2048, F32, lim=OFF_D) for _ in range(2)]
        o = [Wreg + 16384]
        cstg4 = [salloc(4096, F32, lim=P0).rearrange("p (c n) -> p c n", c=8) for _ in range(2)]
        wcb4 = [salloc(2048, BF16, lim=P0).rearrange("p (c n) -> p c n", c=8) for _ in range(2)]
        nld[0] = 0

        def load_chunk2(col0):
            s = nld[0] % 2; nld[0] += 1
            D("sync", f"cstg{s}", lambda e: e.dma_start(out=cstg4[s], in_=w_in[:, col0:col0 + 128].rearrange("(c p) n -> p c n", p=128)), w=[f"cstg{s}"])
            G(lambda e: e.tensor_copy(out=wcb4[s], in_=cstg4[s]), [f"cstg{s}"], [f"wcb{s}"])
            return wcb4[s], f"wcb{s}"
        kco = []; kao = []
        gch = [load_chunk2(4608), load_chunk2(5632)]
        def p4_step(oc, nt, wgc, wga):
            if True:
                tk = slice(nt * 512, (nt + 1) * 512)
                byc, bgc, bya, bga = nb(), nb(), nb(), nb()
                for kc in range(8):
                    T(lambda e, kc=kc, byc=byc: e.matmul(PS[:, byc, :], lhsT=wco[:, kc, oc * 128:(oc + 1) * 128], rhs=mT[:, kc, tk], start=(kc == 0), stop=(kc == 7)), kco, [("ps", byc)])
                mm8(bgc, wgc[0], wgc[1], lambda kc: uT[:, kc, tk], [])
                for kc in range(8):
                    T(lambda e, kc=kc, bya=bya: e.matmul(PS[:, bya, :], lhsT=wao[:, kc, oc * 128:(oc + 1) * 128], rhs=OT[:, kc, tk], start=(kc == 0), stop=(kc == 7)), kao, [("ps", bya)])
                mm8(bga, wga[0], wga[1], lambda kc: uT[:, kc, tk], [])
                A(lambda e, bgc=bgc: e.activation(out=sg[0], in_=PS[:, bgc, :], func=AF.Sigmoid), [("ps", bgc)], ["sg0"])
                A(lambda e, bga=bga: e.activation(out=sg[1], in_=PS[:, bga, :], func=AF.Sigmoid), [("ps", bga)], ["sg1"])
                V(lambda e, byc=byc: e.tensor_tensor(out=tm[0], in0=PS[:, byc, :], in1=sg[0], op=ALU.mult), [("ps", byc), "sg0"], ["tm0"])
                V(lambda e, bya=bya: e.tensor_tensor(out=tm[1], in0=PS[:, bya, :], in1=sg[1], op=ALU.mult), [("ps", bya), "sg1"], ["tm1"])
                G(lambda e: e.tensor_tensor(out=Bbuf[:, oc, tk], in0=tm[0], in1=tm[1], op=ALU.add), ["tm0", "tm1"], [("mg", oc, nt)])

        for oc in range(8):
            wgc, wga = gch
            for nt in range(4):
                p4_step(oc, nt, wgc, wga)
            if oc + 1 < 8:
                gch = [load_chunk2(4608 + (oc + 1) * 128), load_chunk2(5632 + (oc + 1) * 128)]
        tap("mg", Bbuf, [("mg", oc, nt) for oc in range(8) for nt in range(4)])
        S.barrier()
        if stop <= 4:
            S.emit(); return nc

        wo = region(Wreg, 16384, BF16).rearrange("p (c n) -> p c n", c=8)
        o = [OFF_A]
        xt5 = [salloc(4096, F32, lim=OFF_B) for _ in range(3)]
        x1t = [salloc(4096, F32, lim=OFF_B) for _ in range(2)]
        junk5 = salloc(4096, F32, lim=OFF_B)
        st5 = salloc(16 * 4 * 4, F32, lim=OFF_B).rearrange("p (t k) -> p t k", t=16)
        eps5 = salloc(4, F32, lim=OFF_B)
        o = [OFF_QT]
        sq2 = [salloc(4096, F32, lim=OFF_D) for _ in range(2)]
        u2f = [salloc(4096, F32, lim=OFF_D) for _ in range(2)]
        u2T = [salloc(4096, F32, lim=OFF_D).rearrange("p (c q) -> p c q", c=8) for _ in range(2)]
        LG = salloc(16 * 72 * 4, F32, lim=OFF_D).rearrange("p (t n) -> p t n", t=16)
        MA = [salloc(16 * 64 * 4, F32, lim=OFF_D).rearrange("p (t e) -> p t e", t=16) for _ in range(2)]
        wstg5 = [salloc(4096, F32, lim=OFF_D) for _ in range(3)]
        RkA = salloc(16 * 64 * 4, F32, lim=OFF_D).rearrange("p (t e) -> p t e", t=16)
        T64 = salloc(16 * 64 * 4, F32, lim=OFF_D).rearrange("p (t e) -> p t e", t=16)
        rb = salloc(16 * 64 * 4, F32, lim=OFF_D).rearrange("p (k t x) -> p k t x", k=8, t=16)
        rs_ = salloc(16 * 16 * 4, F32, lim=OFF_D).rearrange("p (k t) -> p k t", k=16)
        U2B = region(OFF_D, 32768, BF16).rearrange("p (t n) -> p t n", t=16)
        kwo = []
        for kc in range(8):
            s_ = kc % 3
            D("sync", f"wstg{s_}", lambda e, kc=kc, s_=s_: e.dma_start(out=wstg5[s_], in_=w_o[kc * 128:(kc + 1) * 128, :]), w=[f"wstg{s_}"])
            if kc % 3 == 2:
                A(lambda e, kc=kc, s_=s_: e.activation(out=wo[:, kc, :], in_=wstg5[s_], func=AF.Copy), [f"wstg{s_}"], [("wo", kc)])
            else:
                (V if kc % 3 == 0 else G)(lambda e, kc=kc, s_=s_: e.tensor_copy(out=wo[:, kc, :], in_=wstg5[s_]), [f"wstg{s_}"], [("wo", kc)])
            kwo.append(("wo", kc))
        V(lambda e: e.memset(eps5, EPS), [], ["eps5"])

        def ld_x5(t):
            if t < 16:
                D("sync", f"x5{t % 3}", lambda e: e.dma_start(out=xt5[t % 3], in_=xa[t * 128:(t + 1) * 128, :]), w=[("x5", t % 3)])

        def q0(t):
            pb = 2 * (t % 2)
            for half in range(2):
                for kc in range(8):
                    T(lambda e, kc=kc, half=half: e.matmul(PS[:, pb + half, :], lhsT=Bbuf[:, kc, t * 128:(t + 1) * 128], rhs=wo[:, kc, half * 512:(half + 1) * 512], start=(kc == 0), stop=(kc == 7)), kwo, [("ps", pb + half)])

        def q1(t):
            pb = 2 * (t % 2); s = t % 2
            V(lambda e: e.tensor_tensor(out=x1t[s].rearrange("p (b n) -> p b n", b=2), in0=PS[:, pb:pb + 2, :], in1=xt5[t % 3].rearrange("p (b n) -> p b n", b=2), op=ALU.add), [("ps", pb), ("ps", pb + 1), ("x5", t % 3)], [("x1", s)])
            D("sync", f"x1o{s}", lambda e: e.dma_start(out=out[t * 128:(t + 1) * 128, :], in_=x1t[s]), r=[("x1", s)], w=[("out", t)])
            ld_x5(t + 2)

        def q2(t):
            s = t % 2
            ss = st5[:, t, 0:1]; rstd = st5[:, t, 1:2]; sk = ("st5", t)
            A(lambda e: e.activation(out=junk5, in_=x1t[s], func=AF.Square, accum_out=ss), [("x1", s)], [sk])
            A(lambda e: e.activation(out=rstd, in_=ss, func=AF.Ln, scale=1.0 / 1024, bias=eps5), [sk, "eps5"], [sk])
            A(lambda e: e.activation(out=rstd, in_=rstd, func=AF.Exp, scale=-0.5), [sk], [sk])
            A(lambda e: e.activation(out=sq2[s], in_=x1t[s], func=AF.Copy, scale=rstd), [("x1", s), sk], [("sq2", s)])

        def q3(t):
            s = t % 2
            V(lambda e: e.tensor_tensor(out=u2f[s], in0=sq2[s], in1=g2_bc, op=ALU.mult), [("sq2", s), "g2"], [("u2f", s)])

        def q4(t):
            s = t % 2
            A(lambda e: e.activation(out=U2B[:, t, :], in_=u2f[s], func=AF.Copy), [("u2f", s)], [("u2b", t)])
            for c in range(8):
                T(lambda e, c=c: e.transpose(out=PS[:, 4 + c // 4, (c % 4) * 128:(c % 4 + 1) * 128], in_=u2f[s][:, c * 128:(c + 1) * 128], identity=id_f), [("u2f", s), "cst"], [("ps", 4 + c // 4)])

        def q5(t):
            s = t % 2
            V(lambda e: e.tensor_copy(out=u2T[s], in_=PS[:, 4:6, :].rearrange("p b (c q) -> p (b c) q", c=4)), [("ps", 4), ("ps", 5)], [("u2T", s)])

        def q6(t):
            s = t % 2; c6 = 128 * (t % 2)
            for kc in range(8):
                T(lambda e, kc=kc: e.matmul(PS[:, 6, c6:c6 + 72], lhsT=u2T[s][:, kc, :], rhs=wgr[:, kc, :], start=(kc == 0), stop=(kc == 7)), [("u2T", s), "wgr"], [("ps", 6)])

        def q7(t):
            c6 = 128 * (t % 2)
            V(lambda e: e.tensor_copy(out=LG[:, t, :], in_=PS[:, 6, c6:c6 + 72]), [("ps", 6)], [("lg", t)])

        tri_f = cst[:, 128:256]; ones_f = cst[:, 256:384]
        NBT = 4

        def route_batch(bi):
            t0 = bi * NBT; t1 = t0 + NBT; nb_ = NBT
            ts = slice(t0, t1)
            K = lambda name: (name, bi)
            lgk = [("lg", t) for t in range(t0, t1)]
            lgg = LG[:, ts, 0:8]
            lge = LG[:, ts, 8:72].rearrange("p t (g x) -> p t g x", g=8)
            gm, nv1, v2, se, pg, dd, ed, w1_ = (rs_[:, i, ts] for i in range(8))
            ohg, eg, sel, oh1, sel2, oh2 = (rb[:, i, ts, :] for i in range(6))

            def bc3(a):
                return a.unsqueeze(2).to_broadcast([128, nb_, 8])
            V(lambda e: e.tensor_reduce(out=gm, in_=lgg, axis=AX.X, op=ALU.max), lgk, [K("gm")])
            V(lambda e: e.tensor_tensor(out=ohg, in0=lgg, in1=bc3(gm), op=ALU.is_equal), lgk + [K("gm")], [K("ohg")])
            V(lambda e: e.tensor_tensor(out=eg, in0=lgg, in1=bc3(gm), op=ALU.subtract), lgk + [K("gm")], [K("eg")])
            A(lambda e: e.activation(out=eg, in_=eg, func=AF.Exp), [K("eg")], [K("eg")])
            V(lambda e: e.tensor_reduce(out=se, in_=eg, axis=AX.X, op=ALU.add), [K("eg")], [K("se")])
            V(lambda e: e.reciprocal(out=pg, in_=se), [K("se")], [K("pg")])
            T64b = T64[:, ts, :]
            V(lambda e: e.tensor_tensor(out=T64b.rearrange("p t (g x) -> p t g x", g=8), in0=lge, in1=ohg.unsqueeze(3).to_broadcast([128, nb_, 8, 8]), op=ALU.mult), lgk + [K("ohg")], [K("t64")])
            V(lambda e: e.tensor_reduce(out=sel, in_=T64b.rearrange("p t (g x) -> p t x g", g=8), axis=AX.X, op=ALU.add), [K("t64")], [K("sel")])
            V(lambda e: e.tensor_reduce(out=nv1, in_=sel, axis=AX.X, op=ALU.max), [K("sel")], [K("v1")])
            V(lambda e: e.tensor_tensor(out=oh1, in0=sel, in1=bc3(nv1), op=ALU.is_equal), [K("sel"), K("v1")], [K("oh1")])
            V(lambda e: e.scalar_tensor_tensor(out=sel2, in0=oh1, scalar=-1e30, in1=sel, op0=ALU.mult, op1=ALU.add), [K("oh1"), K("sel")], [K("sel2")])
            V(lambda e: e.tensor_reduce(out=v2, in_=sel2, axis=AX.X, op=ALU.max), [K("sel2")], [K("v2")])
            V(lambda e: e.tensor_tensor(out=oh2, in0=sel2, in1=bc3(v2), op=ALU.is_equal), [K("sel2"), K("v2")], [K("oh2")])
            V(lambda e: e.tensor_tensor(out=dd, in0=v2, in1=nv1, op=ALU.subtract), [K("v1"), K("v2")], [K("dd")])
            A(lambda e: e.activation(out=ed, in_=dd, func=AF.Exp), [K("dd")], [K("ed")])
            V(lambda e: e.tensor_scalar(out=w1_, in0=ed, scalar1=1.0, scalar2=None, op0=ALU.add), [K("ed")], [K("w1_")])
            V(lambda e: e.reciprocal(out=w1_, in_=w1_), [K("w1_")], [K("w1_")])
            V(lambda e: e.tensor_tensor(out=Wk[:, 0, ts], in0=w1_, in1=pg, op=ALU.mult), [K("w1_"), K("pg")], [K("Wk0")])
            V(lambda e: e.tensor_tensor(out=Wk[:, 1, ts], in0=Wk[:, 0, ts], in1=ed, op=ALU.mult), [K("Wk0"), K("ed")], [K("Wk1")])
            for k, ohk in enumerate((oh1, oh2)):
                V(lambda e, k=k, ohk=ohk: e.tensor_tensor(out=MA[k][:, ts, :].rearrange("p t (g x) -> p t g x", g=8), in0=ohg.unsqueeze(3).to_broadcast([128, nb_, 8, 8]), in1=ohk.unsqueeze(2).to_broadcast([128, nb_, 8, 8]), op=ALU.mult), [K("ohg"), K("oh1"), K("oh2")], [("MA", k, bi)])
            V(lambda e: e.tensor_tensor(out=Mb[:, ts, :], in0=MA[0][:, ts, :], in1=MA[1][:, ts, :], op=ALU.add), [("MA", 0, bi), ("MA", 1, bi)], [("Mf", bi)])
            mfk = [("Mf", j) for j in range(bi + 1)]
            first = True
            for t in range(t0, t1):
                c0 = (t - t0) * 64
                T(lambda e, t=t, c0=c0, first=first: e.matmul(PS[:, 7, c0:c0 + 64], lhsT=tri_b, rhs=Mb[:, t, :], start=first, stop=(t == 0), skip_group_check=True), mfk + ["cstb"], [("ps", 7)])
                first = False
                for i in range(t):
                    T(lambda e, i=i, c0=c0, t=t: e.matmul(PS[:, 7, c0:c0 + 64], lhsT=ones_b, rhs=Mb[:, i, :], start=False, stop=(i == t - 1), skip_group_check=True), mfk + ["cstb"], [("ps", 7)])
            RkB = RkA[:, ts, :]
            V(lambda e: e.tensor_copy(out=RkB.rearrange("p t e -> p (t e)"), in_=PS[:, 7, 0:nb_ * 64]), [("ps", 7)], [K("Rk")])
            rsel, bsel, ov = (rs_[:, 8 + i, ts] for i in range(3))
            for k in range(2):
                V(lambda e, k=k: e.tensor_tensor(out=T64b, in0=RkB, in1=MA[k][:, ts, :], op=ALU.mult), [K("Rk"), ("MA", k, bi)], [K("t64")])
                V(lambda e: e.tensor_reduce(out=rsel, in_=T64b, axis=AX.X, op=ALU.add), [K("t64")], [K("rsel")])
                V(lambda e, k=k: e.tensor_tensor(out=T64b, in0=MA[k][:, ts, :], in1=ebase.unsqueeze(1).to_broadcast([128, nb_, 64]), op=ALU.mult), [("MA", k, bi), "cst", K("rsel")], [K("t64")])
                V(lambda e: e.tensor_reduce(out=bsel, in_=T64b, axis=AX.X, op=ALU.add), [K("t64")], [K("bsel")])
                V(lambda e: e.tensor_scalar(out=ov, in0=rsel, scalar1=float(CAP) - 0.5, scalar2=100000.0, op0=ALU.is_ge, op1=ALU.mult), [K("rsel")], [K("ov")])
                V(lambda e: e.tensor_tensor(out=bsel, in0=bsel, in1=rsel, op=ALU.add), [K("bsel"), K("rsel")], [K("bsel")])
                V(lambda e: e.tensor_tensor(out=bsel, in0=bsel, in1=ov, op=ALU.add), [K("bsel"), K("ov")], [K("bsel")])
                V(lambda e, k=k: e.tensor_copy(out=idx[:, k, ts], in_=bsel), [K("bsel")], [("idx", k, bi)])
            for t in range(t0, t1):
                for k in range(2):
                    D("gpsimd", "scat", lambda e, k=k, t=t: e.indirect_dma_start(out=XS, out_offset=bass.IndirectOffsetOnAxis(ap=idx[:, k, t:t + 1], axis=0), in_=U2B[:, t, :], in_offset=None, bounds_check=bnd['r'], oob_is_err=False), r=[("idx", k, bi), ("u2b", t), "XS"], w=["XSs"])

        qst = [q0, q1, q2, q3, q4, q5, q6, q7]
        ld_x5(0); ld_x5(1)
        for it in range(16 + 7):
            for off in range(7, -1, -1):
                t_ = it - off
                if 0 <= t_ < 16:
                    qst[off](t_)
            tdone = it - 7
            if tdone >= 0 and (tdone + 1) % NBT == 0:
                route_batch(tdone // NBT)
        tap("idx", idx, [("idx", k, bi) for k in range(2) for bi in range(4)])
        tap("Wk", Wk, [(n_, bi) for n_ in ("Wk0", "Wk1") for bi in range(4)])
        if stop <= 5:
            S.core_barrier(); S.emit(); return nc

        o = [0]
        LIM = P0
        wst = [salloc(16384, F32, lim=LIM) for _ in range(4)]
        wb1 = [salloc(8192, BF16, lim=LIM).rearrange("p (c n) -> p c n", c=8) for _ in range(2)]
        wb3 = [salloc(8192, BF16, lim=LIM).rearrange("p (c n) -> p c n", c=8) for _ in range(2)]
        wb2 = [salloc(8192, BF16, lim=LIM).rearrange("p (c n) -> p c n", c=4) for _ in range(2)]
        xs_sb = [salloc(2048, BF16, lim=LIM) for _ in range(3)]
        xsT = [salloc(2048, BF16, lim=LIM).rearrange("p (c q) -> p c q", c=8) for _ in range(2)]
        sh = [salloc(2048, F32, lim=LIM) for _ in range(2)]
        act = [salloc(1024, BF16, lim=LIM) for _ in range(2)]
        aT = [salloc(1024, BF16, lim=LIM).rearrange("p (c q) -> p c q", c=4) for _ in range(2)]
        ysb = [salloc(2048, BF16, lim=LIM) for _ in range(2)]
        nst = [0]

        def load_w(j):
            for (wsrc, wdst, ceng, name) in ((w1, wb1, "vector", "wb1"), (w3, wb3, "scalar", "wb3"), (w2, wb2, "vector", "wb2")):
                s = nst[0] % 4; nst[0] += 1
                d = wdst[j % 2]
                cc = 4 if name == "wb2" else 8
                src = wsrc[j].rearrange("(c p) n -> p c n", p=128)
                stv = wst[s].rearrange("p (c n) -> p c n", c=cc)
                D("sync", f"wst{s}", lambda e, stv=stv, src=src: e.dma_start(out=stv, in_=src), w=[f"wst{s}"])
                if ceng == "scalar":
                    S.add(ceng, lambda e, d=d, stv=stv: e.activation(out=d, in_=stv, func=AF.Copy), [f"wst{s}"], [(name, j % 2)])
                else:
                    S.add(ceng, lambda e, d=d, stv=stv: e.tensor_copy(out=d, in_=stv), [f"wst{s}"], [(name, j % 2)])

        def load_xs(k):
            if k >= 2 * NEH:
                return
            s = k % 3
            D("gpsimd", f"xs{s}", lambda e: e.indirect_dma_start(out=xs_sb[s], out_offset=None, in_=XS, in_offset=bass.IndirectOffsetOnAxis(ap=xblk[:, k:k + 1], axis=0), bounds_check=bnd['r'], oob_is_err=False), r=["xblk"], w=[("xs", s)])

        def p6_block(k):
            j = k // 2; ws = j % 2; s = k % 2; s3 = k % 3
            if k % 2 == 0 and j + 1 < NEH:
                load_w(j + 1)
            load_xs(k + 2)
            pt = psb(0).rearrange("p (c q) -> p c q", c=8)
            for c in range(8):
                T(lambda e, c=c: e.transpose(out=pt[:, c, :], in_=xs_sb[s3][:, c * 128:(c + 1) * 128], identity=id_b), [("xs", s3)], [("ps", 0)])
            V(lambda e: e.tensor_copy(out=xsT[s], in_=pt), [("ps", 0)], [("xsT", s)])
            for kc in range(8):
                T(lambda e, kc=kc: e.matmul(PS[:, 1, :], lhsT=xsT[s][:, kc, :], rhs=wb1[ws][:, kc, :], start=(kc == 0), stop=(kc == 7)), [("xsT", s), ("wb1", ws)], [("ps", 1)])
            for kc in range(8):
                T(lambda e, kc=kc: e.matmul(PS[:, 2, :], lhsT=xsT[s][:, kc, :], rhs=wb3[ws][:, kc, :], start=(kc == 0), stop=(kc == 7)), [("xsT", s), ("wb3", ws)], [("ps", 2)])
            A(lambda e: e.activation(out=sh[s], in_=PS[:, 1, :], func=AF.Silu), [("ps", 1)], [("sh", s)])
            V(lambda e: e.tensor_tensor(out=act[s], in0=PS[:, 2, :], in1=sh[s], op=ALU.mult), [("ps", 2), ("sh", s)], [("act", s)])
            pa = psb(3).rearrange("p (c q) -> p c q", c=8)
            for c in range(4):
                T(lambda e, c=c: e.transpose(out=pa[:, c, :], in_=act[s][:, c * 128:(c + 1) * 128], identity=id_b), [("act", s)], [("ps", 3)])
            V(lambda e: e.tensor_copy(out=aT[s], in_=pa[:, 0:4, :]), [("ps", 3)], [("aT", s)])
            for half in range(2):
                for c in range(4):
                    T(lambda e, c=c, half=half: e.matmul(PS[:, 4 + half, :], lhsT=aT[s][:, c, :], rhs=wb2[ws][:, c, half * 512:(half + 1) * 512], start=(c == 0), stop=(c == 3)), [("aT", s), ("wb2", ws)], [("ps", 4 + half)])
            yk = ("ysb", s)
            A(lambda e: e.activation(out=ysb[s][:, 0:512], in_=PS[:, 4, :], func=AF.Copy), [("ps", 4)], [(yk, 0)])
            A(lambda e: e.activation(out=ysb[s][:, 512:1024], in_=PS[:, 5, :], func=AF.Copy), [("ps", 5)], [(yk, 1)])
            D("gpsimd", f"ys{s}", lambda e: e.indirect_dma_start(out=YS, out_offset=bass.IndirectOffsetOnAxis(ap=xblk[:, k:k + 1], axis=0), in_=ysb[s], in_offset=None, bounds_check=bnd['r'], oob_is_err=False), r=[(yk, 0), (yk, 1), "xblk"], w=["YS"])

        S.core_barrier(prefetch=lambda: load_w(0))
        load_xs(0); load_xs(1)
        for k2 in range(2 * NEH):
            p6_block(k2)
        if stop <= 6:
            S.core_barrier(); S.emit(); return nc

        o = [0]
        y0 = [salloc(2048, BF16, lim=LIM) for _ in range(3)]
        y1 = [salloc(2048, BF16, lim=LIM) for _ in range(3)]
        x1b = [salloc(4096, F32, lim=LIM) for _ in range(3)]

        def ld_x7(t):
            if t < 16:
                D("sync", f"x7{t % 3}", lambda e: e.dma_start(out=x1b[t % 3], in_=out[t * 128:(t + 1) * 128, :]), w=[("x7", t % 3)])

        def gat7(t):
            if t >= 16:
                return
            s3 = t % 3
            for k, yb in enumerate((y0, y1)):
                (V if k == 0 else G)(lambda e, yb=yb: e.memset(yb[s3], 0.0), [], [("y", k, s3)])
                D("gpsimd", f"gat{k}{s3}", lambda e, k=k, yb=yb: e.indirect_dma_start(out=yb[s3], out_offset=None, in_=YS, in_offset=bass.IndirectOffsetOnAxis(ap=idx[:, k, t:t + 1], axis=0), bounds_check=bnd['r'], oob_is_err=False), r=[("y", k, s3)], w=[("y", k, s3)])

        def p7_tile(t):
            s3 = t % 3
            V(lambda e: e.scalar_tensor_tensor(out=x1b[s3], in0=y0[s3], scalar=Wk[:, 0, t:t + 1], in1=x1b[s3], op0=ALU.mult, op1=ALU.add), [("y", 0, s3), ("x7", s3)], [("x7", s3)])
            V(lambda e: e.scalar_tensor_tensor(out=x1b[s3], in0=y1[s3], scalar=Wk[:, 1, t:t + 1], in1=x1b[s3], op0=ALU.mult, op1=ALU.add), [("y", 1, s3), ("x7", s3)], [("x7", s3)])
            D("sync", f"fo{s3}", lambda e: e.dma_start(out=out[t * 128:(t + 1) * 128, :], in_=x1b[s3]), r=[("x7", s3)])
            ld_x7(t + 3)
            gat7(t + 3)

        S.core_barrier(prefetch=lambda: [ld_x7(t_) for t_ in range(3)])
        for t_ in range(3):
            gat7(t_)
        for t_ in range(16):
            p7_tile(t_)
        S.emit()
    return nc


def _rope_tables():
    S = 4096; GW = 64
    rows = S // GW
    row = np.repeat(np.arange(rows, dtype=np.float32), GW)
    col = np.tile(np.arange(GW, dtype=np.float32), rows)
    inv = (np.float32(10000.0) ** (-np.arange(0, 64, 2, dtype=np.float32) / np.float32(64))).astype(np.float32)
    ang = np.concatenate([row[:, None] * inv, col[:, None] * inv], axis=-1).astype(np.float32)
    return np.concatenate([np.cos(ang), np.sin(ang)], axis=-1).astype(np.float32)


def _consts(sp):
    c = np.zeros((128, 512), np.float32)
    c[:, 0:128] = np.eye(128, dtype=np.float32)
    c[:, 128:256] = np.triu(np.ones((128, 128), np.float32), k=1)
    c[:, 256:384] = 1.0
    c[:, 384:448] = ((sp * NEXP + np.arange(64, dtype=np.float32)) * CAP)[None, :]
    return c


def _xblk(sp):
    t = np.zeros((128, 64), np.int32)
    for k in range(64):
        j, src = k // 2, k % 2
        t[:, k] = (src * NEXP + sp * (NEXP // 2) + j) * CAP + np.arange(128)
    return t


def _core_private(inputs, c, tab):
    b, hq = c // 2, c % 2
    x = np.asarray(inputs["x"], np.float32)
    t0 = hq * 2048
    xa = np.zeros((4224, 1024), np.float32)
    xa[0:2048] = x[b, t0:t0 + 2048]
    xa[2048:4096] = x[b, (1 - hq) * 2048:(1 - hq) * 2048 + 2048]
    if t0 > 0:
        xa[4096] = x[b, t0 - 1]
    if t0 + 2048 < 4096:
        xa[4097] = x[b, t0 + 2048]
    cs = np.concatenate([tab[t0:t0 + 2048], tab[(1 - hq) * 2048:(1 - hq) * 2048 + 2048]], axis=0)
    return {"xa": xa, "cs": np.ascontiguousarray(cs)}


def make_in_maps(inputs, cores):
    f = lambda k: np.ascontiguousarray(np.asarray(inputs[k], np.float32)[0])
    shared = {
        "norm1_g": f("norm1_g"), "norm2_g": f("norm2_g"), "w_in": f("w_in"), "conv_w": f("conv_w"),
        "q_norm_g": f("q_norm_g"), "k_norm_g": f("k_norm_g"), "w_conv_out": f("w_conv_out"),
        "w_attn_out": f("w_attn_out"), "w_o": f("w_o"),
        "w_gr": np.ascontiguousarray(np.concatenate([f("w_group"), f("w_router")], axis=1)),
    }
    w1, w3, w2 = f("w1"), f("w3"), f("w2")
    halves = [{"w1": np.ascontiguousarray(w1[sp * 32:(sp + 1) * 32]), "w3": np.ascontiguousarray(w3[sp * 32:(sp + 1) * 32]),
               "w2": np.ascontiguousarray(w2[sp * 32:(sp + 1) * 32]), "consts": _consts(sp), "xblk": _xblk(sp)} for sp in range(2)]
    tab = _rope_tables()
    maps = []
    for c in cores:
        m = dict(shared)
        m.update(halves[c % 2])
        m.update(_core_private(inputs, c, tab))
        maps.append(m)
    return maps


def kernel(**inputs):
    nc = build()
    in_maps = make_in_maps(inputs, list(range(8)))
    res = run_bass_kernel_spmd(nc, in_maps, core_ids=list(range(8)))
    outp = np.zeros((4, 4096, 1024), np.float32)
    for c in range(8):
        b, hq = c // 2, c % 2
        outp[b, hq * 2048:(hq + 1) * 2048] = res.results[c]["out"]
    return outp
```

```python
import contextlib
import numpy as np
import concourse.bass as bass
import concourse.mybir as mybir
from concourse.bass_utils import run_bass_kernel_spmd

F32 = mybir.dt.float32
BF16 = mybir.dt.bfloat16
I32 = mybir.dt.int32
U8 = mybir.dt.uint8
AF = mybir.ActivationFunctionType
ALU = mybir.AluOpType
AX = mybir.AxisListType

NT_OWN = 16
NT_ALL = 32
CAP = 128
NEXP = 64
EPS = 1e-6


class Op:
    __slots__ = ("eng", "fn", "idx", "waits", "inc", "count", "is_dma", "semkey")


class Sched:
    ENGS = ("sync", "scalar", "vector", "gpsimd", "tensor")

    def __init__(self, nc):
        self.nc = nc
        self.ops = {e: [] for e in self.ENGS}
        self.tiles = {}
        self.dma_count = {}
        self.seg_marks = []

    def core_barrier(self, prefetch=None):
        self.barrier()
        if prefetch is not None:
            prefetch()
        self.seg_marks.append({e: len(self.ops[e]) for e in self.ENGS})

    def add(self, eng, fn, reads=(), writes=(), semkey=None):
        op = Op()
        op.eng = eng; op.fn = fn; op.idx = len(self.ops[eng]); op.inc = False
        op.is_dma = semkey is not None; op.semkey = semkey; op.count = None
        deps = []
        for k in reads:
            st = self.tiles.get(k)
            if st and st[0] is not None:
                deps.append(st[0])
        for k in writes:
            st = self.tiles.get(k)
            if st:
                if st[0] is not None:
                    deps.append(st[0])
                deps.extend(st[1])
        waits = {}
        for d in deps:
            if d.is_dma:
                key = ("dma", d.semkey); val = self.dma_count[d.semkey]
                waits[key] = max(waits.get(key, 0), val)
            else:
                if d.eng == "tensor" and eng == "tensor" and not op.is_dma:
                    continue
                d.inc = True
                key = ("eng", d.eng)
                cur = waits.get(key)
                if cur is None or d.idx > cur.idx:
                    waits[key] = d
        op.waits = waits
        if op.is_dma:
            self.dma_count[semkey] = self.dma_count.get(semkey, 0) + 16
        for k in reads:
            st = self.tiles.setdefault(k, [None, []])
            st[1].append(op)
        for k in writes:
            self.tiles[k] = [op, []]
        self.ops[eng].append(op)
        return op

    def barrier(self):
        last = {}
        for e in self.ENGS:
            for op in reversed(self.ops[e]):
                if not op.is_dma and op.fn is not None:
                    last[e] = op
                    break
        dma_snapshot = dict(self.dma_count)
        for e in self.ENGS:
            op = Op()
            op.eng = e; op.fn = None; op.idx = len(self.ops[e]); op.inc = False
            op.is_dma = False; op.semkey = None; op.count = None
            waits = {}
            for f, lo in last.items():
                if f == "tensor" and e == "tensor":
                    continue
                lo.inc = True
                waits[("eng", f)] = lo
            for k, v in dma_snapshot.items():
                waits[("dma", k)] = v
            op.waits = waits
            self.ops[e].append(op)
        self.tiles.clear()

    def emit(self, final_eng="gpsimd"):
        nc = self.nc
        for e in self.ENGS:
            c = 0
            for op in self.ops[e]:
                if op.inc and not op.is_dma:
                    c += 1
                    op.count = c
        semkeys = sorted(self.dma_count.keys(), key=str)
        marks = self.seg_marks + [{e: len(self.ops[e]) for e in self.ENGS}]
        with contextlib.ExitStack() as st:
            esem = {e: st.enter_context(nc.semaphore("es_" + e)) for e in self.ENGS}
            dsem = {k: st.enter_context(nc.semaphore("ds_" + str(k))) for k in semkeys}
            known = {e: {} for e in self.ENGS}

            def run(e, eng, lo, hi, last):
                kn = known[e]
                for op in self.ops[e][lo:hi]:
                    for key, val in op.waits.items():
                        if key[0] == "dma":
                            sem = dsem[key[1]]; v = val
                        else:
                            sem = esem[key[1]]; v = val.count
                        if kn.get(key, 0) >= v:
                            continue
                        kn[key] = v
                        eng.wait_ge(sem, v)
                    if op.fn is None:
                        continue
                    ins = op.fn(eng)
                    if op.is_dma:
                        ins.then_inc(dsem[op.semkey], 16)
                    elif op.inc:
                        ins.then_inc(esem[e], 1)
                if last and e == final_eng:
                    for k in semkeys:
                        if kn.get(("dma", k), 0) < self.dma_count[k]:
                            eng.wait_ge(dsem[k], self.dma_count[k])

            prev = {e: 0 for e in self.ENGS}
            for si, mk in enumerate(marks):
                last = si == len(marks) - 1
                with nc.Block() as block:
                    @block.sync
                    def _(eng): run("sync", eng, prev["sync"], mk["sync"], last)

                    @block.scalar
                    def _(eng): run("scalar", eng, prev["scalar"], mk["scalar"], last)

                    @block.vector
                    def _(eng): run("vector", eng, prev["vector"], mk["vector"], last)

                    @block.gpsimd
                    def _(eng): run("gpsimd", eng, prev["gpsimd"], mk["gpsimd"], last)

                    @block.tensor
                    def _(eng): run("tensor", eng, prev["tensor"], mk["tensor"], last)
                prev = mk
                if not last:
                    nc.all_core_barrier()


def build(stop=99, taps=(), ncores=8):
    nc = bass.Bass("TRN2", target_bir_lowering=False, num_devices=ncores)

    def din(name, shape, dt=F32):
        return nc.dram_tensor(name, list(shape), dt, kind="ExternalInput").ap()

    xa = din("xa", [4224, 1024])
    cs = din("cs", [4096, 128])
    norm1_g = din("norm1_g", [1024])
    norm2_g = din("norm2_g", [1024])
    w_in = din("w_in", [1024, 6656])
    conv_w = din("conv_w", [3, 1024])
    q_norm_g = din("q_norm_g", [128])
    k_norm_g = din("k_norm_g", [128])
    w_conv_out = din("w_conv_out", [1024, 1024])
    w_attn_out = din("w_attn_out", [1024, 1024])
    w_o = din("w_o", [1024, 1024])
    w_gr = din("w_gr", [1024, 72])
    NEH = NEXP // 2
    w1 = din("w1", [NEH, 1024, 512])
    w3 = din("w3", [NEH, 1024, 512])
    w2 = din("w2", [NEH, 512, 1024])
    xblk_d = din("xblk", [128, 64], I32)
    consts = din("consts", [128, 512])
    out = nc.dram_tensor("out", [2048, 1024], F32, kind="ExternalOutput").ap()
    NROW = 2 * NEXP * CAP
    XS = nc.dram_tensor("XS", [NROW, 1024], BF16, addr_space="Shared").ap()
    YS = nc.dram_tensor("YS", [NROW, 1024], BF16, addr_space="Shared").ap()
    tap_out = {}
    for (name, shape, dt) in taps:
        tap_out[name] = nc.dram_tensor("tap_" + name, list(shape), dt, kind="ExternalOutput").ap()

    with contextlib.ExitStack() as st:
        TOTAL = 212000
        arena = st.enter_context(nc.sbuf_tensor("arena", [128, TOTAL], U8))
        PS = st.enter_context(nc.psum_tensor("ps", [128, 8, 512], F32))
        PSb = PS[:].bitcast(BF16) if hasattr(PS[:], "bitcast") else None

        def region(off, nbytes, dt):
            assert off % 4 == 0 and off + nbytes <= TOTAL, (off, nbytes)
            return arena[:, off:off + nbytes].bitcast(dt)

        S = Sched(nc)

        def V(fn, r=(), w=()): return S.add("vector", fn, r, w)
        def A(fn, r=(), w=()): return S.add("scalar", fn, r, w)
        def G(fn, r=(), w=()): return S.add("gpsimd", fn, r, w)
        def T(fn, r=(), w=()): return S.add("tensor", fn, r, w)
        def D(q, key, fn, r=(), w=()): return S.add(q, fn, r, w, semkey=key)

        def psb(bank):
            return PSb[:, bank, :]

        P0 = 198000
        p = [P0]

        def palloc(nbytes, dt):
            r = region(p[0], nbytes, dt); p[0] += (nbytes + 3) // 4 * 4
            return r
        cst = palloc(512 * 4, F32)
        id_f = cst[:, 0:128]; ebase = cst[:, 384:448]
        cstb = palloc(384 * 2, BF16)
        id_b = cstb[:, 0:128]; tri_b = cstb[:, 128:256]; ones_b = cstb[:, 256:384]
        g1T = palloc(8 * 4, F32)
        cw = palloc(24 * 4, F32).rearrange("p (k c) -> p k c", k=3)
        gq_bc = palloc(128 * 4, F32)
        gk_bc = palloc(128 * 4, F32)
        negc = palloc(4 * 4, F32)
        wgr = palloc(8 * 72 * 4, F32).rearrange("p (c n) -> p c n", c=8)
        Wk = palloc(2 * 16 * 4, F32).rearrange("p (k t) -> p k t", k=2)
        idx = palloc(2 * 16 * 4, I32).rearrange("p (k t) -> p k t", k=2)
        Mb = palloc(16 * 64 * 2, BF16).rearrange("p (t e) -> p t e", t=16)
        g2_bc = palloc(1024 * 4, F32)
        gq_p = palloc(128 * 4, F32)
        xblk = palloc(64 * 4, I32)
        gk_p = palloc(128 * 4, F32)
        assert p[0] <= TOTAL, p[0]

        OFF_A, OFF_B, OFF_QT, OFF_KT, OFF_VX, OFF_D, OFF_W = 0, 34816, 67584, 100352, 116736, 133376, 166144
        uT = region(OFF_A, 34816, BF16).rearrange("p (c t) -> p c t", c=8)
        Bbuf = region(OFF_B, 32768, BF16).rearrange("p (c t) -> p c t", c=8)
        QT = region(OFF_QT, 32768, BF16).rearrange("p (t h q) -> p t h q", t=16, h=8)
        KT = region(OFF_KT, 16384, BF16).rearrange("p (g t) -> p g t", g=2)
        VX = region(OFF_VX, 16640, BF16).rearrange("p (t g d) -> p t g d", t=32, g=2)
        OT = region(OFF_D, 32768, BF16).rearrange("p (c t) -> p c t", c=8)
        mT = region(OFF_QT, 32768, BF16).rearrange("p (c t) -> p c t", c=8)
        Wreg = OFF_W

        D("sync", "const", lambda e: e.dma_start(out=cst, in_=consts), w=["cst"])
        D("sync", "const", lambda e: e.dma_start(out=g1T, in_=norm1_g.rearrange("(c p) -> p c", p=128), allow_slow_non_contiguous=True), w=["g1T"])
        D("sync", "const", lambda e: e.dma_start(out=cw, in_=conv_w.rearrange("k (c p) -> p k c", p=128), allow_slow_non_contiguous=True), w=["cw"])
        D("sync", "const", lambda e: e.dma_start(out=gq_bc, in_=q_norm_g.partition_broadcast(128)), w=["gq"])
        D("sync", "const", lambda e: e.dma_start(out=gk_bc, in_=k_norm_g.partition_broadcast(128)), w=["gk"])
        D("sync", "const", lambda e: e.dma_start(out=g2_bc, in_=norm2_g.partition_broadcast(128)), w=["g2"])
        D("sync", "const", lambda e: e.dma_start(out=wgr, in_=w_gr.rearrange("(c p) n -> p c n", p=128)), w=["wgr"])
        D("sync", "const", lambda e: e.dma_start(out=xblk, in_=xblk_d), w=["xblk"])
        V(lambda e: e.tensor_copy(out=cstb, in_=cst[:, 0:384]), ["cst"], ["cstb"])
        V(lambda e: e.tensor_reduce(out=negc[:, 0:1], in_=gq_bc, axis=AX.X, op=ALU.max, apply_absolute_value=True), ["gq"], ["negc0"])
        V(lambda e: e.tensor_reduce(out=negc[:, 1:2], in_=gk_bc, axis=AX.X, op=ALU.max, apply_absolute_value=True), ["gk"], ["negc1"])
        V(lambda e: e.tensor_tensor(out=negc[:, 2:3], in0=negc[:, 0:1], in1=negc[:, 1:2], op=ALU.mult), ["negc0", "negc1"], ["negc2"])
        V(lambda e: e.tensor_scalar(out=negc[:, 3:4], in0=negc[:, 2:3], scalar1=-(128.0 ** 0.5), scalar2=None, op0=ALU.mult), ["negc2"], ["negc"])
        bnd = {}

        def mk_bnd(e):
            bnd['r'] = e.alloc_register("bnd")
            return e.reg_mov(bnd['r'], NROW - 1)
        G(mk_bnd)
        G(lambda e: e.memset(VX[:, :, :, 128:130], 1.0), [], ["vx1"])

        def tap(name, src_ap, keys):
            if name in tap_out:
                D("gpsimd", "tap_" + name, lambda e: e.dma_start(out=tap_out[name], in_=src_ap), r=keys)

        wqkv = region(Wreg, 24576, BF16).rearrange("p (c n) -> p c n", c=8)
        o = [OFF_B]

        def salloc(nbytes, dt, lim=OFF_B + 32768):
            r = region(o[0], nbytes, dt); o[0] += (nbytes + 3) // 4 * 4
            assert o[0] <= lim, (o[0], lim)
            return r
        xt = [salloc(4096, F32) for _ in range(3)]
        xn = [salloc(2048, BF16) for _ in range(2)]
        junk = salloc(4096, F32)
        uTo = [salloc(2048, BF16).rearrange("p (c t) -> p c t", c=8) for _ in range(2)]
        qkc = salloc(5120, F32)
        qkc_o = salloc(1024, F32)
        o = [OFF_D]

        def salloc2(nbytes, dt):
            return salloc(nbytes, dt, lim=OFF_D + 32768)
        qn = [salloc2(5120, F32) for _ in range(2)]
        qn_o = [salloc2(1024, F32) for _ in range(2)]
        tmp4 = [salloc2(2560, F32) for _ in range(4)]
        qr = [salloc2(2560, BF16) for _ in range(2)]
        cst_t = [salloc2(512, F32) for _ in range(3)]
        st1 = salloc2(4 * 32 * 4, F32).rearrange("p (s k) -> p s k", s=4)
        epsb = salloc2(4, F32)
        stg = [region(OFF_D + 12288 + i * 6144, 6144, F32) for i in range(2)]

        V(lambda e: e.memset(epsb, EPS), [], ["eps"])
        wq_keys = [f"wqkv{kc}" for kc in range(8)]
        V(lambda e: e.tensor_scalar(out=gq_p.rearrange("p (two i) -> p two i", two=2), in0=gq_bc.rearrange("p (i two) -> p two i", two=2), scalar1=128.0 ** -0.5, scalar2=None, op0=ALU.mult), ["gq"], ["gqp"])
        V(lambda e: e.tensor_copy(out=gk_p.rearrange("p (two i) -> p two i", two=2), in_=gk_bc.rearrange("p (i two) -> p two i", two=2)), ["gk"], ["gkp"])

        tile_order = []
        for i in range(16):
            tile_order += [i, 16 + i]
        tile_order.append(32)
        NTL = len(tile_order)

        def load_x(n):
            if n >= NTL:
                return
            t = tile_order[n]; s = n % 3
            D("sync", f"xt{s}", lambda e: e.dma_start(out=xt[s], in_=xa[t * 128:(t + 1) * 128, :]), w=[f"xt{s}"])

        def udst_of(n):
            t = tile_order[n]
            if t < 16:
                return uT[:, :, t * 128:(t + 1) * 128], ("uT", t)
            if t == 32:
                return uT[:, :, 2048:2176], ("uT", 16)
            return uTo[(n // 2) % 2], ("uTo", (n // 2) % 2)

        def tinfo(n):
            t = tile_order[n]
            own = t < 16
            nq = 10 if own else 2
            return t, own, nq

        def st_S0(n):
            s = n % 3; s2 = n % 2
            xts = xt[s]; xns = xn[s2]
            ss = st1[:, n % 4, 0:1]; rstd = st1[:, n % 4, 1:2]
            sk = ("ss", n % 4)
            A(lambda e: e.activation(out=junk[:, 0:1024], in_=xts, func=AF.Square, accum_out=ss), [f"xt{s}"], [sk])
            A(lambda e: e.activation(out=rstd, in_=ss, func=AF.Ln, scale=1.0 / 1024, bias=epsb), [sk, "eps"], [sk])
            A(lambda e: e.activation(out=rstd, in_=rstd, func=AF.Exp, scale=-0.5), [sk], [sk])
            A(lambda e: e.activation(out=xns, in_=xts, func=AF.Copy, scale=rstd), [f"xt{s}", sk], [f"xn{s2}"])
            load_x(n + 2)

        def st_S1(n):
            s2 = n % 2
            xns = xn[s2]
            pt = psb(s2).rearrange("p (c q) -> p c q", c=8)
            for c in range(8):
                T(lambda e, c=c: e.transpose(out=pt[:, c, :], in_=xns[:, c * 128:(c + 1) * 128], identity=id_b), [f"xn{s2}", "cstb"], [("ps", s2)])

        def st_S2(n):
            s2 = n % 2
            pt = psb(s2).rearrange("p (c q) -> p c q", c=8)
            udst, ukey = udst_of(n)
            V(lambda e: e.tensor_tensor(out=udst, in0=pt, in1=g1T.unsqueeze(2).to_broadcast([128, 8, 128]), op=ALU.mult), [("ps", s2), "g1T"], [ukey])

        def st_S3(n):
            t, own, nq = tinfo(n)
            if t == 32:
                return
            udst, ukey = udst_of(n)
            kvb = 4 if own else 5
            for kc in range(8):
                lhs = udst[:, kc, :]
                if own:
                    T(lambda e, kc=kc, lhs=lhs: e.matmul(PS[:, 2, :], lhsT=lhs, rhs=wqkv[:, kc, 0:512], start=(kc == 0), stop=(kc == 7)), [ukey] + wq_keys, [("ps", 2)])
                    T(lambda e, kc=kc, lhs=lhs: e.matmul(PS[:, 3, :], lhsT=lhs, rhs=wqkv[:, kc, 512:1024], start=(kc == 0), stop=(kc == 7)), [ukey], [("ps", 3)])
                T(lambda e, kc=kc, lhs=lhs: e.matmul(PS[:, kvb, :], lhsT=lhs, rhs=wqkv[:, kc, 1024:1536], start=(kc == 0), stop=(kc == 7)), [ukey] + wq_keys, [("ps", kvb)])

        def st_S4(n):
            t, own, nq = tinfo(n)
            if t == 32:
                return
            s3 = n % 3
            D("sync", f"cs{s3}", lambda e: e.dma_start(out=cst_t[s3], in_=cs[t * 128:(t + 1) * 128, :]), w=[f"cs{s3}"])
            if own:
                qkps = PS[:, 2:5, :].rearrange("p b n -> p (b n)")[:, 0:1280]
                pkeys = [("ps", 2), ("ps", 3), ("ps", 4)]; kvb = 4
                cdst = qkc; ckey = "qkc"
            else:
                qkps = PS[:, 5, 0:256]
                pkeys = [("ps", 5)]; kvb = 5
                cdst = qkc_o[:, 0:256]; ckey = "qkco"
            ssq = st1[:, n % 4, 8:8 + nq]; rs = st1[:, n % 4, 20:20 + nq]
            rk = ("rs", n % 4)
            for h in range(nq):
                A(lambda e, h=h: e.activation(out=junk[:, 0:128], in_=qkps[:, h * 128:(h + 1) * 128], func=AF.Square, accum_out=ssq[:, h:h + 1]), pkeys, [rk])
            A(lambda e: e.activation(out=rs, in_=ssq, func=AF.Ln, scale=1.0 / 128, bias=epsb), [rk, "eps"], [rk])
            A(lambda e: e.activation(out=rs, in_=rs, func=AF.Exp, scale=-0.5), [rk], [rk])
            A(lambda e: e.activation(out=cdst, in_=qkps, func=AF.Copy), pkeys, [ckey])
            A(lambda e: e.activation(out=VX[:, t, :, 0:128], in_=PS[:, kvb, 256:512].rearrange("p (g d) -> p g d", g=2), func=AF.Copy), [("ps", kvb)], [("vx", t)])

        def st_S5(n):
            t, own, nq = tinfo(n)
            if t == 32:
                return
            rs = st1[:, n % 4, 20:20 + nq]; rk = ("rs", n % 4)
            if own:
                src = qkc; ckey = "qkc"; qs = qn[(n // 2) % 2]
            else:
                src = qkc_o[:, 0:256]; ckey = "qkco"; qs = qn_o[(n // 2) % 2]
            qkey = ("qn", (n // 2) % 2, own)
            src3 = src.rearrange("p (h i two) -> p h two i", i=64, two=2)
            for h in range(nq):
                gp = gq_p if (own and h < 8) else gk_p
                dst = qs[:, h * 128:(h + 1) * 128].rearrange("p (two i) -> p two i", two=2)
                V(lambda e, h=h, gp=gp, dst=dst: e.scalar_tensor_tensor(out=dst, in0=src3[:, h, :, :], scalar=rs[:, h:h + 1], in1=gp.rearrange("p (two i) -> p two i", two=2), op0=ALU.mult, op1=ALU.mult), [ckey, rk, "gqp", "gkp"], [qkey])

        def rope_views(n):
            t, own, nq = tinfo(n)
            qs = qn[(n // 2) % 2] if own else qn_o[(n // 2) % 2]
            q3 = qs[:, 0:nq * 128].rearrange("p (h d) -> p h d", d=128)
            tt = [tmp4[i][:, 0:nq * 64].rearrange("p (h i) -> p h i", i=64) for i in range(4)]
            return t, own, nq, q3, tt

        def st_S6(n):
            t, own, nq, q3, tt = rope_views(n)
            if t == 32:
                return
            s3 = n % 3
            qkey = ("qn", (n // 2) % 2, own)
            x0 = q3[:, :, 0:64]; x1 = q3[:, :, 64:128]
            cosb = cst_t[s3][:, 0:64].unsqueeze(1).to_broadcast([128, nq, 64])
            sinb = cst_t[s3][:, 64:128].unsqueeze(1).to_broadcast([128, nq, 64])
            ck = f"cs{s3}"
            V(lambda e: e.tensor_tensor(out=tt[0], in0=x0, in1=cosb, op=ALU.mult), [qkey, ck], ["t0"])
            G(lambda e: e.tensor_tensor(out=tt[1], in0=x1, in1=sinb, op=ALU.mult), [qkey, ck], ["t1"])
            G(lambda e: e.tensor_tensor(out=tt[2], in0=x0, in1=sinb, op=ALU.mult), [qkey, ck], ["t2"])
            V(lambda e: e.tensor_tensor(out=tt[3], in0=x1, in1=cosb, op=ALU.mult), [qkey, ck], ["t3"])

        def st_S7(n):
            t, own, nq, q3, tt = rope_views(n)
            if t == 32:
                return
            h0 = 10 - nq
            qrs = qr[n % 2]; qk_ = ("qr", n % 2)
            r3 = qrs[:, h0 * 128:1280].rearrange("p (h d) -> p h d", d=128)
            V(lambda e: e.tensor_tensor(out=r3[:, :, 0:64], in0=tt[0], in1=tt[1], op=ALU.subtract), ["t0", "t1"], [(qk_, 0)])
            G(lambda e: e.tensor_tensor(out=r3[:, :, 64:128], in0=tt[2], in1=tt[3], op=ALU.add), ["t2", "t3"], [(qk_, 1)])

        def st_S8(n):
            t, own, nq = tinfo(n)
            if t == 32:
                return
            qrs = qr[n % 2]; qk_ = ("qr", n % 2)
            pq = psb(6).rearrange("p (h q) -> p h q", h=8)
            pk = psb(7).rearrange("p (h q) -> p h q", h=8)
            if own:
                for h in range(8):
                    T(lambda e, h=h: e.transpose(out=pq[:, h, :], in_=qrs[:, h * 128:(h + 1) * 128], identity=id_b), [(qk_, 0), (qk_, 1)], [("ps", 6)])
            for g in range(2):
                T(lambda e, g=g: e.transpose(out=pk[:, g, :], in_=qrs[:, (8 + g) * 128:(9 + g) * 128], identity=id_b), [(qk_, 0), (qk_, 1)], [("ps", 7)])

        def st_S9(n):
            t, own, nq = tinfo(n)
            if t == 32:
                return
            pq = psb(6).rearrange("p (h q) -> p h q", h=8)
            pk = psb(7).rearrange("p (h q) -> p h q", h=8)
            if own:
                A(lambda e: e.activation(out=QT[:, t, :, :], in_=pq, func=AF.Copy), [("ps", 6)], [("QT", t)])
            V(lambda e: e.tensor_copy(out=KT[:, :, t * 128:(t + 1) * 128], in_=pk[:, 0:2, :]), [("ps", 7)], [("KT", t)])

        stages = [st_S0, st_S1, st_S2, st_S3, st_S4, st_S5, st_S6, st_S7, st_S8, st_S9]
        load_x(0); load_x(1)
        for kc in range(8):
            s = kc % 2
            D("sync", f"stg{s}", lambda e, kc=kc, s=s: e.dma_start(out=stg[s], in_=w_in[kc * 128:(kc + 1) * 128, 3072:4608]), w=[f"stg{s}"])
            if kc % 2 == 0:
                G(lambda e, kc=kc, s=s: e.tensor_copy(out=wqkv[:, kc, :], in_=stg[s]), [f"stg{s}"], [f"wqkv{kc}"])
            else:
                V(lambda e, kc=kc, s=s: e.tensor_copy(out=wqkv[:, kc, :], in_=stg[s]), [f"stg{s}"], [f"wqkv{kc}"])
        for it in range(NTL + 9):
            for off in range(9, -1, -1):
                n = it - off
                if 0 <= n < NTL:
                    stages[off](n)

        tap("uT", uT, [("uT", i) for i in range(17)])
        tap("QT", QT, [("QT", i) for i in range(16)])
        tap("KT", KT, [("KT", i) for i in range(32)])
        tap("VX", VX, [("vx", i) for i in range(32)] + ["vx1"])
        S.barrier()
        if stop <= 1:
            S.emit(); return nc

        zt = region(Wreg, 2048, BF16)
        G(lambda e: e.memset(zt, 0.0), [], ["zt"])
        for i in range(16):
            D("gpsimd", "xsz", lambda e, i=i: e.dma_start(out=XS[i * 1024:(i + 1) * 1024, :].rearrange("(a p) n -> p a n", p=128), in_=zt.unsqueeze(1).to_broadcast([128, 8, 1024])), r=["zt"], w=["XS"])

        o = [OFF_B]
        PTs = [salloc(1024, BF16) for _ in range(4)]
        Otok = [salloc(2048, BF16).rearrange("p (h d) -> p h d", h=8) for _ in range(2)]
        rd = salloc(64, F32)
        SB = [0, 1, 6]
        seq = [(qt, g, kt) for qt in range(16) for g in range(2) for kt in range(32)]
        nbias = negc[:, 3:4]

        def issue_S(i):
            qt, g, kt = seq[i]
            b = SB[i % 3]
            T(lambda e: e.matmul(PS[:, b, :].rearrange("p (h q) -> p h q", h=4), lhsT=KT[:, g, kt * 128:(kt + 1) * 128], rhs=QT[:, qt, 4 * g:4 * g + 4, :], start=True, stop=True), [], [("ps", b)])

        def oacc(qt, g, h):
            sset = (qt * 2 + g) % 2
            bank = 2 + 2 * sset + h // 2
            return PS[:, bank, :].rearrange("p (j n) -> p j n", j=2), bank, h % 2

        issue_S(0); issue_S(1)

        def p2_step(i, qt, g, kt):
            if i + 2 < len(seq):
                issue_S(i + 2)
            b = SB[i % 3]; ps_ = PTs[i % 4]; pk_ = ("PT", i % 4)
            A(lambda e: e.activation(out=ps_, in_=PS[:, b, :], func=AF.Exp, bias=nbias, scale=1.0), [("ps", b), "negc"], [pk_])
            for h in range(4):
                ov, bank, j = oacc(qt, g, h)
                T(lambda e, h=h, ov=ov, j=j: e.matmul(ov[:, j, 0:129], lhsT=ps_[:, h * 128:(h + 1) * 128], rhs=VX[:, kt, g, 0:129], start=(kt == 0 and j == 0), stop=(kt == 31), skip_group_check=True), [pk_], [("ps", bank)])
            if kt == 31:
                os_ = Otok[qt % 2]; ok_ = ("Otok", qt % 2)
                for hb in range(2):
                    ov, bank, _ = oacc(qt, g, hb * 2)
                    V(lambda e, ov=ov, hb=hb: e.reciprocal(out=rd[:, 2 * hb:2 * hb + 2], in_=ov[:, :, 128]), [("ps", bank)], [("rd", hb)])
                    V(lambda e, ov=ov, hb=hb: e.tensor_tensor(out=os_[:, 4 * g + 2 * hb:4 * g + 2 * hb + 2, :], in0=ov[:, :, 0:128], in1=rd[:, 2 * hb:2 * hb + 2].unsqueeze(2).to_broadcast([128, 2, 128]), op=ALU.mult), [("ps", bank), ("rd", hb)], [ok_])
                if g == 1:
                    pt = psb(7).rearrange("p (c q) -> p c q", c=8)
                    for c in range(8):
                        T(lambda e, c=c: e.transpose(out=pt[:, c, :], in_=os_[:, c, :], identity=id_b), [ok_], [("ps", 7)])
                    V(lambda e: e.tensor_copy(out=OT[:, :, qt * 128:(qt + 1) * 128], in_=pt), [("ps", 7)], [("OT", qt)])

        for i_, (qt_, g_, kt_) in enumerate(seq):
            p2_step(i_, qt_, g_, kt_)
        tap("OT", OT, [("OT", i) for i in range(16)])
        S.core_barrier()
        if stop <= 2:
            S.emit(); return nc

        o = [OFF_B]
        ybuf = salloc(2052 * 4, F32)
        zb = [salloc(2048, F32) for _ in range(2)]
        ccs = [salloc(2048, F32) for _ in range(2)]
        cstg3 = [salloc(4096, F32).rearrange("p (c n) -> p c n", c=8) for _ in range(3)]
        o = [Wreg + 16384]
        wcb3 = [salloc(2048, BF16, lim=P0).rearrange("p (c n) -> p c n", c=8) for _ in range(6)]
        wco = region(Wreg, 16384, BF16).rearrange("p (c n) -> p c n", c=8)
        wao = region(OFF_KT, 16384, BF16).rearrange("p (c n) -> p c n", c=8)
        wstg4 = [region(OFF_VX + i * 4096, 4096, F32) for i in range(2)]

        def load_big(wdst, wsrc, name, ceng, wstg):
            for kc in range(8):
                s = kc % 2
                D("sync", f"wstg{s}", lambda e, kc=kc, s=s: e.dma_start(out=wstg[s], in_=wsrc[kc * 128:(kc + 1) * 128, :]), w=[f"wstg{s}"])
                S.add(ceng, lambda e, kc=kc, s=s: e.tensor_copy(out=wdst[:, kc, :], in_=wstg[s]), [f"wstg{s}"], [(name, kc)])
            return [(name, kc) for kc in range(8)]
        nld = [0]

        def load_chunk(col0):
            s = nld[0] % 3; sb_ = nld[0] % 6; nld[0] += 1
            D("sync", f"cstg{s}", lambda e: e.dma_start(out=cstg3[s], in_=w_in[:, col0:col0 + 128].rearrange("(c p) n -> p c n", p=128)), w=[f"cstg{s}"])
            A(lambda e: e.activation(out=wcb3[sb_], in_=cstg3[s], func=AF.Copy), [f"cstg{s}"], [f"wcb{sb_}"])
            return wcb3[sb_], f"wcb{sb_}"

        def mm8(bank, wch, wkey, rhs_fn, rkeys, n=512):
            for kc in range(8):
                T(lambda e, kc=kc: e.matmul(PS[:, bank, 0:n], lhsT=wch[:, kc, :], rhs=rhs_fn(kc), start=(kc == 0), stop=(kc == 7)), [wkey] + rkeys, [("ps", bank)])

        ukeys_all = [("uT", i) for i in range(17)]
        chunks = [load_chunk(1024 + 0), load_chunk(2048 + 0), load_chunk(0)]
        big_steps = [(wco, w_conv_out, "wco", kc) for kc in range(8)] + [(wao, w_attn_out, "wao", kc) for kc in range(8)]

        def big_step():
            if not big_steps:
                return
            wdst, wsrc, name, kc = big_steps.pop(0)
            s = kc % 2
            D("sync", f"wstg{s}", lambda e: e.dma_start(out=wstg4[s], in_=wsrc[kc * 128:(kc + 1) * 128, :]), w=[f"wstg{s}"])
            G(lambda e: e.tensor_copy(out=wdst[:, kc, :], in_=wstg4[s]), [f"wstg{s}"], [(name, kc)])
        bk = [0]

        def nb():
            b = bk[0] % 8; bk[0] += 1
            return b
        for j in range(8):
            wcc, wcx, wcb_ = chunks
            if j + 1 < 8:
                chunks = [load_chunk(1024 + (j + 1) * 128), load_chunk(2048 + (j + 1) * 128), load_chunk((j + 1) * 128)]
            big_step(); big_step()
            for nt in range(5):
                if nt < 4:
                    rf = lambda kc, nt=nt: uT[:, kc, nt * 512:(nt + 1) * 512]; n = 512; ycol = 1 + nt * 512
                else:
                    rf = lambda kc: uT[:, kc, 2048:2050]; n = 2
                b1 = nb(); b2 = nb()
                mm8(b1, wcc[0], wcc[1], rf, [], n)
                mm8(b2, wcx[0], wcx[1], rf, [], n)
                cs_ = ccs[nt % 2]; ck = ("ccs", nt % 2)
                A(lambda e, b1=b1, n=n, cs_=cs_: e.activation(out=cs_[:, 0:n], in_=PS[:, b1, 0:n], func=AF.Copy), [("ps", b1)], [ck])
                if nt < 4:
                    V(lambda e, b2=b2, cs_=cs_, ycol=ycol: e.tensor_tensor(out=ybuf[:, ycol:ycol + 512], in0=PS[:, b2, :], in1=cs_, op=ALU.mult), [("ps", b2), ck], [("y", nt)])
                else:
                    V(lambda e, b2=b2, cs_=cs_: e.tensor_tensor(out=ybuf[:, 0:1], in0=PS[:, b2, 0:1], in1=cs_[:, 0:1], op=ALU.mult), [("ps", b2), ck], [("y", 4)])
                    V(lambda e, b2=b2, cs_=cs_: e.tensor_tensor(out=ybuf[:, 2049:2050], in0=PS[:, b2, 1:2], in1=cs_[:, 1:2], op=ALU.mult), [("ps", b2), ck], [("y", 5)])
            ykeys = [("y", i) for i in range(6)]
            for nt in range(4):
                z = zb[nt % 2]; zk = ("z", nt % 2)
                c0 = nt * 512
                V(lambda e, z=z, c0=c0, j=j: e.tensor_scalar(out=z, in0=ybuf[:, c0:c0 + 512], scalar1=cw[:, 0, j:j + 1], scalar2=None, op0=ALU.mult), ykeys + ["cw"], [zk])
                V(lambda e, z=z, c0=c0, j=j: e.scalar_tensor_tensor(out=z, in0=ybuf[:, c0 + 1:c0 + 513], scalar=cw[:, 1, j:j + 1], in1=z, op0=ALU.mult, op1=ALU.add), ykeys + [zk], [zk])
                V(lambda e, z=z, c0=c0, j=j: e.scalar_tensor_tensor(out=z, in0=ybuf[:, c0 + 2:c0 + 514], scalar=cw[:, 2, j:j + 1], in1=z, op0=ALU.mult, op1=ALU.add), ykeys + [zk], [zk])
                b3 = nb()
                mm8(b3, wcb_[0], wcb_[1], lambda kc, nt=nt: uT[:, kc, nt * 512:(nt + 1) * 512], [])
                V(lambda e, b3=b3, z=z, nt=nt, j=j: e.tensor_tensor(out=mT[:, j, nt * 512:(nt + 1) * 512], in0=PS[:, b3, :], in1=z, op=ALU.mult), [("ps", b3), zk], [("mT", j, nt)])
        tap("mT", mT, [("mT", j, nt) for j in range(8) for nt in range(4)])
        S.barrier()
        if stop <= 3:
            S.emit(); return nc

        o = [OFF_VX + 8192]
        sg = [salloc(2048, F32, lim=OFF_D) for _ in range(2)]
        tm = [salloc(2048, F32, lim=OFF_D) for _ in range(2)]
        o = [Wreg + 16384]
        cstg4 = [salloc(4096, F32, lim=P0).rearrange("p (c n) -> p c n", c=8) for _ in range(2)]
        wcb4 = [salloc(2048, BF16, lim=P0).rearrange("p (c n) -> p c n", c=8) for _ in range(2)]
        nld[0] = 0

        def load_chunk2(col0):
            s = nld[0] % 2; nld[0] += 1
            D("sync", f"cstg{s}", lambda e: e.dma_start(out=cstg4[s], in_=w_in[:, col0:col0 + 128].rearrange("(c p) n -> p c n", p=128)), w=[f"cstg{s}"])
            G(lambda e: e.tensor_copy(out=wcb4[s], in_=cstg4[s]), [f"cstg{s}"], [f"wcb{s}"])
            return wcb4[s], f"wcb{s}"
        kco = []; kao = []
        gch = [load_chunk2(4608), load_chunk2(5632)]
        def p4_step(oc, nt, wgc, wga):
            if True:
                tk = slice(nt * 512, (nt + 1) * 512)
                byc, bgc, bya, bga = nb(), nb(), nb(), nb()
                for kc in range(8):
                    T(lambda e, kc=kc, byc=byc: e.matmul(PS[:, byc, :], lhsT=wco[:, kc, oc * 128:(oc + 1) * 128], rhs=mT[:, kc, tk], start=(kc == 0), stop=(kc == 7)), kco, [("ps", byc)])
                mm8(bgc, wgc[0], wgc[1], lambda kc: uT[:, kc, tk], [])
                for kc in range(8):
                    T(lambda e, kc=kc, bya=bya: e.matmul(PS[:, bya, :], lhsT=wao[:, kc, oc * 128:(oc + 1) * 128], rhs=OT[:, kc, tk], start=(kc == 0), stop=(kc == 7)), kao, [("ps", bya)])
                mm8(bga, wga[0], wga[1], lambda kc: uT[:, kc, tk], [])
                A(lambda e, bgc=bgc: e.activation(out=sg[0], in_=PS[:, bgc, :], func=AF.Sigmoid), [("ps", bgc)], ["sg0"])
                A(lambda e, bga=bga: e.activation(out=sg[1], in_=PS[:, bga, :], func=AF.Sigmoid), [("ps", bga)], ["sg1"])
                V(lambda e, byc=byc: e.tensor_tensor(out=tm[0], in0=PS[:, byc, :], in1=sg[0], op=ALU.mult), [("ps", byc), "sg0"], ["tm0"])
                V(lambda e, bya=bya: e.tensor_tensor(out=tm[1], in0=PS[:, bya, :], in1=sg[1], op=ALU.mult), [("ps", bya), "sg1"], ["tm1"])
                G(lambda e: e.tensor_tensor(out=Bbuf[:, oc, tk], in0=tm[0], in1=tm[1], op=ALU.add), ["tm0", "tm1"], [("mg", oc, nt)])

        for oc in range(8):
            wgc, wga = gch
            for nt in range(4):
                p4_step(oc, nt, wgc, wga)
            if oc + 1 < 8:
                gch = [load_chunk2(4608 + (oc + 1) * 128), load_chunk2(5632 + (oc + 1) * 128)]
        tap("mg", Bbuf, [("mg", oc, nt) for oc in range(8) for nt in range(4)])
        S.barrier()
        if stop <= 4:
            S.emit(); return nc

        wo = region(Wreg, 16384, BF16).rearrange("p (c n) -> p c n", c=8)
        o = [OFF_A]
        xt5 = [salloc(4096, F32, lim=OFF_B) for _ in range(3)]
        x1t = [salloc(4096, F32, lim=OFF_B) for _ in range(2)]
        junk5 = salloc(4096, F32, lim=OFF_B)
        st5 = salloc(16 * 4 * 4, F32, lim=OFF_B).rearrange("p (t k) -> p t k", t=16)
        eps5 = salloc(4, F32, lim=OFF_B)
        o = [OFF_QT]
        sq2 = [salloc(4096, F32, lim=OFF_D) for _ in range(2)]
        u2f = [salloc(4096, F32, lim=OFF_D) for _ in range(2)]
        u2T = [salloc(4096, F32, lim=OFF_D).rearrange("p (c q) -> p c q", c=8) for _ in range(2)]
        LG = salloc(16 * 72 * 4, F32, lim=OFF_D).rearrange("p (t n) -> p t n", t=16)
        MA = [salloc(16 * 64 * 4, F32, lim=OFF_D).rearrange("p (t e) -> p t e", t=16) for _ in range(2)]
        wstg5 = [salloc(4096, F32, lim=OFF_D) for _ in range(3)]
        lgTs = [salloc(512, F32, lim=OFF_D) for _ in range(2)]
        RkA = salloc(16 * 64 * 4, F32, lim=OFF_D).rearrange("p (t e) -> p t e", t=16)
        T64 = salloc(16 * 64 * 4, F32, lim=OFF_D).rearrange("p (t e) -> p t e", t=16)
        rb = salloc(16 * 64 * 4, F32, lim=OFF_D).rearrange("p (k t x) -> p k t x", k=8, t=16)
        rs_ = salloc(16 * 16 * 4, F32, lim=OFF_D).rearrange("p (k t) -> p k t", k=16)
        U2B = region(OFF_D, 32768, BF16).rearrange("p (t n) -> p t n", t=16)
        kwo = []
        for kc in range(8):
            s_ = kc % 3
            D("sync", f"wstg{s_}", lambda e, kc=kc, s_=s_: e.dma_start(out=wstg5[s_], in_=w_o[kc * 128:(kc + 1) * 128, :]), w=[f"wstg{s_}"])
            if kc % 3 == 2:
                A(lambda e, kc=kc, s_=s_: e.activation(out=wo[:, kc, :], in_=wstg5[s_], func=AF.Copy), [f"wstg{s_}"], [("wo", kc)])
            else:
                (V if kc % 3 == 0 else G)(lambda e, kc=kc, s_=s_: e.tensor_copy(out=wo[:, kc, :], in_=wstg5[s_]), [f"wstg{s_}"], [("wo", kc)])
            kwo.append(("wo", kc))
        V(lambda e: e.memset(eps5, EPS), [], ["eps5"])

        def ld_x5(t):
            if t < 16:
                D("sync", f"x5{t % 3}", lambda e: e.dma_start(out=xt5[t % 3], in_=xa[t * 128:(t + 1) * 128, :]), w=[("x5", t % 3)])

        def q0(t):
            pb = 2 * (t % 2)
            for half in range(2):
                for kc in range(8):
                    T(lambda e, kc=kc, half=half: e.matmul(PS[:, pb + half, :], lhsT=Bbuf[:, kc, t * 128:(t + 1) * 128], rhs=wo[:, kc, half * 512:(half + 1) * 512], start=(kc == 0), stop=(kc == 7)), kwo, [("ps", pb + half)])

        def q1(t):
            pb = 2 * (t % 2); s = t % 2
            V(lambda e: e.tensor_tensor(out=x1t[s].rearrange("p (b n) -> p b n", b=2), in0=PS[:, pb:pb + 2, :], in1=xt5[t % 3].rearrange("p (b n) -> p b n", b=2), op=ALU.add), [("ps", pb), ("ps", pb + 1), ("x5", t % 3)], [("x1", s)])
            D("sync", f"x1o{s}", lambda e: e.dma_start(out=out[t * 128:(t + 1) * 128, :], in_=x1t[s]), r=[("x1", s)], w=[("out", t)])
            ld_x5(t + 2)

        def q2(t):
            s = t % 2
            ss = st5[:, t, 0:1]; rstd = st5[:, t, 1:2]; sk = ("st5", t)
            A(lambda e: e.activation(out=junk5, in_=x1t[s], func=AF.Square, accum_out=ss), [("x1", s)], [sk])
            A(lambda e: e.activation(out=rstd, in_=ss, func=AF.Ln, scale=1.0 / 1024, bias=eps5), [sk, "eps5"], [sk])
            A(lambda e: e.activation(out=rstd, in_=rstd, func=AF.Exp, scale=-0.5), [sk], [sk])
            A(lambda e: e.activation(out=sq2[s], in_=x1t[s], func=AF.Copy, scale=rstd), [("x1", s), sk], [("sq2", s)])

        def q3(t):
            s = t % 2
            V(lambda e: e.tensor_tensor(out=u2f[s], in0=sq2[s], in1=g2_bc, op=ALU.mult), [("sq2", s), "g2"], [("u2f", s)])

        def q4(t):
            s = t % 2
            A(lambda e: e.activation(out=U2B[:, t, :], in_=u2f[s], func=AF.Copy), [("u2f", s)], [("u2b", t)])
            for c in range(8):
                T(lambda e, c=c: e.transpose(out=PS[:, 4 + c // 4, (c % 4) * 128:(c % 4 + 1) * 128], in_=u2f[s][:, c * 128:(c + 1) * 128], identity=id_f), [("u2f", s), "cst"], [("ps", 4 + c // 4)])

        def q5(t):
            s = t % 2
            V(lambda e: e.tensor_copy(out=u2T[s], in_=PS[:, 4:6, :].rearrange("p b (c q) -> p (b c) q", c=4)), [("ps", 4), ("ps", 5)], [("u2T", s)])

        def q6(t):
            s = t % 2
            for kc in range(8):
                T(lambda e, kc=kc: e.matmul(PS[0:72, 6, 0:128], lhsT=wgr[:, kc, :], rhs=u2T[s][:, kc, :], start=(kc == 0), stop=(kc == 7)), [("u2T", s), "wgr"], [("ps", 6)])

        def q7(t):
            s = t % 2
            V(lambda e: e.tensor_copy(out=lgTs[s][0:72, :], in_=PS[0:72, 6, 0:128]), [("ps", 6)], [("lgT", s)])

        def q8(t):
            s = t % 2
            T(lambda e: e.transpose(out=PS[:, 6, 256:328], in_=lgTs[s][0:72, :], identity=id_f[0:72, 0:72]), [("lgT", s), "cst"], [("ps", 6)])

        def q9(t):
            V(lambda e: e.tensor_copy(out=LG[:, t, :], in_=PS[:, 6, 256:328]), [("ps", 6)], [("lg", t)])

        tri_f = cst[:, 128:256]; ones_f = cst[:, 256:384]
        NBT = 4

        def route_batch(bi):
            t0 = bi * NBT; t1 = t0 + NBT; nb_ = NBT
            ts = slice(t0, t1)
            K = lambda name: (name, bi)
            lgk = [("lg", t) for t in range(t0, t1)]
            lgg = LG[:, ts, 0:8]
            lge = LG[:, ts, 8:72].rearrange("p t (g x) -> p t g x", g=8)
            gm, nv1, v2, se, pg, dd, ed, w1_ = (rs_[:, i, ts] for i in range(8))
            ohg, eg, sel, oh1, sel2, oh2 = (rb[:, i, ts, :] for i in range(6))

            def bc3(a):
                return a.unsqueeze(2).to_broadcast([128, nb_, 8])
            V(lambda e: e.tensor_reduce(out=gm, in_=lgg, axis=AX.X, op=ALU.max), lgk, [K("gm")])
            V(lambda e: e.tensor_tensor(out=ohg, in0=lgg, in1=bc3(gm), op=ALU.is_equal), lgk + [K("gm")], [K("ohg")])
            V(lambda e: e.tensor_tensor(out=eg, in0=lgg, in1=bc3(gm), op=ALU.subtract), lgk + [K("gm")], [K("eg")])
            A(lambda e: e.activation(out=eg, in_=eg, func=AF.Exp), [K("eg")], [K("eg")])
            V(lambda e: e.tensor_reduce(out=se, in_=eg, axis=AX.X, op=ALU.add), [K("eg")], [K("se")])
            V(lambda e: e.reciprocal(out=pg, in_=se), [K("se")], [K("pg")])
            T64b = T64[:, ts, :]
            V(lambda e: e.tensor_tensor(out=T64b.rearrange("p t (g x) -> p t g x", g=8), in0=lge, in1=ohg.unsqueeze(3).to_broadcast([128, nb_, 8, 8]), op=ALU.mult), lgk + [K("ohg")], [K("t64")])
            V(lambda e: e.tensor_reduce(out=sel, in_=T64b.rearrange("p t (g x) -> p t x g", g=8), axis=AX.X, op=ALU.add), [K("t64")], [K("sel")])
            V(lambda e: e.tensor_reduce(out=nv1, in_=sel, axis=AX.X, op=ALU.max), [K("sel")], [K("v1")])
            V(lambda e: e.tensor_tensor(out=oh1, in0=sel, in1=bc3(nv1), op=ALU.is_equal), [K("sel"), K("v1")], [K("oh1")])
            V(lambda e: e.scalar_tensor_tensor(out=sel2, in0=oh1, scalar=-1e30, in1=sel, op0=ALU.mult, op1=ALU.add), [K("oh1"), K("sel")], [K("sel2")])
            V(lambda e: e.tensor_reduce(out=v2, in_=sel2, axis=AX.X, op=ALU.max), [K("sel2")], [K("v2")])
            V(lambda e: e.tensor_tensor(out=oh2, in0=sel2, in1=bc3(v2), op=ALU.is_equal), [K("sel2"), K("v2")], [K("oh2")])
            V(lambda e: e.tensor_tensor(out=dd, in0=v2, in1=nv1, op=ALU.subtract), [K("v1"), K("v2")], [K("dd")])
            A(lambda e: e.activation(out=ed, in_=dd, func=AF.Exp), [K("dd")], [K("ed")])
            V(lambda e: e.tensor_scalar(out=w1_, in0=ed, scalar1=1.0, scalar2=None, op0=ALU.add), [K("ed")], [K("w1_")])
            V(lambda e: e.reciprocal(out=w1_, in_=w1_), [K("w1_")], [K("w1_")])
            V(lambda e: e.tensor_tensor(out=Wk[:, 0, ts], in0=w1_, in1=pg, op=ALU.mult), [K("w1_"), K("pg")], [K("Wk0")])
            V(lambda e: e.tensor_tensor(out=Wk[:, 1, ts], in0=Wk[:, 0, ts], in1=ed, op=ALU.mult), [K("Wk0"), K("ed")], [K("Wk1")])
            for k, ohk in enumerate((oh1, oh2)):
                V(lambda e, k=k, ohk=ohk: e.tensor_tensor(out=MA[k][:, ts, :].rearrange("p t (g x) -> p t g x", g=8), in0=ohg.unsqueeze(3).to_broadcast([128, nb_, 8, 8]), in1=ohk.unsqueeze(2).to_broadcast([128, nb_, 8, 8]), op=ALU.mult), [K("ohg"), K("oh1"), K("oh2")], [("MA", k, bi)])
            V(lambda e: e.tensor_tensor(out=Mb[:, ts, :], in0=MA[0][:, ts, :], in1=MA[1][:, ts, :], op=ALU.add), [("MA", 0, bi), ("MA", 1, bi)], [("Mf", bi)])
            mfk = [("Mf", j) for j in range(bi + 1)]
            first = True
            for t in range(t0, t1):
                c0 = (t - t0) * 64
                T(lambda e, t=t, c0=c0, first=first: e.matmul(PS[:, 7, c0:c0 + 64], lhsT=tri_b, rhs=Mb[:, t, :], start=first, stop=(t == 0), skip_group_check=True), mfk + ["cstb"], [("ps", 7)])
                first = False
                for i in range(t):
                    T(lambda e, i=i, c0=c0, t=t: e.matmul(PS[:, 7, c0:c0 + 64], lhsT=ones_b, rhs=Mb[:, i, :], start=False, stop=(i == t - 1), skip_group_check=True), mfk + ["cstb"], [("ps", 7)])
            RkB = RkA[:, ts, :]
            V(lambda e: e.tensor_copy(out=RkB.rearrange("p t e -> p (t e)"), in_=PS[:, 7, 0:nb_ * 64]), [("ps", 7)], [K("Rk")])
            rsel, bsel, ov = (rs_[:, 8 + i, ts] for i in range(3))
            for k in range(2):
                V(lambda e, k=k: e.tensor_tensor(out=T64b, in0=RkB, in1=MA[k][:, ts, :], op=ALU.mult), [K("Rk"), ("MA", k, bi)], [K("t64")])
                V(lambda e: e.tensor_reduce(out=rsel, in_=T64b, axis=AX.X, op=ALU.add), [K("t64")], [K("rsel")])
                V(lambda e, k=k: e.tensor_tensor(out=T64b, in0=MA[k][:, ts, :], in1=ebase.unsqueeze(1).to_broadcast([128, nb_, 64]), op=ALU.mult), [("MA", k, bi), "cst", K("rsel")], [K("t64")])
                V(lambda e: e.tensor_reduce(out=bsel, in_=T64b, axis=AX.X, op=ALU.add), [K("t64")], [K("bsel")])
                V(lambda e: e.tensor_scalar(out=ov, in0=rsel, scalar1=float(CAP) - 0.5, scalar2=100000.0, op0=ALU.is_ge, op1=ALU.mult), [K("rsel")], [K("ov")])
                V(lambda e: e.tensor_tensor(out=bsel, in0=bsel, in1=rsel, op=ALU.add), [K("bsel"), K("rsel")], [K("bsel")])
                V(lambda e: e.tensor_tensor(out=bsel, in0=bsel, in1=ov, op=ALU.add), [K("bsel"), K("ov")], [K("bsel")])
                V(lambda e, k=k: e.tensor_copy(out=idx[:, k, ts], in_=bsel), [K("bsel")], [("idx", k, bi)])
            for t in range(t0, t1):
                for k in range(2):
                    D("gpsimd", "scat", lambda e, k=k, t=t: e.indirect_dma_start(out=XS, out_offset=bass.IndirectOffsetOnAxis(ap=idx[:, k, t:t + 1], axis=0), in_=U2B[:, t, :], in_offset=None, bounds_check=bnd['r'], oob_is_err=False), r=[("idx", k, bi), ("u2b", t)])

        qst = [q0, q1, q2, q3, q4, q5, q6, q7, q8, q9]
        ld_x5(0); ld_x5(1)
        for it in range(16 + 9):
            for off in range(9, -1, -1):
                t_ = it - off
                if 0 <= t_ < 16:
                    qst[off](t_)
            tdone = it - 9
            if tdone >= 0 and (tdone + 1) % NBT == 0:
                route_batch(tdone // NBT)
        tap("idx", idx, [("idx", k, bi) for k in range(2) for bi in range(4)])
        tap("Wk", Wk, [(n_, bi) for n_ in ("Wk0", "Wk1") for bi in range(4)])
        if stop <= 5:
            S.core_barrier(); S.emit(); return nc

        o = [0]
        LIM = P0
        wst = [salloc(16384, F32, lim=LIM) for _ in range(4)]
        wb1 = [salloc(8192, BF16, lim=LIM).rearrange("p (c n) -> p c n", c=8) for _ in range(2)]
        wb3 = [salloc(8192, BF16, lim=LIM).rearrange("p (c n) -> p c n", c=8) for _ in range(2)]
        wb2 = [salloc(8192, BF16, lim=LIM).rearrange("p (c n) -> p c n", c=4) for _ in range(2)]
        xs_sb = [salloc(2048, BF16, lim=LIM) for _ in range(3)]
        xsT = [salloc(2048, BF16, lim=LIM).rearrange("p (c q) -> p c q", c=8) for _ in range(2)]
        sh = [salloc(2048, F32, lim=LIM) for _ in range(2)]
        act = [salloc(1024, BF16, lim=LIM) for _ in range(2)]
        aT = [salloc(1024, BF16, lim=LIM).rearrange("p (c q) -> p c q", c=4) for _ in range(2)]
        ysb = [salloc(2048, BF16, lim=LIM) for _ in range(2)]
        nst = [0]

        def load_w(j):
            for (wsrc, wdst, ceng, name) in ((w1, wb1, "vector", "wb1"), (w3, wb3, "scalar", "wb3"), (w2, wb2, "vector", "wb2")):
                s = nst[0] % 4; nst[0] += 1
                d = wdst[j % 2]
                cc = 4 if name == "wb2" else 8
                src = wsrc[j].rearrange("(c p) n -> p c n", p=128)
                stv = wst[s].rearrange("p (c n) -> p c n", c=cc)
                D("sync", f"wst{s}", lambda e, stv=stv, src=src: e.dma_start(out=stv, in_=src), w=[f"wst{s}"])
                if ceng == "scalar":
                    S.add(ceng, lambda e, d=d, stv=stv: e.activation(out=d, in_=stv, func=AF.Copy), [f"wst{s}"], [(name, j % 2)])
                else:
                    S.add(ceng, lambda e, d=d, stv=stv: e.tensor_copy(out=d, in_=stv), [f"wst{s}"], [(name, j % 2)])

        def load_xs(k):
            if k >= 2 * NEH:
                return
            s = k % 3
            D("gpsimd", f"xs{s}", lambda e: e.indirect_dma_start(out=xs_sb[s], out_offset=None, in_=XS, in_offset=bass.IndirectOffsetOnAxis(ap=xblk[:, k:k + 1], axis=0), bounds_check=bnd['r'], oob_is_err=False), r=["xblk"], w=[("xs", s)])

        def p6_block(k):
            j = k // 2; ws = j % 2; s = k % 2; s3 = k % 3
            if k % 2 == 0 and j + 1 < NEH:
                load_w(j + 1)
            load_xs(k + 2)
            pt = psb(0).rearrange("p (c q) -> p c q", c=8)
            for c in range(8):
                T(lambda e, c=c: e.transpose(out=pt[:, c, :], in_=xs_sb[s3][:, c * 128:(c + 1) * 128], identity=id_b), [("xs", s3)], [("ps", 0)])
            V(lambda e: e.tensor_copy(out=xsT[s], in_=pt), [("ps", 0)], [("xsT", s)])
            for kc in range(8):
                T(lambda e, kc=kc: e.matmul(PS[:, 1, :], lhsT=xsT[s][:, kc, :], rhs=wb1[ws][:, kc, :], start=(kc == 0), stop=(kc == 7)), [("xsT", s), ("wb1", ws)], [("ps", 1)])
            for kc in range(8):
                T(lambda e, kc=kc: e.matmul(PS[:, 2, :], lhsT=xsT[s][:, kc, :], rhs=wb3[ws][:, kc, :], start=(kc == 0), stop=(kc == 7)), [("xsT", s), ("wb3", ws)], [("ps", 2)])
            A(lambda e: e.activation(out=sh[s], in_=PS[:, 1, :], func=AF.Silu), [("ps", 1)], [("sh", s)])
            V(lambda e: e.tensor_tensor(out=act[s], in0=PS[:, 2, :], in1=sh[s], op=ALU.mult), [("ps", 2), ("sh", s)], [("act", s)])
            pa = psb(3).rearrange("p (c q) -> p c q", c=8)
            for c in range(4):
                T(lambda e, c=c: e.transpose(out=pa[:, c, :], in_=act[s][:, c * 128:(c + 1) * 128], identity=id_b), [("act", s)], [("ps", 3)])
            V(lambda e: e.tensor_copy(out=aT[s], in_=pa[:, 0:4, :]), [("ps", 3)], [("aT", s)])
            for half in range(2):
                for c in range(4):
                    T(lambda e, c=c, half=half: e.matmul(PS[:, 4 + half, :], lhsT=aT[s][:, c, :], rhs=wb2[ws][:, c, half * 512:(half + 1) * 512], start=(c == 0), stop=(c == 3)), [("aT", s), ("wb2", ws)], [("ps", 4 + half)])
            yk = ("ysb", s)
            A(lambda e: e.activation(out=ysb[s][:, 0:512], in_=PS[:, 4, :], func=AF.Copy), [("ps", 4)], [(yk, 0)])
            A(lambda e: e.activation(out=ysb[s][:, 512:1024], in_=PS[:, 5, :], func=AF.Copy), [("ps", 5)], [(yk, 1)])
            D("gpsimd", f"ys{s}", lambda e: e.indirect_dma_start(out=YS, out_offset=bass.IndirectOffsetOnAxis(ap=xblk[:, k:k + 1], axis=0), in_=ysb[s], in_offset=None, bounds_check=bnd['r'], oob_is_err=False), r=[(yk, 0), (yk, 1), "xblk"])

        S.core_barrier(prefetch=lambda: load_w(0))
        load_xs(0); load_xs(1)
        for k2 in range(2 * NEH):
            p6_block(k2)
        if stop <= 6:
            S.core_barrier(); S.emit(); return nc

        o = [0]
        y0 = [salloc(2048, BF16, lim=LIM) for _ in range(3)]
        y1 = [salloc(2048, BF16, lim=LIM) for _ in range(3)]
        x1b = [salloc(4096, F32, lim=LIM) for _ in range(3)]

        def ld_x7(t):
            if t < 16:
                D("sync", f"x7{t % 3}", lambda e: e.dma_start(out=x1b[t % 3], in_=out[t * 128:(t + 1) * 128, :]), w=[("x7", t % 3)])

        def gat7(t):
            if t >= 16:
                return
            s3 = t % 3
            for k, yb in enumerate((y0, y1)):
                (V if k == 0 else G)(lambda e, yb=yb: e.memset(yb[s3], 0.0), [], [("y", k, s3)])
                D("gpsimd", f"gat{k}{s3}", lambda e, k=k, yb=yb: e.indirect_dma_start(out=yb[s3], out_offset=None, in_=YS, in_offset=bass.IndirectOffsetOnAxis(ap=idx[:, k, t:t + 1], axis=0), bounds_check=bnd['r'], oob_is_err=False), r=[("y", k, s3)], w=[("y", k, s3)])

        def p7_tile(t):
            s3 = t % 3
            V(lambda e: e.scalar_tensor_tensor(out=x1b[s3], in0=y0[s3], scalar=Wk[:, 0, t:t + 1], in1=x1b[s3], op0=ALU.mult, op1=ALU.add), [("y", 0, s3), ("x7", s3)], [("x7", s3)])
            V(lambda e: e.scalar_tensor_tensor(out=x1b[s3], in0=y1[s3], scalar=Wk[:, 1, t:t + 1], in1=x1b[s3], op0=ALU.mult, op1=ALU.add), [("y", 1, s3), ("x7", s3)], [("x7", s3)])
            D("sync", f"fo{s3}", lambda e: e.dma_start(out=out[t * 128:(t + 1) * 128, :], in_=x1b[s3]), r=[("x7", s3)])
            ld_x7(t + 3)
            gat7(t + 3)

        S.core_barrier(prefetch=lambda: [ld_x7(t_) for t_ in range(3)])
        for t_ in range(3):
            gat7(t_)
        for t_ in range(16):
            p7_tile(t_)
        S.emit()
    return nc


def _rope_tables():
    S = 4096; GW = 64
    rows = S // GW
    row = np.repeat(np.arange(rows, dtype=np.float32), GW)
    col = np.tile(np.arange(GW, dtype=np.float32), rows)
    inv = (np.float32(10000.0) ** (-np.arange(0, 64, 2, dtype=np.float32) / np.float32(64))).astype(np.float32)
    ang = np.concatenate([row[:, None] * inv, col[:, None] * inv], axis=-1).astype(np.float32)
    return np.concatenate([np.cos(ang), np.sin(ang)], axis=-1).astype(np.float32)


def _consts(sp):
    c = np.zeros((128, 512), np.float32)
    c[:, 0:128] = np.eye(128, dtype=np.float32)
    c[:, 128:256] = np.triu(np.ones((128, 128), np.float32), k=1)
    c[:, 256:384] = 1.0
    c[:, 384:448] = ((sp * NEXP + np.arange(64, dtype=np.float32)) * CAP)[None, :]
    return c


def _xblk(sp):
    t = np.zeros((128, 64), np.int32)
    for k in range(64):
        j, src = k // 2, k % 2
        t[:, k] = (src * NEXP + sp * (NEXP // 2) + j) * CAP + np.arange(128)
    return t


def _core_private(inputs, c, tab):
    b, hq = c // 2, c % 2
    x = np.asarray(inputs["x"], np.float32)
    t0 = hq * 2048
    xa = np.zeros((4224, 1024), np.float32)
    xa[0:2048] = x[b, t0:t0 + 2048]
    xa[2048:4096] = x[b, (1 - hq) * 2048:(1 - hq) * 2048 + 2048]
    if t0 > 0:
        xa[4096] = x[b, t0 - 1]
    if t0 + 2048 < 4096:
        xa[4097] = x[b, t0 + 2048]
    cs = np.concatenate([tab[t0:t0 + 2048], tab[(1 - hq) * 2048:(1 - hq) * 2048 + 2048]], axis=0)
    return {"xa": xa, "cs": np.ascontiguousarray(cs)}


def make_in_maps(inputs, cores):
    f = lambda k: np.ascontiguousarray(np.asarray(inputs[k], np.float32)[0])
    shared = {
        "norm1_g": f("norm1_g"), "norm2_g": f("norm2_g"), "w_in": f("w_in"), "conv_w": f("conv_w"),
        "q_norm_g": f("q_norm_g"), "k_norm_g": f("k_norm_g"), "w_conv_out": f("w_conv_out"),
        "w_attn_out": f("w_attn_out"), "w_o": f("w_o"),
        "w_gr": np.ascontiguousarray(np.concatenate([f("w_group"), f("w_router")], axis=1)),
    }
    w1, w3, w2 = f("w1"), f("w3"), f("w2")
    halves = [{"w1": np.ascontiguousarray(w1[sp * 32:(sp + 1) * 32]), "w3": np.ascontiguousarray(w3[sp * 32:(sp + 1) * 32]),
               "w2": np.ascontiguousarray(w2[sp * 32:(sp + 1) * 32]), "consts": _consts(sp), "xblk": _xblk(sp)} for sp in range(2)]
    tab = _rope_tables()
    maps = []
    for c in cores:
        m = dict(shared)
        m.update(halves[c % 2])
        m.update(_core_private(inputs, c, tab))
        maps.append(m)
    return maps


def kernel(**inputs):
    nc = build()
    in_maps = make_in_maps(inputs, list(range(8)))
    res = run_bass_kernel_spmd(nc, in_maps, core_ids=list(range(8)))
    outp = np.zeros((4, 4096, 1024), np.float32)
    for c in range(8):
        b, hq = c // 2, c % 2
        outp[b, hq * 2048:(hq + 1) * 2048] = res.results[c]["out"]
    return outp
```
